# Optimizing a Trainium2 kernel written in Bass

```python
import math
import jax, jax.numpy as jnp
from jax import lax
import numpy as np

D_MODEL = 1024
BATCH = 4
SEQ = 8192
DEPTH = 4

N_MIXERS = 2
N_ATTN_LAYERS = (DEPTH + 1) // 2
N_POOL_LAYERS = DEPTH // 2
N_HEADS = 8
HEAD_DIM = D_MODEL // (2 * N_HEADS)
V_DIM = 2 * HEAD_DIM
QK_WIDTH = 2 * N_HEADS * HEAD_DIM
ROT_DIM = HEAD_DIM // 4
ROPE_THETA = 500000.0
Q_BLOCK = 128
SUBLN_EPS = 1e-5
POOL_WINDOWS = (2, 4, 8, 16)
N_POOL_GROUPS = len(POOL_WINDOWS)
POOL_GROUP_DIM = D_MODEL // N_POOL_GROUPS
N_GROUPS = 4
EXPERTS_PER_GROUP = 8
N_EXPERTS = N_GROUPS * EXPERTS_PER_GROUP
TOP_K = 2
D_EXPERT = D_MODEL // 2
MOE_BLOCK = 128
ROUTER_BIAS_SCALE = 0.01
DEEPNORM_ALPHA = (2 * DEPTH) ** 0.25
DEEPNORM_BETA = (8 * DEPTH) ** -0.25
LN_EPS = 1e-5

kernel_name = "hybrid_diffattn_pool_hiermoe_deepnorm"


def layer_norm(x, g, b):
    xf = x.astype(jnp.float32)
    mu = jnp.mean(xf, -1, keepdims=True)
    var = jnp.mean(jnp.square(xf - mu), -1, keepdims=True)
    return ((xf - mu) * lax.rsqrt(var + LN_EPS) * g + b).astype(x.dtype)


def rotary_tables(seq):
    inv = ROPE_THETA ** (-jnp.arange(0, ROT_DIM, 2, dtype=jnp.float32) / ROT_DIM)
    ang = jnp.arange(seq, dtype=jnp.float32)[:, None] * inv[None, :]
    return jnp.cos(ang), jnp.sin(ang)


def apply_partial_rope(t, cos, sin):
    half = ROT_DIM // 2
    shape = (1, cos.shape[0]) + (1,) * (t.ndim - 3) + (half,)
    c = cos.reshape(shape).astype(t.dtype)
    s = sin.reshape(shape).astype(t.dtype)
    r1, r2, rest = t[..., :half], t[..., half:ROT_DIM], t[..., ROT_DIM:]
    return jnp.concatenate([r1 * c - r2 * s, r1 * s + r2 * c, rest], axis=-1)


def diff_attention(x, w_qkv, w_o, lq1, lk1, lq2, lk2, sub_g, lambda_init, cos, sin):
    B, S, _ = x.shape
    qkv = x @ w_qkv
    q, k, v = jnp.split(qkv, [QK_WIDTH, 2 * QK_WIDTH], axis=-1)
    q = apply_partial_rope(q.reshape(B, S, N_HEADS, 2, HEAD_DIM), cos, sin)
    k = apply_partial_rope(k.reshape(B, S, N_HEADS, 2, HEAD_DIM), cos, sin)
    q = q.transpose(0, 2, 3, 1, 4)
    k = k.transpose(0, 2, 3, 1, 4)
    v = v.reshape(B, S, N_HEADS, V_DIM).transpose(0, 2, 1, 3)
    lam = (jnp.exp(jnp.sum(lq1 * lk1).astype(jnp.float32))
           - jnp.exp(jnp.sum(lq2 * lk2).astype(jnp.float32)) + lambda_init)
    scale = HEAD_DIM ** -0.5
    outs = []
    for blk in range(S // Q_BLOCK):
        lo, hi = blk * Q_BLOCK, (blk + 1) * Q_BLOCK
        qb = q[:, :, :, lo:hi]
        kb = k[:, :, :, :hi]
        vb = v[:, :, :hi]
        s = jnp.einsum('bhmqd,bhmkd->bhmqk', qb, kb).astype(jnp.float32) * scale
        mask = jnp.arange(lo, hi)[:, None] >= jnp.arange(hi)[None, :]
        p = jax.nn.softmax(jnp.where(mask, s, -jnp.inf), axis=-1)
        a = (p[:, :, 0] - lam * p[:, :, 1]).astype(v.dtype)
        outs.append(jnp.einsum('bhqk,bhkd->bhqd', a, vb))
    o = jnp.concatenate(outs, axis=2).astype(jnp.float32)
    o = o * lax.rsqrt(jnp.mean(jnp.square(o), -1, keepdims=True) + SUBLN_EPS) * sub_g
    o = (o * (1.0 - lambda_init)).astype(x.dtype)
    o = o.transpose(0, 2, 1, 3).reshape(B, S, N_HEADS * V_DIM)
    return o @ w_o


def pool_mixer(x, w_in, w_grp, ls, w_out):
    B, S, _ = x.shape
    u = (x @ w_in).reshape(B, S, N_POOL_GROUPS, POOL_GROUP_DIM)
    count = jnp.arange(1, S + 1, dtype=jnp.float32)
    feats = []
    for g, w in enumerate(POOL_WINDOWS):
        ug = u[:, :, g].astype(jnp.float32)
        cs = jnp.cumsum(ug, axis=1)
        lag = jnp.pad(cs[:, :S - w], ((0, 0), (w, 0), (0, 0)))
        mean = (cs - lag) / jnp.minimum(count, float(w))[None, :, None]
        feats.append(mean - ug)
    p = jnp.stack(feats, axis=2).astype(x.dtype)
    y = jnp.einsum('bsgc,gcd->bsgd', p, w_grp).reshape(B, S, D_MODEL) * ls
    return y @ w_out


def hier_moe(x, w_grp_router, b_grp_router, w_exp_router, b_exp_router, w_gate, w_up, w_down):
    B, S, D = x.shape
    N = B * S
    xf = x.reshape(N, D)
    g_logits = (xf @ w_grp_router + b_grp_router).astype(jnp.float32)
    g_idx = jnp.argmax(g_logits, axis=-1)
    g_p = jnp.take_along_axis(jax.nn.softmax(g_logits, -1), g_idx[:, None], -1)
    e_logits = (xf @ w_exp_router + b_exp_router).astype(jnp.float32)
    e_logits = e_logits.reshape(N, N_GROUPS, EXPERTS_PER_GROUP)
    e_in = jnp.take_along_axis(e_logits, g_idx[:, None, None], axis=1)[:, 0]
    top_v, top_i = lax.top_k(e_in, TOP_K)
    gate = g_p * jax.nn.softmax(top_v, axis=-1)
    expert_id = g_idx[:, None] * EXPERTS_PER_GROUP + top_i
    A = N * TOP_K
    eid = expert_id.reshape(A)
    tok = jnp.repeat(jnp.arange(N, dtype=jnp.int32), TOP_K)
    wts = gate.reshape(A)
    order = jnp.argsort(eid)
    eid_s, tok_s, w_s = eid[order], tok[order], wts[order]
    counts = jnp.bincount(eid, length=N_EXPERTS)
    starts = jnp.cumsum(counts) - counts
    padded = (counts + MOE_BLOCK - 1) // MOE_BLOCK * MOE_BLOCK
    pad_ends = jnp.cumsum(padded)
    pad_starts = pad_ends - padded
    dest = pad_starts[eid_s] + jnp.arange(A) - starts[eid_s]
    P = A + N_EXPERTS * MOE_BLOCK
    NB = P // MOE_BLOCK
    tok_pad = jnp.zeros((P,), jnp.int32).at[dest].set(tok_s)
    w_pad = jnp.zeros((P,), wts.dtype).at[dest].set(w_s)
    blk_e = jnp.minimum(jnp.searchsorted(pad_ends, jnp.arange(NB) * MOE_BLOCK, side='right'),
                        N_EXPERTS - 1)
    xin = xf[tok_pad].reshape(NB, MOE_BLOCK, D)

    def run_block(args):
        xb, e = args
        h = jax.nn.silu(xb @ w_gate[e]) * (xb @ w_up[e])
        return h @ w_down[e]

    y = lax.map(run_block, (xin, blk_e)).reshape(P, D)
    out = jnp.zeros((N, D), x.dtype).at[tok_pad].add(y * w_pad[:, None].astype(y.dtype))
    return out.reshape(B, S, D)


def setup_inputs(seed: int = 0) -> dict:
    key = jax.random.key(seed)
    ks = jax.random.split(key, 24)
    f32 = jnp.float32
    nrm = lambda k, shp, sc: jax.random.normal(k, shp, f32) * sc
    D = D_MODEL
    w_qk = nrm(ks[1], (N_ATTN_LAYERS, D, 2 * QK_WIDTH), D ** -0.5)
    w_v = nrm(ks[2], (N_ATTN_LAYERS, D, N_HEADS * V_DIM), D ** -0.5) * DEEPNORM_BETA
    return {
        "x": jax.random.normal(ks[0], (BATCH, SEQ, D), f32),
        "attn_w_qkv": jnp.concatenate([w_qk, w_v], axis=-1),
        "attn_w_o": nrm(ks[3], (N_ATTN_LAYERS, N_HEADS * V_DIM, D), (N_HEADS * V_DIM) ** -0.5) * DEEPNORM_BETA,
        "attn_lq1": nrm(ks[4], (N_ATTN_LAYERS, HEAD_DIM), 0.1),
        "attn_lk1": nrm(ks[5], (N_ATTN_LAYERS, HEAD_DIM), 0.1),
        "attn_lq2": nrm(ks[6], (N_ATTN_LAYERS, HEAD_DIM), 0.1),
        "attn_lk2": nrm(ks[7], (N_ATTN_LAYERS, HEAD_DIM), 0.1),
        "attn_sub_g": 1.0 + nrm(ks[8], (N_ATTN_LAYERS, V_DIM), 0.02),
        "pool_w_in": nrm(ks[9], (N_POOL_LAYERS, D, D), D ** -0.5),
        "pool_w_grp": nrm(ks[10], (N_POOL_LAYERS, N_POOL_GROUPS, POOL_GROUP_DIM, POOL_GROUP_DIM), POOL_GROUP_DIM ** -0.5),
        "pool_scale": 1.0 + nrm(ks[11], (N_POOL_LAYERS, D), 0.02),
        "pool_w_out": nrm(ks[12], (N_POOL_LAYERS, D, D), D ** -0.5) * DEEPNORM_BETA,
        "ln_g": 1.0 + nrm(ks[13], (DEPTH, 2, D), 0.02),
        "ln_b": nrm(ks[14], (DEPTH, 2, D), 0.02),
        "moe_w_grp_router": nrm(ks[15], (DEPTH, D, N_GROUPS), D ** -0.5),
        "moe_b_grp_router": nrm(ks[16], (DEPTH, N_GROUPS), ROUTER_BIAS_SCALE),
        "moe_w_exp_router": nrm(ks[17], (DEPTH, D, N_EXPERTS), D ** -0.5),
        "moe_b_exp_router": nrm(ks[18], (DEPTH, N_EXPERTS), ROUTER_BIAS_SCALE),
        "moe_w_gate": nrm(ks[19], (DEPTH, N_EXPERTS, D, D_EXPERT), D ** -0.5),
        "moe_w_up": nrm(ks[20], (DEPTH, N_EXPERTS, D, D_EXPERT), D ** -0.5),
        "moe_w_down": nrm(ks[21], (DEPTH, N_EXPERTS, D_EXPERT, D), D_EXPERT ** -0.5) * DEEPNORM_BETA,
    }


def reference(x, attn_w_qkv, attn_w_o, attn_lq1, attn_lk1, attn_lq2, attn_lk2, attn_sub_g,
              pool_w_in, pool_w_grp, pool_scale, pool_w_out, ln_g, ln_b,
              moe_w_grp_router, moe_b_grp_router, moe_w_exp_router, moe_b_exp_router,
              moe_w_gate, moe_w_up, moe_w_down):
    cos, sin = rotary_tables(x.shape[1])
    h = x
    for i in range(DEPTH):
        j = i // N_MIXERS
        if i % N_MIXERS == 0:
            lambda_init = 0.8 - 0.6 * math.exp(-0.3 * i)
            mix = diff_attention(h, attn_w_qkv[j], attn_w_o[j], attn_lq1[j], attn_lk1[j],
                                 attn_lq2[j], attn_lk2[j], attn_sub_g[j], lambda_init, cos, sin)
        else:
            mix = pool_mixer(h, pool_w_in[j], pool_w_grp[j], pool_scale[j], pool_w_out[j])
        h = layer_norm(DEEPNORM_ALPHA * h + mix, ln_g[i, 0], ln_b[i, 0])
        ffn = hier_moe(h, moe_w_grp_router[i], moe_b_grp_router[i], moe_w_exp_router[i],
                       moe_b_exp_router[i], moe_w_gate[i], moe_w_up[i], moe_w_down[i])
        h = layer_norm(DEEPNORM_ALPHA * h + ffn, ln_g[i, 1], ln_b[i, 1])
    return h
```

```python
import contextlib
import math
import numpy as np
import concourse.bass as bass
import concourse.mybir as mybir
from concourse.bass_utils import run_bass_kernel_spmd

F32 = mybir.dt.float32
BF16 = mybir.dt.bfloat16
I32 = mybir.dt.int32
ALU = mybir.AluOpType
AF = mybir.ActivationFunctionType
AX = mybir.AxisListType

D = 1024
NE = 32
DE = 512
NCORES = 8
DEPTH = 4
ALPHA = (2 * DEPTH) ** 0.25
LN_EPS = 1e-5
SUBLN_EPS = 1e-5
POOL_WINDOWS = (2, 4, 8, 16)
ENGINES = ("pe", "act", "dve", "pool", "sp")
EPOCH = 20000
SB_BASE = 16512
SB_TOP = 229344


class Op:
    __slots__ = ("eng", "fn", "waits", "sig", "is_dma", "semkey", "inc")


class Prog:
    def __init__(self, nc):
        self.nc = nc
        self.ops = []
        self.last_writer = {}
        self.readers = {}
        self.stack = contextlib.ExitStack()
        self.uid = 0
        self.sb_off = SB_BASE

    def sbuf(self, shape, dtype, name=None):
        self.uid += 1
        nbytes = int(np.prod(shape[1:])) * (4 if dtype in (F32, I32) else 2)
        off = (self.sb_off + 63) // 64 * 64
        assert off + nbytes <= SB_TOP, f"SBUF overflow allocating {name} {shape}: {off}+{nbytes}"
        self.sb_off = off + nbytes
        return self.nc.alloc_sbuf_tensor_at(f"s{self.uid}_" + (name or "t"), list(shape), dtype, offset=off)

    def reset_sbuf(self):
        self.sb_off = SB_BASE

    def fence(self):
        last = {}
        dmas = {}
        for o in self.ops:
            if o.fn is None:
                continue
            if o.is_dma:
                dmas[o.semkey] = o
            else:
                last[o.eng] = o
        targets = list(last.values()) + list(dmas.values())
        for t in targets:
            t.sig = True
        for eng in ENGINES:
            op = Op()
            op.eng, op.fn, op.is_dma, op.semkey, op.sig, op.inc = eng, None, False, None, False, 1
            op.waits = [t for t in targets if not (t.eng == eng and not t.is_dma)]
            self.ops.append(op)
        self.last_writer.clear()
        self.readers.clear()

    def psum(self, shape, dtype=F32, name=None):
        self.uid += 1
        return self.stack.enter_context(self.nc.psum_tensor("p_" + (name or f"ps{self.uid}"), list(shape), dtype))

    def _add(self, eng, fn, reads, writes, is_dma=False, semkey=None):
        op = Op()
        op.eng, op.fn, op.is_dma, op.semkey = eng, fn, is_dma, semkey
        op.sig = False
        op.inc = 16 if is_dma else 1
        excl = [k for k in reads if k.startswith("pb")]
        if excl:
            reads = [k for k in reads if not k.startswith("pb")]
            writes = list(writes) + [k for k in excl if k not in writes]
        deps = []
        for k in reads:
            w = self.last_writer.get(k)
            if w is not None:
                deps.append(w)
        for k in writes:
            w = self.last_writer.get(k)
            if w is not None:
                deps.append(w)
            deps.extend(self.readers.get(k, ()))
        seen = set()
        op.waits = []
        for d in deps:
            if id(d) in seen or d is op:
                continue
            seen.add(id(d))
            if d.eng == "pe" and eng == "pe" and not d.is_dma and not is_dma:
                continue
            op.waits.append(d)
            d.sig = True
        for k in reads:
            self.readers.setdefault(k, []).append(op)
        for k in writes:
            self.last_writer[k] = op
            self.readers[k] = []
        self.ops.append(op)
        return op

    def op(self, eng, fn, reads=(), writes=()):
        return self._add(eng, fn, reads, writes)

    def dma(self, eng, fn, reads=(), writes=(), semkey=None):
        o = self._add(eng, fn, reads, writes, is_dma=True, semkey=semkey)
        o.sig = True
        return o

    def coll(self, fn, reads, writes, semkey, inc=1):
        o = self._add("pool", fn, reads, writes, is_dma=True, semkey=semkey)
        o.sig = True
        o.inc = inc
        return o

    def mm(self, out, lhsT, rhs, start, stop, r, w):
        return self.op("pe", lambda e: e.matmul(out, lhsT, rhs, start=start, stop=stop), r, w)

    def tr(self, out, in_, ident, r, w):
        return self.op("pe", lambda e: e.transpose(out, in_, ident), r, w)

    def act(self, out, in_, func, r, w, bias=None, scale=1.0, accum=None):
        def f(e):
            kw = {}
            if bias is not None:
                kw["bias"] = bias
            if accum is not None:
                kw["accum_out"] = accum
            return e.activation(out=out, in_=in_, func=func, scale=scale, **kw)
        return self.op("act", f, r, w)

    def tt(self, eng, out, in0, in1, op, r, w):
        return self.op(eng, lambda e: e.tensor_tensor(out=out, in0=in0, in1=in1, op=op), r, w)

    def ts(self, eng, out, in0, s1, s2, op0, op1, r, w):
        if s2 is None:
            return self.op(eng, lambda e: e.tensor_scalar(out=out, in0=in0, scalar1=s1, scalar2=None, op0=op0), r, w)
        return self.op(eng, lambda e: e.tensor_scalar(out=out, in0=in0, scalar1=s1, scalar2=s2, op0=op0, op1=op1), r, w)

    def stt(self, out, in0, scalar, in1, op0, op1, r, w, accum=None):
        def f(e):
            if accum is not None:
                return e.scalar_tensor_tensor(out=out, in0=in0, scalar=scalar, in1=in1, op0=op0, op1=op1, accum_out=accum)
            return e.scalar_tensor_tensor(out=out, in0=in0, scalar=scalar, in1=in1, op0=op0, op1=op1)
        return self.op("dve", f, r, w)

    def cp(self, eng, out, in_, r, w):
        if eng == "act":
            return self.op("act", lambda e: e.copy(out=out, in_=in_), r, w)
        return self.op(eng, lambda e: e.tensor_copy(out=out, in_=in_), r, w)

    def red(self, out, in_, op, r, w):
        return self.op("dve", lambda e: e.tensor_reduce(out=out, in_=in_, axis=AX.X, op=op), r, w)

    def ld(self, eng, out, in_, w, semkey=None, r=()):
        return self.dma(eng, lambda e: e.dma_start(out=out, in_=in_), r, w, semkey=semkey or w[0])

    def st(self, eng, out, in_, r, semkey, w=()):
        return self.dma(eng, lambda e: e.dma_start(out=out, in_=in_), r, w, semkey=semkey)

    def emit(self, final_wait_ops=()):
        nc = self.nc
        sem_of = {}
        cnt = {}
        semnames = []
        eng_sig_count = {e: 0 for e in ENGINES}
        for o in self.ops:
            if not o.sig:
                continue
            if o.is_dma:
                name = "d_" + str(o.semkey)
                cnt[name] = cnt.get(name, 0) + o.inc
                sem_of[id(o)] = (name, cnt[name])
            else:
                n = eng_sig_count[o.eng]
                name = f"c_{o.eng}_{n // EPOCH}"
                eng_sig_count[o.eng] = n + 1
                sem_of[id(o)] = (name, n % EPOCH + 1)
            if name not in semnames:
                semnames.append(name)
        sems = {}
        for name in semnames:
            sems[name] = self.stack.enter_context(nc.semaphore(name))
        self.nsems = len(semnames)
        per_eng = {e: [] for e in ENGINES}
        for o in self.ops:
            per_eng[o.eng].append(o)
        finals = list(final_wait_ops)

        def run(engname, eng):
            waited = {}
            for o in per_eng[engname]:
                for d in o.waits:
                    name, val = sem_of[id(d)]
                    if waited.get(name, 0) >= val:
                        continue
                    waited[name] = val
                    eng.wait_ge(sems[name], val)
                if o.fn is None:
                    continue
                ins = o.fn(eng)
                if o.sig:
                    name, val = sem_of[id(o)]
                    ins.then_inc(sems[name], o.inc)
            if engname == "sp":
                for d in finals:
                    name, val = sem_of[id(d)]
                    if waited.get(name, 0) >= val:
                        continue
                    waited[name] = val
                    eng.wait_ge(sems[name], val)

        with nc.Block() as block:
            @block.tensor
            def _(e):
                run("pe", e)

            @block.scalar
            def _(e):
                run("act", e)

            @block.vector
            def _(e):
                run("dve", e)

            @block.gpsimd
            def _(e):
                run("pool", e)

            @block.sync
            def _(e):
                run("sp", e)
        self.stack.close()


def build_tail(kind, T, C, NEXP=NE):
    nc = bass.Bass("TRN2", target_bir_lowering=False)
    P = Prog(nc)
    NTL = T // 128
    NSB = C // 128
    inp = lambda name, shape: nc.dram_tensor(name, list(shape), F32, kind="ExternalInput")
    h_d = inp("h", [T, D])
    if kind == "attn":
        oT_d = inp("oT", [D, T])
        wo_d = inp("w_o", [D, D])
    else:
        halo_d = inp("halo", [128, D])
        am0_d = inp("am0", [4, 128, 128])
        amd_d = inp("amd", [4, 128, 128])
        amo_d = inp("amo", [4, 128, 128])
        win_d = inp("w_in", [D, D])
        wgrp_d = inp("w_grp", [4, 256, 256])
        lsT_d = inp("lsT", [128, 8])
        wout_d = inp("w_out", [D, D])
    lng_d = inp("ln_g", [2, D])
    lnb_d = inp("ln_b", [2, D])
    wrt_d = inp("w_rt", [D, 36])
    brt_d = inp("b_rt", [1, 36])
    wg_d = inp("w_gate", [NEXP, D, DE])
    wu_d = inp("w_up", [NEXP, D, DE])
    wd_d = inp("w_down", [NEXP, DE, D])
    ident_d = inp("ident", [128, 128])
    ustr_d = inp("ustrict", [128, 128])
    ecoff_d = inp("ecoff", [1, 32])
    out_d = nc.dram_tensor("out", [T, D], F32, kind="ExternalOutput")
    h1_d = nc.dram_tensor("h1_scr", [T, D], F32)
    xin_d = nc.dram_tensor("xin_scr", [NE * C, D], BF16)
    y_d = nc.dram_tensor("y_scr", [NE * C, D], F32)

    ident = P.sbuf([128, 128], F32, "ident")
    identb = P.sbuf([128, 128], BF16, "identb")
    ustr = P.sbuf([128, 128], BF16, "ustr")
    ones = P.sbuf([128, 128], BF16, "ones")
    ecoff = P.sbuf([128, 32], F32, "ecoff")
    brt = P.sbuf([128, 36], F32, "brt")
    wrt = P.sbuf([128, 8, 36], F32, "wrt")
    lng = P.sbuf([128, 2, D], F32, "lng")
    lnb = P.sbuf([128, 2, D], F32, "lnb")
    epst = P.sbuf([128, 1], F32, "epst")
    Scnt = P.sbuf([128, 32], BF16, "Scnt")
    gates = P.sbuf([128, NTL, 2], F32, "gates")
    dest = P.sbuf([128, NTL, 2], I32, "dest")

    P.ld("sp", ident[:], ident_d.ap(), ["ident"])
    P.ld("pool", identb[:], ident_d.ap(), ["identb"])
    P.ld("pool", ustr[:], ustr_d.ap(), ["ustr"])
    P.op("pool", lambda e: e.memset(ones[:], 1.0), (), ["ones"])
    P.op("pool", lambda e: e.memset(epst[:], LN_EPS), (), ["epst"])
    P.op("pool", lambda e: e.memset(Scnt[:], 0.0), (), ["Scnt"])
    P.ld("sp", ecoff[:], ecoff_d.ap().partition_broadcast(128), ["ecoff"])
    P.ld("sp", brt[:], brt_d.ap().partition_broadcast(128), ["brt"])
    P.ld("sp", wrt[:], wrt_d.ap().rearrange("(k p) n -> p k n", p=128), ["wrt"])
    for j in range(2):
        P.ld("sp", lng[:, j, :], lng_d.ap()[j:j + 1, :].partition_broadcast(128), [f"lng{j}"])
        P.ld("sp", lnb[:, j, :], lnb_d.ap()[j:j + 1, :].partition_broadcast(128), [f"lnb{j}"])

    zt = P.sbuf([128, D], BF16, "zt")
    P.op("pool", lambda e: e.memset(zt[:], 0.0), (), ["zt"])
    nz = (NE * C) // 128
    for z in range(nz):
        P.dma("sp", (lambda z: lambda e: e.dma_start(out=xin_d.ap()[z * 128:(z + 1) * 128, :], in_=zt[:]))(z), ["zt"], ["xinz"] if z == nz - 1 else [f"xinz{z}"], semkey="xinz")
    pb = [P.psum([128, 512], F32, f"pb{i}") for i in range(6)]
    pbb = [P.psum([128, 1024], BF16, f"pbb{i}") for i in range(2)]

    if kind == "attn":
        wo = P.sbuf([128, 8, D], BF16, "wo")
        P.ld("pool", wo[:], wo_d.ap().rearrange("(k p) n -> p k n", p=128), ["wo"])
        OCH = min(512, T)
        oT = [P.sbuf([128, 8, OCH], BF16, f"oT{i}") for i in range(2)]
    else:
        win = P.sbuf([128, 8, D], BF16, "win")
        wout = P.sbuf([128, 8, D], BF16, "wout")
        wgrp = P.sbuf([128, 4, 2, 256], BF16, "wgrp")
        lsT = P.sbuf([128, 8], F32, "lsT")
        am0 = P.sbuf([128, 4, 128], BF16, "am0")
        amd = P.sbuf([128, 4, 128], BF16, "amd")
        amo = P.sbuf([128, 4, 128], BF16, "amo")
        P.ld("pool", win[:], win_d.ap().rearrange("(k p) n -> p k n", p=128), ["win"])
        P.ld("pool", wout[:], wout_d.ap().rearrange("(k p) n -> p k n", p=128), ["wout"])
        P.ld("pool", wgrp[:], wgrp_d.ap().rearrange("g (k p) n -> p g k n", p=128), ["wgrp"])
        P.ld("sp", lsT[:], lsT_d.ap(), ["lsT"])
        P.ld("pool", am0[:], am0_d.ap().rearrange("w p n -> p w n"), ["am0"])
        P.ld("pool", amd[:], amd_d.ap().rearrange("w p n -> p w n"), ["amd"])
        P.ld("pool", amo[:], amo_d.ap().rearrange("w p n -> p w n"), ["amo"])
        ubuf = [P.sbuf([128, D], BF16, f"u{i}") for i in range(3)]
        hTb = P.sbuf([128, 8, 128], BF16, "hTb")
        pT = P.sbuf([128, 8, 128], BF16, "pT")
        qT = P.sbuf([128, 8, 128], BF16, "qT")

    htile = [P.sbuf([128, D], F32, f"ht{i}") for i in range(2)]
    rt = [P.sbuf([128, D], F32, f"rt{i}") for i in range(2)]
    h1b = [P.sbuf([128, D], BF16, f"h1b{i}") for i in range(2)]
    h1T = P.sbuf([128, 8, 128], F32, "h1T")
    stats = P.sbuf([128, 2, 6], F32, "stats")
    mv = P.sbuf([128, 2], F32, "mv")
    rstd = P.sbuf([128, 1], F32, "rstd")
    nmr = P.sbuf([128, 1], F32, "nmr")
    L = P.sbuf([128, 36], F32, "L")
    gmax = P.sbuf([128, 1], F32, "gmax")
    ngmax = P.sbuf([128, 1], F32, "ngmax")
    G1 = P.sbuf([128, 4], F32, "G1")
    ge = P.sbuf([128, 4], F32, "ge")
    gsum = P.sbuf([128, 1], F32, "gsum")
    pen = P.sbuf([128, 4], F32, "pen")
    Lm = P.sbuf([128, 32], F32, "Lm")
    Lm2 = P.sbuf([128, 32], F32, "Lm2")
    m1 = P.sbuf([128, 1], F32, "m1")
    m2 = P.sbuf([128, 1], F32, "m2")
    OH1 = P.sbuf([128, 32], F32, "OH1")
    OH2 = P.sbuf([128, 32], F32, "OH2")
    OHc = P.sbuf([128, 32], BF16, "OHc")
    dd = P.sbuf([128, 1], F32, "dd")
    ex = P.sbuf([128, 1], F32, "ex")
    den = P.sbuf([128, 1], F32, "den")
    Pf = P.sbuf([128, 32], F32, "Pf")
    tmp32 = P.sbuf([128, 32], F32, "tmp32")
    dflt = P.sbuf([128, 2], F32, "dflt")

    def layer_norm(src, j, dst, keys_r, key_w):
        sv = src.rearrange("p (c f) -> p c f", c=2)
        for c in range(2):
            P.op("dve", (lambda c: lambda e: e.bn_stats(out=stats[:, c, :], in_=sv[:, c, :]))(c), keys_r, [f"stats{c}"])
        P.op("dve", lambda e: e.bn_aggr(out=mv[:], in_=stats[:]), ["stats0", "stats1"], ["mv"])
        P.act(rstd[:], mv[:, 1:2], AF.Sqrt, ["mv", "epst"], ["rstd"], bias=epst[:, 0:1])
        P.op("dve", lambda e: e.reciprocal(out=rstd[:], in_=rstd[:]), ["rstd"], ["rstd"])
        P.stt(nmr[:], mv[:, 0:1], -1.0, rstd[:], ALU.mult, ALU.mult, ["mv", "rstd"], ["nmr"])
        P.act(dst, src, AF.Identity, list(keys_r) + ["rstd", "nmr"], [key_w], bias=nmr[:, 0:1], scale=rstd[:, 0:1])
        P.tt("dve", dst, dst, lng[:, j, :], ALU.mult, [key_w, f"lng{j}"], [key_w])
        P.tt("pool", dst, dst, lnb[:, j, :], ALU.add, [key_w, f"lnb{j}"], [key_w])

    def pool_u(i, slot, hslot):
        hs = htile[hslot]
        hk = f"ht{hslot}"
        if i < 0:
            P.ld("sp", hs[:], halo_d.ap(), [hk])
        for k in range(8):
            P.tr(pb[2 + k // 4][:, (k % 4) * 128:(k % 4 + 1) * 128], hs[:, k * 128:(k + 1) * 128], ident[:], [hk, "ident"], [f"pb{2 + k // 4}"])
        for hf in range(2):
            P.cp("act" if hf == 0 else "dve", hTb[:, hf * 4:(hf + 1) * 4, :], pb[2 + hf][:].rearrange("p (k t) -> p k t", k=4), [f"pb{2 + hf}"], [f"hTb{hf}"])
        for hf in range(2):
            for k in range(8):
                P.mm(pb[hf][:], hTb[:, k, :], win[:, k, hf * 512:(hf + 1) * 512], k == 0, k == 7, [f"hTb{k // 4}", "win"], [f"pb{hf}"])
        for hf in range(2):
            P.cp("act" if hf == 0 else "dve", ubuf[slot][:, hf * 512:(hf + 1) * 512], pb[hf][:], [f"pb{hf}"], [f"u{slot}_{hf}"])

    if kind == "pool":
        pool_u(-1, 2, 1)

    for i in range(NTL):
        s = i % 2
        hk = f"ht{s}"
        P.ld("sp", htile[s][:], h_d.ap()[i * 128:(i + 1) * 128, :], [hk])
        if kind == "attn":
            ch = (i * 128) // OCH
            if (i * 128) % OCH == 0:
                P.ld("pool", oT[ch % 2][:], oT_d.ap()[:, ch * OCH:(ch + 1) * OCH].rearrange("(k p) t -> p k t", p=128), [f"oT{ch % 2}"])
            off = i * 128 - ch * OCH
            for hf in range(2):
                for k in range(8):
                    P.mm(pb[hf][:], oT[ch % 2][:, k, off:off + 128], wo[:, k, hf * 512:(hf + 1) * 512], k == 0, k == 7, [f"oT{ch % 2}", "wo"], [f"pb{hf}"])
        else:
            us = i % 3
            up = (i - 1) % 3
            pool_u(i, us, s)
            A = am0 if i == 0 else amd
            Ak = "am0" if i == 0 else "amd"
            for j in range(8):
                g = j // 2
                o_ = pb[4 + j // 4][:, (j % 4) * 128:(j % 4 + 1) * 128]
                P.mm(o_, ubuf[us][:, j * 128:(j + 1) * 128], A[:, g, :], True, False, [f"u{us}_{j // 4}", Ak], [f"pb{4 + j // 4}"])
                P.mm(o_, ubuf[up][:, j * 128:(j + 1) * 128], amo[:, g, :], False, True, [f"u{up}_{j // 4}", "amo"], [f"pb{4 + j // 4}"])
            for hf in range(2):
                P.cp("act" if hf == 0 else "dve", pT[:, hf * 4:(hf + 1) * 4, :], pb[4 + hf][:].rearrange("p (k t) -> p k t", k=4), [f"pb{4 + hf}"], [f"pT{hf}"])
            for j in range(8):
                g = j // 2
                o_ = pb[4 + j // 4][:, (j % 4) * 128:(j % 4 + 1) * 128]
                for kk in range(2):
                    P.mm(o_, wgrp[:, g, kk, (j % 2) * 128:(j % 2 + 1) * 128], pT[:, 2 * g + kk, :], kk == 0, kk == 1, [f"pT{(2 * g + kk) // 4}", "wgrp"], [f"pb{4 + j // 4}"])
            for j in range(8):
                P.act(qT[:, j, :], pb[4 + j // 4][:, (j % 4) * 128:(j % 4 + 1) * 128], AF.Identity, [f"pb{4 + j // 4}", "lsT"], [f"qT{j}"], scale=lsT[:, j:j + 1])
            for hf in range(2):
                for k in range(8):
                    P.mm(pb[hf][:], qT[:, k, :], wout[:, k, hf * 512:(hf + 1) * 512], k == 0, k == 7, [f"qT{k}", "wout"], [f"pb{hf}"])
        r = rt[s]
        rk = f"rt{s}"
        for hf in range(2):
            P.stt(r[:, hf * 512:(hf + 1) * 512], htile[s][:, hf * 512:(hf + 1) * 512], ALPHA, pb[hf][:], ALU.mult, ALU.add, [hk, f"pb{hf}"], [rk])
        layer_norm(r[:], 0, r[:], [rk], rk)
        P.st("sp", h1_d.ap()[i * 128:(i + 1) * 128, :], r[:], [rk], f"h1st{s}", [f"h1d{i}"])
        P.cp("act", h1b[s][:], r[:], [rk], [f"h1b{s}"])
        for k in range(8):
            P.tr(pb[2 + k // 4][:, (k % 4) * 128:(k % 4 + 1) * 128], r[:, k * 128:(k + 1) * 128], ident[:], [rk, "ident"], [f"pb{2 + k // 4}"])
        for hf in range(2):
            P.cp("act" if hf == 0 else "dve", h1T[:, hf * 4:(hf + 1) * 4, :], pb[2 + hf][:].rearrange("p (k t) -> p k t", k=4), [f"pb{2 + hf}"], [f"h1T{hf}"])
        for k in range(8):
            P.mm(pb[4][:, 0:36], h1T[:, k, :], wrt[:, k, :], k == 0, k == 7, [f"h1T{k // 4}", "wrt"], ["pb4"])
        P.tt("dve", L[:], pb[4][:, 0:36], brt[:], ALU.add, ["pb4", "brt"], ["L"])
        P.red(gmax[:], L[:, 0:4], ALU.max, ["L"], ["gmax"])
        P.tt("dve", G1[:], L[:, 0:4], gmax[:, 0:1].to_broadcast([128, 4]), ALU.is_equal, ["L", "gmax"], ["G1"])
        P.ts("dve", ngmax[:], gmax[:], -1.0, None, ALU.mult, None, ["gmax"], ["ngmax"])
        P.act(ge[:], L[:, 0:4], AF.Exp, ["L", "ngmax"], ["ge", "gsum"], bias=ngmax[:, 0:1], accum=gsum[:, 0:1])
        P.ts("dve", pen[:], G1[:], -1.0, 1e30, ALU.add, ALU.mult, ["G1"], ["pen"])
        P.tt("dve", Lm[:].rearrange("p (g e) -> p g e", g=4), L[:, 4:36].rearrange("p (g e) -> p g e", g=4),
             pen[:].unsqueeze(2).to_broadcast([128, 4, 8]), ALU.add, ["L", "pen"], ["Lm"])
        P.red(m1[:], Lm[:], ALU.max, ["Lm"], ["m1"])
        P.tt("dve", OH1[:], Lm[:], m1[:, 0:1].to_broadcast([128, 32]), ALU.is_equal, ["Lm", "m1"], ["OH1"])
        P.stt(Lm2[:], OH1[:], -1e30, Lm[:], ALU.mult, ALU.add, ["OH1", "Lm"], ["Lm2"])
        P.red(m2[:], Lm2[:], ALU.max, ["Lm2"], ["m2"])
        P.tt("dve", OH2[:], Lm2[:], m2[:, 0:1].to_broadcast([128, 32]), ALU.is_equal, ["Lm2", "m2"], ["OH2"])
        P.tt("dve", dd[:], m2[:], m1[:], ALU.subtract, ["m1", "m2"], ["dd"])
        P.act(ex[:], dd[:], AF.Exp, ["dd"], ["ex"])
        P.stt(den[:], ex[:], 1.0, gsum[:], ALU.add, ALU.mult, ["ex", "gsum"], ["den"])
        P.op("dve", (lambda i: lambda e: e.reciprocal(out=gates[:, i, 0:1], in_=den[:]))(i), ["den"], [f"gate{i}"])
        P.tt("dve", gates[:, i, 1:2], gates[:, i, 0:1], ex[:], ALU.mult, [f"gate{i}", "ex"], [f"gate{i}"])
        P.tt("dve", OHc[:], OH1[:], OH2[:], ALU.add, ["OH1", "OH2"], ["OHc"])
        P.mm(pb[5][:, 0:32], ustr[:], OHc[:], True, False, ["ustr", "OHc"], ["pb5"])
        P.mm(pb[5][:, 0:32], ones[:], Scnt[:], False, True, ["ones", "Scnt"], ["pb5"])
        P.tt("dve", Pf[:], pb[5][:, 0:32], ecoff[:], ALU.add, ["pb5", "ecoff"], ["Pf"])
        P.tt("dve", Scnt[:], Scnt[:], OHc[:], ALU.add, ["Scnt", "OHc"], ["Scnt"])
        P.stt(tmp32[:], OH1[:], 1.0, Pf[:], ALU.mult, ALU.mult, ["OH1", "Pf"], ["tmp32", "dflt0"], accum=dflt[:, 0:1])
        P.stt(tmp32[:], OH2[:], 1.0, Pf[:], ALU.mult, ALU.mult, ["OH2", "Pf"], ["tmp32", "dflt1"], accum=dflt[:, 1:2])
        P.cp("dve", dest[:, i, :], dflt[:], ["dflt0", "dflt1"], [f"dest{i}"])
        for kk in range(2):
            P.dma("pool", (lambda i, kk, s: lambda e: e.indirect_dma_start(
                out=xin_d.ap(), out_offset=bass.IndirectOffsetOnAxis(ap=dest[:, i, kk:kk + 1], axis=0),
                in_=h1b[s][:], in_offset=None))(i, kk, s), [f"h1b{s}", f"dest{i}", "xinz"], [f"xinw{i}_{kk}"], semkey=f"scat{s}{kk}")

    wgs = [P.sbuf([128, 8, DE], BF16, f"wg{i}") for i in range(2)]
    wus = [P.sbuf([128, 8, DE], BF16, f"wu{i}") for i in range(2)]
    wds = [P.sbuf([128, 4, D], BF16, f"wd{i}") for i in range(2)]
    xin = [P.sbuf([128, NSB, D], BF16, f"xin{i}") for i in range(2)]
    xT = [P.sbuf([128, 8, C], BF16, f"xT{i}") for i in range(2)]
    actT = [P.sbuf([128, 4, C], BF16, f"actT{i}") for i in range(2)]
    sg = [P.sbuf([128, C], F32, f"sg{i}") for i in range(2)]
    yb = [P.sbuf([128, D], F32, f"yb{i}") for i in range(2)]
    ycount = 0
    xin_keys = [f"xinw{i}_{kk}" for i in range(NTL) for kk in range(2)]
    y_keys = [f"y_{e_}_{sb}" for e_ in range(NEXP) for sb in range(NSB)]
    for e_ in range(NEXP):
        s = e_ % 2
        P.ld("pool", wgs[s][:], wg_d.ap()[e_].rearrange("(k p) n -> p k n", p=128), [f"wg{s}"])
        P.ld("pool", wus[s][:], wu_d.ap()[e_].rearrange("(k p) n -> p k n", p=128), [f"wu{s}"])
        P.ld("pool", wds[s][:], wd_d.ap()[e_].rearrange("(k p) n -> p k n", p=128), [f"wd{s}"])
        P.ld("sp", xin[s][:], xin_d.ap()[e_ * C:(e_ + 1) * C, :].rearrange("(b p) n -> p b n", p=128), [f"xin{s}"], r=xin_keys)
        for sb in range(NSB):
            pbt = pbb[sb % 2]
            for k in range(8):
                P.tr(pbt[:, k * 128:(k + 1) * 128], xin[s][:, sb, k * 128:(k + 1) * 128], identb[:], [f"xin{s}", "identb"], [f"pbb{sb % 2}"])
            P.cp("dve" if sb % 2 else "act", xT[s][:, :, sb * 128:(sb + 1) * 128], pbt[:].rearrange("p (k t) -> p k t", k=8), [f"pbb{sb % 2}"], [f"xT{s}_{sb}"])
        xkeys = [f"xT{s}_{sb}" for sb in range(NSB)]
        for fc in range(4):
            pg = pb[(fc % 2) * 2]
            pu = pb[(fc % 2) * 2 + 1]
            kg, ku = f"pb{(fc % 2) * 2}", f"pb{(fc % 2) * 2 + 1}"
            for k in range(8):
                P.mm(pg[:, 0:C], wgs[s][:, k, fc * 128:(fc + 1) * 128], xT[s][:, k, :], k == 0, k == 7, xkeys + [f"wg{s}"], [kg])
            for k in range(8):
                P.mm(pu[:, 0:C], wus[s][:, k, fc * 128:(fc + 1) * 128], xT[s][:, k, :], k == 0, k == 7, xkeys + [f"wu{s}"], [ku])
            P.act(sg[fc % 2][:], pg[:, 0:C], AF.Silu, [kg], [f"sg{fc % 2}"])
            P.tt("dve", actT[s][:, fc, :], sg[fc % 2][:], pu[:, 0:C], ALU.mult, [f"sg{fc % 2}", ku], [f"actT{s}_{fc}"])
        akeys = [f"actT{s}_{fc}" for fc in range(4)]
        for sb in range(NSB):
            ys = ycount % 2
            ycount += 1
            for hf in range(2):
                py = pb[4 + hf]
                for fc in range(4):
                    P.mm(py[:], actT[s][:, fc, sb * 128:(sb + 1) * 128], wds[s][:, fc, hf * 512:(hf + 1) * 512], fc == 0, fc == 3, akeys + [f"wd{s}"], [f"pb{4 + hf}"])
                P.cp("act" if hf == 0 else "dve", yb[ys][:, hf * 512:(hf + 1) * 512], py[:], [f"pb{4 + hf}"], [f"yb{ys}"])
            r0 = e_ * C + sb * 128
            P.st("sp", y_d.ap()[r0:r0 + 128, :], yb[ys][:], [f"yb{ys}"], f"yst{ys}", [f"y_{e_}_{sb}"])

    y0 = [P.sbuf([128, D], F32, f"y0_{i}") for i in range(2)]
    y1 = [P.sbuf([128, D], F32, f"y1_{i}") for i in range(2)]
    outs = []
    for i in range(NTL):
        s = i % 2
        hk = f"ht{s}"
        P.ld("sp", htile[s][:], h1_d.ap()[i * 128:(i + 1) * 128, :], [hk], r=[f"h1d{i}"])
        for kk, yt in ((0, y0), (1, y1)):
            P.dma("pool", (lambda i, kk, yt, s: lambda e: e.indirect_dma_start(
                out=yt[s][:], out_offset=None, in_=y_d.ap(),
                in_offset=bass.IndirectOffsetOnAxis(ap=dest[:, i, kk:kk + 1], axis=0)))(i, kk, yt, s),
                y_keys + [f"dest{i}"], [f"y{kk}_{s}"], semkey=f"gath{kk}{s}")
        r = rt[s]
        rk = f"rt{s}"
        P.ts("pool", r[:], htile[s][:], ALPHA, None, ALU.mult, None, [hk], [rk])
        P.stt(r[:], y0[s][:], gates[:, i, 0:1], r[:], ALU.mult, ALU.add, [f"y0_{s}", f"gate{i}", rk], [rk])
        P.stt(r[:], y1[s][:], gates[:, i, 1:2], r[:], ALU.mult, ALU.add, [f"y1_{s}", f"gate{i}", rk], [rk])
        layer_norm(r[:], 1, r[:], [rk], rk)
        outs.append(P.st("sp", out_d.ap()[i * 128:(i + 1) * 128, :], r[:], [rk], f"ost{s}"))
    P.emit(final_wait_ops=outs)
    return nc


def host_consts(C):
    ident = np.eye(128, dtype=np.float32)
    ustrict = (np.arange(128)[:, None] < np.arange(128)[None, :]).astype(np.float32)
    ecoff = (np.arange(32, dtype=np.float32) * C).reshape(1, 32)
    return dict(ident=ident, ustrict=ustrict, ecoff=ecoff)


def pool_consts(first):
    am0 = np.zeros((4, 128, 128), np.float32)
    amd = np.zeros((4, 128, 128), np.float32)
    amo = np.zeros((4, 128, 128), np.float32)
    sp = np.arange(128)[:, None]
    s = np.arange(128)[None, :]
    for g, w in enumerate(POOL_WINDOWS):
        band = ((sp <= s) & (sp > s - w)).astype(np.float32)
        amd[g] = band / w - np.eye(128, dtype=np.float32)
        cnt = np.minimum(s + 1, w).astype(np.float32)
        am0[g] = band / cnt - np.eye(128, dtype=np.float32)
        amo[g] = ((sp - 128 > s - w)).astype(np.float32) / w
    return dict(am0=am0 if first else amd.copy(), amd=amd, amo=amo)


def build_attn(S, dbg=0):
    nc = bass.Bass("TRN2", target_bir_lowering=False)
    P = Prog(nc)
    NT = S // 128
    NCH = S // 512
    inp = lambda name, shape: nc.dram_tensor(name, list(shape), F32, kind="ExternalInput")
    xT_d = inp("xT", [D, S])
    wq_d = inp("wq", [D, 512])
    wk_d = inp("wk", [D, 512])
    wv_d = inp("wv", [D, 512])
    cs_d = inp("cs", [128, (S // 128) * 16])
    lq1_d = inp("lq1", [1, 64])
    lk1_d = inp("lk1", [1, 64])
    lq2_d = inp("lq2", [1, 64])
    lk2_d = inp("lk2", [1, 64])
    linit_d = inp("linit", [1, 1])
    subg_d = inp("subg", [1, 128])
    ident_d = inp("ident", [128, 128])
    tri_d = inp("trimask", [128, 128])
    o_d = nc.dram_tensor("o", [S, 512], F32, kind="ExternalOutput")

    identb = P.sbuf([128, 128], BF16, "identb")
    trib = P.sbuf([128, 128], BF16, "trib")
    cs = P.sbuf([128, NT, 16], F32, "cs")
    lqk = P.sbuf([128, 4, 64], F32, "lqk")
    linit = P.sbuf([128, 1], F32, "linit")
    subg = P.sbuf([128, 128], F32, "subg")
    subgs = P.sbuf([128, 128], F32, "subgs")
    junk64 = P.sbuf([128, 64], F32, "junk64")
    ssum = P.sbuf([128, 2], F32, "ssum")
    esum = P.sbuf([128, 2], F32, "esum")
    nlam = P.sbuf([128, 1], F32, "nlam")
    om = P.sbuf([128, 1], F32, "om")
    epst = P.sbuf([128, 1], F32, "epst")
    P.ld("pool", identb[:], ident_d.ap(), ["identb"])
    P.ld("pool", trib[:], tri_d.ap(), ["trib"])
    P.ld("sp", cs[:].rearrange("p t c -> p (t c)"), cs_d.ap(), ["cs"])
    for n, dd_ in enumerate((lq1_d, lk1_d, lq2_d, lk2_d)):
        P.ld("sp", lqk[:, n, :], dd_.ap().partition_broadcast(128), [f"lqk{n}"])
    P.ld("sp", linit[:], linit_d.ap().partition_broadcast(128), ["linit"])
    P.ld("sp", subg[:], subg_d.ap().partition_broadcast(128), ["subg"])
    P.op("pool", lambda e: e.memset(epst[:], SUBLN_EPS), (), ["epst"])
    P.stt(junk64[:], lqk[:, 0, :], 1.0, lqk[:, 1, :], ALU.mult, ALU.mult, ["lqk0", "lqk1"], ["junk64", "ssum0"], accum=ssum[:, 0:1])
    P.stt(junk64[:], lqk[:, 2, :], 1.0, lqk[:, 3, :], ALU.mult, ALU.mult, ["lqk2", "lqk3"], ["junk64", "ssum1"], accum=ssum[:, 1:2])
    P.act(esum[:], ssum[:], AF.Exp, ["ssum0", "ssum1"], ["esum"])
    P.tt("dve", nlam[:], esum[:, 1:2], esum[:, 0:1], ALU.subtract, ["esum"], ["nlam"])
    P.tt("dve", nlam[:], nlam[:], linit[:], ALU.subtract, ["nlam", "linit"], ["nlam"])
    P.ts("dve", om[:], linit[:], -1.0, 1.0, ALU.mult, ALU.add, ["linit"], ["om"])
    P.ts("dve", subgs[:], subg[:], om[:, 0:1], None, ALU.mult, None, ["subg", "om"], ["subgs"])

    pb = [P.psum([128, 512], F32, f"pb{i}") for i in range(7)]
    pbb = P.psum([128, 1024], BF16, "pbb")

    QKT = P.sbuf([128, 4, S], BF16, "QKT")
    Vaug = P.sbuf([128, NT, 2, 129], BF16, "Vaug")
    wq = P.sbuf([128, 8, 256], BF16, "wq")
    wk = P.sbuf([128, 8, 256], BF16, "wk")
    wv = P.sbuf([128, 8, 256], BF16, "wv")
    xTb = [P.sbuf([128, 8, 512], BF16, f"xT{i}") for i in range(2)]
    qksb = [P.sbuf([128, 512], BF16, f"qksb{i}") for i in range(2)]
    tA = P.sbuf([128, 8, 8], F32, "tA")
    tB = P.sbuf([128, 8, 8], F32, "tB")
    ET = [[P.sbuf([128, 512], BF16, f"ET{m}_{i}") for i in range(3)] for m in range(2)]
    ocp = [P.sbuf([128, 3, 512], F32, f"ocp{i}") for i in range(2)]
    rl = P.sbuf([128, 2], F32, "rl")
    t0 = P.sbuf([128, 128], F32, "t0")
    av = P.sbuf([128, 128], F32, "av")
    junk = P.sbuf([128, 128], F32, "junk")
    ss = P.sbuf([128, 1], F32, "ss")
    rstd = P.sbuf([128, 1], F32, "rstd")
    ot = [P.sbuf([128, 128], F32, f"ot{i}") for i in range(2)]
    P.op("pool", lambda e: e.memset(Vaug[:], 1.0), (), ["Vaug_init"])
    outs = []
    ocount = [0]

    for hp in range(2):
        if dbg == 4:
            break
        for w_, wd_, nm in ((wq, wq_d, "wq"), (wk, wk_d, "wk"), (wv, wv_d, "wv")):
            P.ld("pool", w_[:], wd_.ap()[:, hp * 256:(hp + 1) * 256].rearrange("(k p) n -> p k n", p=128), [nm])
        for tt in range(NT):
            c = tt // 4
            if tt % 4 == 0:
                P.ld("pool", xTb[c % 2][:], xT_d.ap()[:, c * 512:(c + 1) * 512].rearrange("(k p) t -> p k t", p=128), [f"xT{c % 2}"])
            xs = xTb[c % 2]
            xk = f"xT{c % 2}"
            off = (tt % 4) * 128
            s = tt % 2
            pqk = pb[s]
            pv = pb[2 + s]
            for k in range(8):
                P.mm(pqk[:, 0:256], xs[:, k, off:off + 128], wq[:, k, :], k == 0, False, [xk, "wq"], [f"pb{s}"])
            for k in range(8):
                P.mm(pqk[:, 256:512], xs[:, k, off:off + 128], wk[:, k, :], False, k == 7, [xk, "wk"], [f"pb{s}"])
            for k in range(8):
                P.mm(pv[:, 0:256], xs[:, k, off:off + 128], wv[:, k, :], k == 0, k == 7, [xk, "wv"], [f"pb{2 + s}"])
            if dbg == 5:
                continue
            qv = pqk[:].rearrange("p (g d) -> p g d", g=8)
            qs_ = qksb[s][:].rearrange("p (g d) -> p g d", g=8)
            P.cp("act", qs_[:, :, 16:64], qv[:, :, 16:64], [f"pb{s}"], [f"qkrest{s}"])
            if dbg == 7:
                continue
            cosb = cs[:, tt, 0:8].unsqueeze(1).to_broadcast([128, 8, 8])
            sinb = cs[:, tt, 8:16].unsqueeze(1).to_broadcast([128, 8, 8])
            P.tt("dve", tA[:], qv[:, :, 0:8], cosb, ALU.mult, [f"pb{s}", "cs"], ["tA"])
            P.tt("dve", tB[:], qv[:, :, 8:16], sinb, ALU.mult, [f"pb{s}", "cs"], ["tB"])
            if dbg == 8:
                continue
            P.tt("dve", qs_[:, :, 0:8], tA[:], tB[:], ALU.subtract, ["tA", "tB"], [f"qkrot{s}a"])
            P.tt("dve", tA[:], qv[:, :, 0:8], sinb, ALU.mult, [f"pb{s}", "cs"], ["tA"])
            P.tt("dve", tB[:], qv[:, :, 8:16], cosb, ALU.mult, [f"pb{s}", "cs"], ["tB"])
            P.tt("dve", qs_[:, :, 8:16], tA[:], tB[:], ALU.add, ["tA", "tB"], [f"qkrot{s}b"])
            if dbg == 6:
                continue
            for blk in range(4):
                P.tr(pbb[:, s * 512 + blk * 128:s * 512 + (blk + 1) * 128], qksb[s][:, blk * 128:(blk + 1) * 128], identb[:],
                     [f"qkrest{s}", f"qkrot{s}a", f"qkrot{s}b", "identb"], [f"pbb{s}"])
            P.cp("dve" if s else "act", QKT[:, :, tt * 128:(tt + 1) * 128], pbb[:, s * 512:(s + 1) * 512].rearrange("p (b t) -> p b t", b=4),
                 [f"pbb{s}"], [f"QKT{tt}"])
            P.cp("act" if s else "dve", Vaug[:, tt, :, 0:128], pv[:, 0:256].rearrange("p (h d) -> p h d", h=2),
                 [f"pb{2 + s}", "Vaug_init"], [f"V{tt}"])

        if dbg == 1:
            break
        steps = [(hh, j, kt) for hh in range(2) for j in range(NCH) for kt in range(4 * j + 4)]

        def emit_qk(n):
            hh, j, kt = steps[n]
            d = kt - 4 * j
            qs = 128 * max(d, 0)
            for m in range(2):
                bank = (n % 2) * 2 + m
                pS = pb[bank]
                rd = [f"QKT{t}" for t in range(4 * j + qs // 128, 4 * j + 4)] + [f"QKT{kt}"]
                P.mm(pS[:, qs:512], QKT[m * 64:(m + 1) * 64, 2 + hh, kt * 128:(kt + 1) * 128],
                     QKT[m * 64:(m + 1) * 64, hh, j * 512 + qs:(j + 1) * 512], True, d < 0, rd, [f"pb{bank}"])
                if d >= 0:
                    P.mm(pS[:, qs:qs + 128], identb[:], trib[:], False, True, ["identb", "trib"], [f"pb{bank}"])

        def emit_exp(n):
            hh, j, kt = steps[n]
            d = kt - 4 * j
            qs = 128 * max(d, 0)
            for m in range(2):
                bank = (n % 2) * 2 + m
                P.act(ET[m][n % 3][:, qs:512], pb[bank][:, qs:512], AF.Exp, [f"pb{bank}"], [f"ET{m}_{n % 3}"], scale=0.125)

        def emit_pv(n):
            hh, j, kt = steps[n]
            d = kt - 4 * j
            for t in range(8):
                m, qsub = t // 4, t % 4
                if qsub < d:
                    continue
                bank = 4 + t // 3
                col = (t % 3) * 129
                first = (kt == 0) and (t % 3 == 0)
                P.op("pe", (lambda bank, col, m, n, qsub, kt, hh, first: lambda e: e.matmul(
                    pb[bank][:, col:col + 129], ET[m][n % 3][:, qsub * 128:(qsub + 1) * 128], Vaug[:, kt, hh, :],
                    start=first, stop=False, skip_group_check=True))(bank, col, m, n, qsub, kt, hh, first),
                    [f"ET{m}_{n % 3}", f"V{kt}"], [f"pb{bank}"])

        def emit_final(n):
            hh, j, kt = steps[n]
            oc = ocp[ocount[0] % 2]
            ock = f"ocp{ocount[0] % 2}"
            ocount[0] += 1
            for b3 in range(3):
                ncol = 387 if b3 < 2 else 258
                P.cp("act" if b3 == 1 else "dve", oc[:, b3, 0:ncol], pb[4 + b3][:, 0:ncol], [f"pb{4 + b3}"], [f"{ock}_{b3}"])
            for qsub in range(4):
                t0_, t1_ = qsub, 4 + qsub
                O0 = oc[:, t0_ // 3, (t0_ % 3) * 129:(t0_ % 3) * 129 + 129]
                O1 = oc[:, t1_ // 3, (t1_ % 3) * 129:(t1_ % 3) * 129 + 129]
                k0, k1 = f"{ock}_{t0_ // 3}", f"{ock}_{t1_ // 3}"
                P.op("dve", (lambda O0: lambda e: e.reciprocal(out=rl[:, 0:1], in_=O0[:, 128:129]))(O0), [k0], ["rl0"])
                P.op("dve", (lambda O1: lambda e: e.reciprocal(out=rl[:, 1:2], in_=O1[:, 128:129]))(O1), [k1], ["rl1"])
                P.tt("dve", rl[:, 1:2], rl[:, 1:2], nlam[:], ALU.mult, ["rl1", "nlam"], ["rl1"])
                P.ts("dve", t0[:], O0[:, 0:128], rl[:, 0:1], None, ALU.mult, None, [k0, "rl0"], ["t0"])
                P.stt(av[:], O1[:, 0:128], rl[:, 1:2], t0[:], ALU.mult, ALU.add, [k1, "rl1", "t0"], ["av"])
                P.stt(junk[:], av[:], 1.0, av[:], ALU.mult, ALU.mult, ["av"], ["junk", "ss"], accum=ss[:, 0:1])
                P.act(rstd[:], ss[:], AF.Sqrt, ["ss", "epst"], ["rstd"], bias=epst[:, 0:1], scale=1.0 / 128.0)
                P.op("dve", lambda e: e.reciprocal(out=rstd[:], in_=rstd[:]), ["rstd"], ["rstd"])
                osl = (j * 4 + qsub) % 2
                P.stt(ot[osl][:], av[:], rstd[:, 0:1], subgs[:], ALU.mult, ALU.mult, ["av", "rstd", "subgs"], [f"ot{osl}"])
                r0 = (j * 4 + qsub) * 128
                hcol = (hp * 2 + hh) * 128
                outs.append(P.st("sp", o_d.ap()[r0:r0 + 128, hcol:hcol + 128], ot[osl][:], [f"ot{osl}"], f"ost{osl}"))

        nsteps = len(steps)
        emit_qk(0)
        for n in range(nsteps):
            emit_exp(n)
            if n + 1 < nsteps:
                emit_qk(n + 1)
            if dbg != 2:
                emit_pv(n)
            hh, j, kt = steps[n]
            if kt == 4 * j + 3 and dbg not in (2, 3):
                emit_final(n)
    P.emit(final_wait_ops=outs)
    return nc


def attn_consts(S):
    inv = (500000.0 ** (-np.arange(0, 16, 2, dtype=np.float32) / 16.0)).astype(np.float32)
    ang = np.arange(S, dtype=np.float32)[:, None] * inv[None, :]
    cs = np.concatenate([np.cos(ang), np.sin(ang)], axis=1).astype(np.float32)
    cs = np.ascontiguousarray(cs.reshape(S // 128, 128, 16).transpose(1, 0, 2).reshape(128, (S // 128) * 16))
    k = np.arange(128)[:, None]
    q = np.arange(128)[None, :]
    tri = np.where(q >= k, 0.0, -30000.0).astype(np.float32)
    return dict(cs=cs, trimask=tri, ident=np.eye(128, dtype=np.float32))


def bfv(pbank):
    return pbank[:].bitcast(BF16)


def attn_phase(P, nc, pb, S, src_d, src_row, wq_a, wk_a, wv_a, cs_d, lqk_a, linit_a, subg_a, ident_d, tri_d, o_d):
    P.reset_sbuf()
    NT = S // 128
    NCH = S // 512
    identb = P.sbuf([128, 128], BF16, "identb")
    trib = P.sbuf([128, 128], BF16, "trib")
    cs = P.sbuf([128, NT, 16], F32, "cs")
    lqk = P.sbuf([128, 4, 64], F32, "lqk")
    linit = P.sbuf([128, 1], F32, "linit")
    subg = P.sbuf([128, 128], F32, "subg")
    subgs = P.sbuf([128, 128], F32, "subgs")
    junk64 = P.sbuf([128, 64], F32, "junk64")
    ssum = P.sbuf([128, 2], F32, "ssum")
    esum = P.sbuf([128, 2], F32, "esum")
    nlam = P.sbuf([128, 1], F32, "nlam")
    om = P.sbuf([128, 1], F32, "om")
    epst = P.sbuf([128, 1], F32, "epst")
    P.ld("pool", identb[:], ident_d.ap(), ["identb"])
    P.ld("pool", trib[:], tri_d.ap(), ["trib"])
    P.ld("sp", cs[:].rearrange("p t c -> p (t c)"), cs_d.ap(), ["cs"])
    for n in range(4):
        P.ld("sp", lqk[:, n, :], lqk_a[n:n + 1, :].partition_broadcast(128), [f"lqk{n}"])
    P.ld("sp", linit[:], linit_a.partition_broadcast(128), ["linit"])
    P.ld("sp", subg[:], subg_a.partition_broadcast(128), ["subg"])
    P.op("pool", lambda e: e.memset(epst[:], SUBLN_EPS), (), ["epst"])
    P.stt(junk64[:], lqk[:, 0, :], 1.0, lqk[:, 1, :], ALU.mult, ALU.mult, ["lqk0", "lqk1"], ["junk64", "ssum0"], accum=ssum[:, 0:1])
    P.stt(junk64[:], lqk[:, 2, :], 1.0, lqk[:, 3, :], ALU.mult, ALU.mult, ["lqk2", "lqk3"], ["junk64", "ssum1"], accum=ssum[:, 1:2])
    P.act(esum[:], ssum[:], AF.Exp, ["ssum0", "ssum1"], ["esum"])
    P.tt("dve", nlam[:], esum[:, 1:2], esum[:, 0:1], ALU.subtract, ["esum"], ["nlam"])
    P.tt("dve", nlam[:], nlam[:], linit[:], ALU.subtract, ["nlam", "linit"], ["nlam"])
    P.ts("dve", om[:], linit[:], -1.0, 1.0, ALU.mult, ALU.add, ["linit"], ["om"])
    P.ts("dve", subgs[:], subg[:], om[:, 0:1], None, ALU.mult, None, ["subg", "om"], ["subgs"])

    QKT = P.sbuf([128, 4, S], BF16, "QKT")
    Vaug = P.sbuf([128, NT, 2, 129], BF16, "Vaug")
    wq = P.sbuf([128, 8, 256], BF16, "wq")
    wk = P.sbuf([128, 8, 256], BF16, "wk")
    wv = P.sbuf([128, 8, 256], BF16, "wv")
    xt = [P.sbuf([128, D], BF16, f"xt{i}") for i in range(2)]
    xTt = [P.sbuf([128, 8, 128], BF16, f"xTt{i}") for i in range(2)]
    qksb = [P.sbuf([128, 512], BF16, f"qksb{i}") for i in range(2)]
    tA = P.sbuf([128, 8, 8], F32, "tA")
    tB = P.sbuf([128, 8, 8], F32, "tB")
    ET = [[P.sbuf([128, 512], BF16, f"ET{m}_{i}") for i in range(3)] for m in range(2)]
    ocp = [P.sbuf([128, 3, 512], F32, f"ocp{i}") for i in range(2)]
    rl = P.sbuf([128, 2], F32, "rl")
    t0 = P.sbuf([128, 128], F32, "t0")
    av = P.sbuf([128, 128], F32, "av")
    junk = P.sbuf([128, 128], F32, "junk")
    ss = P.sbuf([128, 1], F32, "ss")
    rstd = P.sbuf([128, 1], F32, "rstd")
    ot = [P.sbuf([128, 128], F32, f"ot{i}") for i in range(2)]
    P.op("pool", lambda e: e.memset(Vaug[:], 1.0), (), ["Vaug_init"])
    ocount = [0]
    pbbq = bfv(pb[7])
    pbbx = bfv(pb[6])

    for hp in range(2):
        for w_, wa_, nm in ((wq, wq_a, "wq"), (wk, wk_a, "wk"), (wv, wv_a, "wv")):
            P.ld("pool", w_[:], wa_[:, hp * 256:(hp + 1) * 256].rearrange("(k p) n -> p k n", p=128), [nm])
        for tt in range(NT):
            s = tt % 2
            P.ld("pool", xt[s][:], src_d.ap()[src_row(tt):src_row(tt) + 128, :], [f"xt{s}"])
            for k in range(8):
                P.tr(pbbx[:, k * 128:(k + 1) * 128], xt[s][:, k * 128:(k + 1) * 128], identb[:], [f"xt{s}", "identb"], ["pb6"])
            P.cp("act", xTt[s][:, 0:4, :], pbbx[:, 0:512].rearrange("p (k t) -> p k t", k=4), ["pb6"], [f"xTt{s}a"])
            P.cp("dve", xTt[s][:, 4:8, :], pbbx[:, 512:1024].rearrange("p (k t) -> p k t", k=4), ["pb6"], [f"xTt{s}b"])
            xk = [f"xTt{s}a", f"xTt{s}b"]
            pqk = pb[s]
            pv = pb[2 + s]
            for k in range(8):
                P.mm(pqk[:, 0:256], xTt[s][:, k, :], wq[:, k, :], k == 0, False, xk + ["wq"], [f"pb{s}"])
            for k in range(8):
                P.mm(pqk[:, 256:512], xTt[s][:, k, :], wk[:, k, :], False, k == 7, xk + ["wk"], [f"pb{s}"])
            for k in range(8):
                P.mm(pv[:, 0:256], xTt[s][:, k, :], wv[:, k, :], k == 0, k == 7, xk + ["wv"], [f"pb{2 + s}"])
            qv = pqk[:].rearrange("p (g d) -> p g d", g=8)
            qs_ = qksb[s][:].rearrange("p (g d) -> p g d", g=8)
            P.cp("act", qs_[:, :, 16:64], qv[:, :, 16:64], [f"pb{s}"], [f"qkrest{s}"])
            cosb = cs[:, tt, 0:8].unsqueeze(1).to_broadcast([128, 8, 8])
            sinb = cs[:, tt, 8:16].unsqueeze(1).to_broadcast([128, 8, 8])
            P.tt("dve", tA[:], qv[:, :, 0:8], cosb, ALU.mult, [f"pb{s}", "cs"], ["tA"])
            P.tt("dve", tB[:], qv[:, :, 8:16], sinb, ALU.mult, [f"pb{s}", "cs"], ["tB"])
            P.tt("dve", qs_[:, :, 0:8], tA[:], tB[:], ALU.subtract, ["tA", "tB"], [f"qkrot{s}a"])
            P.tt("dve", tA[:], qv[:, :, 0:8], sinb, ALU.mult, [f"pb{s}", "cs"], ["tA"])
            P.tt("dve", tB[:], qv[:, :, 8:16], cosb, ALU.mult, [f"pb{s}", "cs"], ["tB"])
            P.tt("dve", qs_[:, :, 8:16], tA[:], tB[:], ALU.add, ["tA", "tB"], [f"qkrot{s}b"])
            for blk in range(4):
                P.tr(pbbq[:, s * 512 + blk * 128:s * 512 + (blk + 1) * 128], qksb[s][:, blk * 128:(blk + 1) * 128], identb[:],
                     [f"qkrest{s}", f"qkrot{s}a", f"qkrot{s}b", "identb"], ["pb7"])
            P.cp("dve" if s else "act", QKT[:, :, tt * 128:(tt + 1) * 128], pbbq[:, s * 512:(s + 1) * 512].rearrange("p (b t) -> p b t", b=4),
                 ["pb7"], [f"QKT{tt}"])
            P.cp("act" if s else "dve", Vaug[:, tt, :, 0:128], pv[:, 0:256].rearrange("p (h d) -> p h d", h=2),
                 [f"pb{2 + s}", "Vaug_init"], [f"V{tt}"])

        steps = [(hh, j, kt) for hh in range(2) for j in range(NCH) for kt in range(4 * j + 4)]

        def emit_qk(n):
            hh, j, kt = steps[n]
            d = kt - 4 * j
            qs = 128 * max(d, 0)
            for m in range(2):
                bank = (n % 2) * 2 + m
                pS = pb[bank]
                rd = [f"QKT{t}" for t in range(4 * j + qs // 128, 4 * j + 4)] + [f"QKT{kt}"]
                P.mm(pS[:, qs:512], QKT[m * 64:(m + 1) * 64, 2 + hh, kt * 128:(kt + 1) * 128],
                     QKT[m * 64:(m + 1) * 64, hh, j * 512 + qs:(j + 1) * 512], True, d < 0, rd, [f"pb{bank}"])
                if d >= 0:
                    P.mm(pS[:, qs:qs + 128], identb[:], trib[:], False, True, ["identb", "trib"], [f"pb{bank}"])

        def emit_exp(n):
            hh, j, kt = steps[n]
            d = kt - 4 * j
            qs = 128 * max(d, 0)
            for m in range(2):
                bank = (n % 2) * 2 + m
                P.act(ET[m][n % 3][:, qs:512], pb[bank][:, qs:512], AF.Exp, [f"pb{bank}"], [f"ET{m}_{n % 3}"], scale=0.125)

        def emit_pv(n):
            hh, j, kt = steps[n]
            d = kt - 4 * j
            for t in range(8):
                m, qsub = t // 4, t % 4
                if qsub < d:
                    continue
                bank = 4 + t // 3
                col = (t % 3) * 129
                first = (kt == 0) and (t % 3 == 0)
                P.op("pe", (lambda bank, col, m, n, qsub, kt, hh, first: lambda e: e.matmul(
                    pb[bank][:, col:col + 129], ET[m][n % 3][:, qsub * 128:(qsub + 1) * 128], Vaug[:, kt, hh, :],
                    start=first, stop=False, skip_group_check=True))(bank, col, m, n, qsub, kt, hh, first),
                    [f"ET{m}_{n % 3}", f"V{kt}"], [f"pb{bank}"])

        def emit_final(n):
            hh, j, kt = steps[n]
            oc = ocp[ocount[0] % 2]
            ock = f"ocp{ocount[0] % 2}"
            ocount[0] += 1
            for b3 in range(3):
                ncol = 387 if b3 < 2 else 258
                P.cp("act" if b3 == 1 else "dve", oc[:, b3, 0:ncol], pb[4 + b3][:, 0:ncol], [f"pb{4 + b3}"], [f"{ock}_{b3}"])
            for qsub in range(4):
                t0_, t1_ = qsub, 4 + qsub
                O0 = oc[:, t0_ // 3, (t0_ % 3) * 129:(t0_ % 3) * 129 + 129]
                O1 = oc[:, t1_ // 3, (t1_ % 3) * 129:(t1_ % 3) * 129 + 129]
                k0, k1 = f"{ock}_{t0_ // 3}", f"{ock}_{t1_ // 3}"
                P.op("dve", (lambda O0: lambda e: e.reciprocal(out=rl[:, 0:1], in_=O0[:, 128:129]))(O0), [k0], ["rl0"])
                P.op("dve", (lambda O1: lambda e: e.reciprocal(out=rl[:, 1:2], in_=O1[:, 128:129]))(O1), [k1], ["rl1"])
                P.tt("dve", rl[:, 1:2], rl[:, 1:2], nlam[:], ALU.mult, ["rl1", "nlam"], ["rl1"])
                P.ts("dve", t0[:], O0[:, 0:128], rl[:, 0:1], None, ALU.mult, None, [k0, "rl0"], ["t0"])
                P.stt(av[:], O1[:, 0:128], rl[:, 1:2], t0[:], ALU.mult, ALU.add, [k1, "rl1", "t0"], ["av"])
                P.stt(junk[:], av[:], 1.0, av[:], ALU.mult, ALU.mult, ["av"], ["junk", "ss"], accum=ss[:, 0:1])
                P.act(rstd[:], ss[:], AF.Sqrt, ["ss", "epst"], ["rstd"], bias=epst[:, 0:1], scale=1.0 / 128.0)
                P.op("dve", lambda e: e.reciprocal(out=rstd[:], in_=rstd[:]), ["rstd"], ["rstd"])
                osl = (j * 4 + qsub) % 2
                P.stt(ot[osl][:], av[:], rstd[:, 0:1], subgs[:], ALU.mult, ALU.mult, ["av", "rstd", "subgs"], [f"ot{osl}"])
                r0 = (j * 4 + qsub) * 128
                hcol = (hp * 2 + hh) * 128
                P.st("sp", o_d.ap()[r0:r0 + 128, hcol:hcol + 128], ot[osl][:], [f"ot{osl}"], f"ost{osl}")

        nsteps = len(steps)
        emit_qk(0)
        for n in range(nsteps):
            emit_exp(n)
            if n + 1 < nsteps:
                emit_qk(n + 1)
            emit_pv(n)
            hh, j, kt = steps[n]
            if kt == 4 * j + 3:
                emit_final(n)


def tail_phase(P, nc, pb, kind, T, C, NEXP, hsrc_fn, hdst_fn, W, scr, final):
    P.reset_sbuf()
    NTL = T // 128
    NSB = C // 128
    h1_d, xin_d, y_d = scr["h1"], scr["xin"], scr["y"]
    ident = P.sbuf([128, 128], F32, "ident")
    identb = P.sbuf([128, 128], BF16, "identb")
    ustr = P.sbuf([128, 128], BF16, "ustr")
    ones = P.sbuf([128, 128], BF16, "ones")
    ecoff = P.sbuf([128, 32], F32, "ecoff")
    brt = P.sbuf([128, 36], F32, "brt")
    wrt = P.sbuf([128, 8, 36], F32, "wrt")
    lng = P.sbuf([128, 2, D], F32, "lng")
    lnb = P.sbuf([128, 2, D], F32, "lnb")
    epst = P.sbuf([128, 1], F32, "epst")
    Scnt = P.sbuf([128, 32], BF16, "Scnt")
    gates = P.sbuf([128, NTL, 2], F32, "gates")
    dest = P.sbuf([128, NTL, 2], I32, "dest")
    P.ld("sp", ident[:], W["ident"], ["ident"])
    P.ld("pool", identb[:], W["ident"], ["identb"])
    P.ld("pool", ustr[:], W["ustrict"], ["ustr"])
    P.op("pool", lambda e: e.memset(ones[:], 1.0), (), ["ones"])
    P.op("pool", lambda e: e.memset(epst[:], LN_EPS), (), ["epst"])
    P.op("pool", lambda e: e.memset(Scnt[:], 0.0), (), ["Scnt"])
    P.ld("sp", ecoff[:], W["ecoff"].partition_broadcast(128), ["ecoff"])
    P.ld("sp", brt[:], W["b_rt"].partition_broadcast(128), ["brt"])
    P.ld("sp", wrt[:], W["w_rt"].rearrange("(k p) n -> p k n", p=128), ["wrt"])
    for j in range(2):
        P.ld("sp", lng[:, j, :], W["ln_g"][j:j + 1, :].partition_broadcast(128), [f"lng{j}"])
        P.ld("sp", lnb[:, j, :], W["ln_b"][j:j + 1, :].partition_broadcast(128), [f"lnb{j}"])
    zt = P.sbuf([128, D], BF16, "zt")
    P.op("pool", lambda e: e.memset(zt[:], 0.0), (), ["zt"])
    nz = (NE * C) // 128
    for z in range(nz):
        P.dma("sp", (lambda z: lambda e: e.dma_start(out=xin_d.ap()[z * 128:(z + 1) * 128, :], in_=zt[:]))(z), ["zt"], ["xinz"] if z == nz - 1 else [f"xinz{z}"], semkey="xinz")
    pbb = bfv(pb[6])

    if kind == "attn":
        wo = P.sbuf([128, 8, D], BF16, "wo")
        P.ld("pool", wo[:], W["w_o"].rearrange("(k p) n -> p k n", p=128), ["wo"])
        oidx = P.sbuf([128, NTL * 2], I32, "oidx")
        P.ld("sp", oidx[:], W["oidx"], ["oidx"])
        og = [P.sbuf([128, D], F32, f"og{i}") for i in range(2)]
        ob = P.sbuf([128, D], BF16, "ob")
        oTt = P.sbuf([128, 8, 128], BF16, "oTt")
    else:
        win = P.sbuf([128, 8, D], BF16, "win")
        wout = P.sbuf([128, 8, D], BF16, "wout")
        wgrp = P.sbuf([128, 4, 2, 256], BF16, "wgrp")
        lsT = P.sbuf([128, 8], F32, "lsT")
        am0 = P.sbuf([128, 4, 128], BF16, "am0")
        amd = P.sbuf([128, 4, 128], BF16, "amd")
        amo = P.sbuf([128, 4, 128], BF16, "amo")
        hidx = P.sbuf([128, 1], I32, "hidx")
        P.ld("sp", hidx[:], W["hidx"], ["hidx"])
        P.ld("pool", win[:], W["w_in"].rearrange("(k p) n -> p k n", p=128), ["win"])
        P.ld("pool", wout[:], W["w_out"].rearrange("(k p) n -> p k n", p=128), ["wout"])
        P.ld("pool", wgrp[:], W["w_grp"].rearrange("g (k p) n -> p g k n", p=128), ["wgrp"])
        P.ld("sp", lsT[:], W["lsT"], ["lsT"])
        P.ld("pool", am0[:], W["am0"].rearrange("w p n -> p w n"), ["am0"])
        P.ld("pool", amd[:], W["amd"].rearrange("w p n -> p w n"), ["amd"])
        P.ld("pool", amo[:], W["amo"].rearrange("w p n -> p w n"), ["amo"])
        ubuf = [P.sbuf([128, D], BF16, f"u{i}") for i in range(3)]
        hTb = P.sbuf([128, 8, 128], BF16, "hTb")
        pT = P.sbuf([128, 8, 128], BF16, "pT")
        qT = P.sbuf([128, 8, 128], BF16, "qT")

    htile = [P.sbuf([128, D], F32, f"ht{i}") for i in range(2)]
    rt = [P.sbuf([128, D], F32, f"rt{i}") for i in range(2)]
    h1b = [P.sbuf([128, D], BF16, f"h1b{i}") for i in range(2)]
    h1T = P.sbuf([128, 8, 128], F32, "h1T")
    stats = P.sbuf([128, 2, 6], F32, "stats")
    mv = P.sbuf([128, 2], F32, "mv")
    rstd = P.sbuf([128, 1], F32, "rstd")
    nmr = P.sbuf([128, 1], F32, "nmr")
    L = P.sbuf([128, 36], F32, "L")
    gmax = P.sbuf([128, 1], F32, "gmax")
    ngmax = P.sbuf([128, 1], F32, "ngmax")
    G1 = P.sbuf([128, 4], F32, "G1")
    ge = P.sbuf([128, 4], F32, "ge")
    gsum = P.sbuf([128, 1], F32, "gsum")
    pen = P.sbuf([128, 4], F32, "pen")
    Lm = P.sbuf([128, 32], F32, "Lm")
    Lm2 = P.sbuf([128, 32], F32, "Lm2")
    m1 = P.sbuf([128, 1], F32, "m1")
    m2 = P.sbuf([128, 1], F32, "m2")
    OH1 = P.sbuf([128, 32], F32, "OH1")
    OH2 = P.sbuf([128, 32], F32, "OH2")
    OHc = P.sbuf([128, 32], BF16, "OHc")
    dd = P.sbuf([128, 1], F32, "dd")
    ex = P.sbuf([128, 1], F32, "ex")
    den = P.sbuf([128, 1], F32, "den")
    Pf = P.sbuf([128, 32], F32, "Pf")
    tmp32 = P.sbuf([128, 32], F32, "tmp32")
    dflt = P.sbuf([128, 2], F32, "dflt")

    def layer_norm(src, j, dst, keys_r, key_w):
        sv = src.rearrange("p (c f) -> p c f", c=2)
        for c in range(2):
            P.op("dve", (lambda c: lambda e: e.bn_stats(out=stats[:, c, :], in_=sv[:, c, :]))(c), keys_r, [f"stats{c}"])
        P.op("dve", lambda e: e.bn_aggr(out=mv[:], in_=stats[:]), ["stats0", "stats1"], ["mv"])
        P.act(rstd[:], mv[:, 1:2], AF.Sqrt, ["mv", "epst"], ["rstd"], bias=epst[:, 0:1])
        P.op("dve", lambda e: e.reciprocal(out=rstd[:], in_=rstd[:]), ["rstd"], ["rstd"])
        P.stt(nmr[:], mv[:, 0:1], -1.0, rstd[:], ALU.mult, ALU.mult, ["mv", "rstd"], ["nmr"])
        P.act(dst, src, AF.Identity, list(keys_r) + ["rstd", "nmr"], [key_w], bias=nmr[:, 0:1], scale=rstd[:, 0:1])
        P.tt("dve", dst, dst, lng[:, j, :], ALU.mult, [key_w, f"lng{j}"], [key_w])
        P.tt("pool", dst, dst, lnb[:, j, :], ALU.add, [key_w, f"lnb{j}"], [key_w])

    def pool_u(i, slot, hslot):
        hs = htile[hslot]
        hk = f"ht{hslot}"
        if i < 0:
            P.dma("pool", lambda e: e.indirect_dma_start(out=hs[:], out_offset=None, in_=scr["halo_all"].ap(),
                                                         in_offset=bass.IndirectOffsetOnAxis(ap=hidx[:, 0:1], axis=0)),
                  ["hidx"], [hk], semkey=hk)
        for k in range(8):
            P.tr(pb[2 + k // 4][:, (k % 4) * 128:(k % 4 + 1) * 128], hs[:, k * 128:(k + 1) * 128], ident[:], [hk, "ident"], [f"pb{2 + k // 4}"])
        for hf in range(2):
            P.cp("act" if hf == 0 else "dve", hTb[:, hf * 4:(hf + 1) * 4, :], pb[2 + hf][:].rearrange("p (k t) -> p k t", k=4), [f"pb{2 + hf}"], [f"hTb{hf}"])
        for hf in range(2):
            for k in range(8):
                P.mm(pb[hf][:], hTb[:, k, :], win[:, k, hf * 512:(hf + 1) * 512], k == 0, k == 7, [f"hTb{k // 4}", "win"], [f"pb{hf}"])
        for hf in range(2):
            P.cp("act" if hf == 0 else "dve", ubuf[slot][:, hf * 512:(hf + 1) * 512], pb[hf][:], [f"pb{hf}"], [f"u{slot}_{hf}"])

    if kind == "pool":
        pool_u(-1, 2, 1)

    for i in range(NTL):
        s = i % 2
        hk = f"ht{s}"
        P.ld("sp", htile[s][:], hsrc_fn(i), [hk])
        if kind == "attn":
            for g in range(2):
                P.dma("pool", (lambda i, g, s: lambda e: e.indirect_dma_start(
                    out=og[s][:, g * 512:(g + 1) * 512], out_offset=None, in_=scr["o_all"].ap(),
                    in_offset=bass.IndirectOffsetOnAxis(ap=oidx[:, i * 2 + g:i * 2 + g + 1], axis=0)))(i, g, s),
                    ["oidx"], [f"og{s}_{g}"], semkey=f"og{s}_{g}")
            P.cp("act", ob[:], og[s][:], [f"og{s}_0", f"og{s}_1"], ["ob"])
            for k in range(8):
                P.tr(pbb[:, k * 128:(k + 1) * 128], ob[:, k * 128:(k + 1) * 128], identb[:], ["ob", "identb"], ["pb6"])
            P.cp("dve", oTt[:].rearrange("p k t -> p (k t)"), pbb, ["pb6"], ["oTt"])
            for hf in range(2):
                for k in range(8):
                    P.mm(pb[hf][:], oTt[:, k, :], wo[:, k, hf * 512:(hf + 1) * 512], k == 0, k == 7, ["oTt", "wo"], [f"pb{hf}"])
        else:
            us = i % 3
            up = (i - 1) % 3
            pool_u(i, us, s)
            A = am0 if i == 0 else amd
            Ak = "am0" if i == 0 else "amd"
            for j in range(8):
                g = j // 2
                o_ = pb[4 + j // 4][:, (j % 4) * 128:(j % 4 + 1) * 128]
                P.mm(o_, ubuf[us][:, j * 128:(j + 1) * 128], A[:, g, :], True, False, [f"u{us}_{j // 4}", Ak], [f"pb{4 + j // 4}"])
                P.mm(o_, ubuf[up][:, j * 128:(j + 1) * 128], amo[:, g, :], False, True, [f"u{up}_{j // 4}", "amo"], [f"pb{4 + j // 4}"])
            for hf in range(2):
                P.cp("act" if hf == 0 else "dve", pT[:, hf * 4:(hf + 1) * 4, :], pb[4 + hf][:].rearrange("p (k t) -> p k t", k=4), [f"pb{4 + hf}"], [f"pT{hf}"])
            for j in range(8):
                g = j // 2
                o_ = pb[4 + j // 4][:, (j % 4) * 128:(j % 4 + 1) * 128]
                for kk in range(2):
                    P.mm(o_, wgrp[:, g, kk, (j % 2) * 128:(j % 2 + 1) * 128], pT[:, 2 * g + kk, :], kk == 0, kk == 1, [f"pT{(2 * g + kk) // 4}", "wgrp"], [f"pb{4 + j // 4}"])
            for j in range(8):
                P.act(qT[:, j, :], pb[4 + j // 4][:, (j % 4) * 128:(j % 4 + 1) * 128], AF.Identity, [f"pb{4 + j // 4}", "lsT"], [f"qT{j}"], scale=lsT[:, j:j + 1])
            for hf in range(2):
                for k in range(8):
                    P.mm(pb[hf][:], qT[:, k, :], wout[:, k, hf * 512:(hf + 1) * 512], k == 0, k == 7, [f"qT{k}", "wout"], [f"pb{hf}"])
        r = rt[s]
        rk = f"rt{s}"
        for hf in range(2):
            P.stt(r[:, hf * 512:(hf + 1) * 512], htile[s][:, hf * 512:(hf + 1) * 512], ALPHA, pb[hf][:], ALU.mult, ALU.add, [hk, f"pb{hf}"], [rk])
        layer_norm(r[:], 0, r[:], [rk], rk)
        P.st("sp", h1_d.ap()[i * 128:(i + 1) * 128, :], r[:], [rk], f"h1st{s}", [f"h1d{i}"])
        P.cp("act", h1b[s][:], r[:], [rk], [f"h1b{s}"])
        for k in range(8):
            P.tr(pb[2 + k // 4][:, (k % 4) * 128:(k % 4 + 1) * 128], r[:, k * 128:(k + 1) * 128], ident[:], [rk, "ident"], [f"pb{2 + k // 4}"])
        for hf in range(2):
            P.cp("act" if hf == 0 else "dve", h1T[:, hf * 4:(hf + 1) * 4, :], pb[2 + hf][:].rearrange("p (k t) -> p k t", k=4), [f"pb{2 + hf}"], [f"h1T{hf}"])
        for k in range(8):
            P.mm(pb[4][:, 0:36], h1T[:, k, :], wrt[:, k, :], k == 0, k == 7, [f"h1T{k // 4}", "wrt"], ["pb4"])
        P.tt("dve", L[:], pb[4][:, 0:36], brt[:], ALU.add, ["pb4", "brt"], ["L"])
        P.red(gmax[:], L[:, 0:4], ALU.max, ["L"], ["gmax"])
        P.tt("dve", G1[:], L[:, 0:4], gmax[:, 0:1].to_broadcast([128, 4]), ALU.is_equal, ["L", "gmax"], ["G1"])
        P.ts("dve", ngmax[:], gmax[:], -1.0, None, ALU.mult, None, ["gmax"], ["ngmax"])
        P.act(ge[:], L[:, 0:4], AF.Exp, ["L", "ngmax"], ["ge", "gsum"], bias=ngmax[:, 0:1], accum=gsum[:, 0:1])
        P.ts("dve", pen[:], G1[:], -1.0, 1e30, ALU.add, ALU.mult, ["G1"], ["pen"])
        P.tt("dve", Lm[:].rearrange("p (g e) -> p g e", g=4), L[:, 4:36].rearrange("p (g e) -> p g e", g=4),
             pen[:].unsqueeze(2).to_broadcast([128, 4, 8]), ALU.add, ["L", "pen"], ["Lm"])
        P.red(m1[:], Lm[:], ALU.max, ["Lm"], ["m1"])
        P.tt("dve", OH1[:], Lm[:], m1[:, 0:1].to_broadcast([128, 32]), ALU.is_equal, ["Lm", "m1"], ["OH1"])
        P.stt(Lm2[:], OH1[:], -1e30, Lm[:], ALU.mult, ALU.add, ["OH1", "Lm"], ["Lm2"])
        P.red(m2[:], Lm2[:], ALU.max, ["Lm2"], ["m2"])
        P.tt("dve", OH2[:], Lm2[:], m2[:, 0:1].to_broadcast([128, 32]), ALU.is_equal, ["Lm2", "m2"], ["OH2"])
        P.tt("dve", dd[:], m2[:], m1[:], ALU.subtract, ["m1", "m2"], ["dd"])
        P.act(ex[:], dd[:], AF.Exp, ["dd"], ["ex"])
        P.stt(den[:], ex[:], 1.0, gsum[:], ALU.add, ALU.mult, ["ex", "gsum"], ["den"])
        P.op("dve", (lambda i: lambda e: e.reciprocal(out=gates[:, i, 0:1], in_=den[:]))(i), ["den"], [f"gate{i}"])
        P.tt("dve", gates[:, i, 1:2], gates[:, i, 0:1], ex[:], ALU.mult, [f"gate{i}", "ex"], [f"gate{i}"])
        P.tt("dve", OHc[:], OH1[:], OH2[:], ALU.add, ["OH1", "OH2"], ["OHc"])
        P.mm(pb[5][:, 0:32], ustr[:], OHc[:], True, False, ["ustr", "OHc"], ["pb5"])
        P.mm(pb[5][:, 0:32], ones[:], Scnt[:], False, True, ["ones", "Scnt"], ["pb5"])
        P.tt("dve", Pf[:], pb[5][:, 0:32], ecoff[:], ALU.add, ["pb5", "ecoff"], ["Pf"])
        P.tt("dve", Scnt[:], Scnt[:], OHc[:], ALU.add, ["Scnt", "OHc"], ["Scnt"])
        P.stt(tmp32[:], OH1[:], 1.0, Pf[:], ALU.mult, ALU.mult, ["OH1", "Pf"], ["tmp32", "dflt0"], accum=dflt[:, 0:1])
        P.stt(tmp32[:], OH2[:], 1.0, Pf[:], ALU.mult, ALU.mult, ["OH2", "Pf"], ["tmp32", "dflt1"], accum=dflt[:, 1:2])
        P.cp("dve", dest[:, i, :], dflt[:], ["dflt0", "dflt1"], [f"dest{i}"])
        for kk in range(2):
            P.dma("pool", (lambda i, kk, s: lambda e: e.indirect_dma_start(
                out=xin_d.ap(), out_offset=bass.IndirectOffsetOnAxis(ap=dest[:, i, kk:kk + 1], axis=0),
                in_=h1b[s][:], in_offset=None))(i, kk, s), [f"h1b{s}", f"dest{i}", "xinz"], [f"xinw{i}_{kk}"], semkey=f"scat{s}{kk}")

    wgs = [P.sbuf([128, 8, DE], BF16, f"wg{i}") for i in range(2)]
    wus = [P.sbuf([128, 8, DE], BF16, f"wu{i}") for i in range(2)]
    wds = [P.sbuf([128, 4, D], BF16, f"wd{i}") for i in range(2)]
    xin = [P.sbuf([128, NSB, D], BF16, f"xin{i}") for i in range(2)]
    xT = [P.sbuf([128, 8, C], BF16, f"xT{i}") for i in range(2)]
    actT = [P.sbuf([128, 4, C], BF16, f"actT{i}") for i in range(2)]
    sg = [P.sbuf([128, C], F32, f"sg{i}") for i in range(2)]
    yb = [P.sbuf([128, D], F32, f"yb{i}") for i in range(2)]
    ycount = 0
    xin_keys = [f"xinw{i}_{kk}" for i in range(NTL) for kk in range(2)]
    y_keys = [f"y_{e_}_{sb}" for e_ in range(NEXP) for sb in range(NSB)]
    for e_ in range(NEXP):
        s = e_ % 2
        P.ld("pool", wgs[s][:], W["w_gate"][e_].rearrange("(k p) n -> p k n", p=128), [f"wg{s}"])
        P.ld("pool", wus[s][:], W["w_up"][e_].rearrange("(k p) n -> p k n", p=128), [f"wu{s}"])
        P.ld("pool", wds[s][:], W["w_down"][e_].rearrange("(k p) n -> p k n", p=128), [f"wd{s}"])
        P.ld("sp", xin[s][:], xin_d.ap()[e_ * C:(e_ + 1) * C, :].rearrange("(b p) n -> p b n", p=128), [f"xin{s}"], r=xin_keys)
        for sb in range(NSB):
            for k in range(8):
                P.tr(pbb[:, k * 128:(k + 1) * 128], xin[s][:, sb, k * 128:(k + 1) * 128], identb[:], [f"xin{s}", "identb"], ["pb6"])
            P.cp("dve" if sb % 2 else "act", xT[s][:, :, sb * 128:(sb + 1) * 128], pbb.rearrange("p (k t) -> p k t", k=8), ["pb6"], [f"xT{s}_{sb}"])
        xkeys = [f"xT{s}_{sb}" for sb in range(NSB)]
        for fc in range(4):
            pg = pb[(fc % 2) * 2]
            pu = pb[(fc % 2) * 2 + 1]
            kg, ku = f"pb{(fc % 2) * 2}", f"pb{(fc % 2) * 2 + 1}"
            for k in range(8):
                P.mm(pg[:, 0:C], wgs[s][:, k, fc * 128:(fc + 1) * 128], xT[s][:, k, :], k == 0, k == 7, xkeys + [f"wg{s}"], [kg])
            for k in range(8):
                P.mm(pu[:, 0:C], wus[s][:, k, fc * 128:(fc + 1) * 128], xT[s][:, k, :], k == 0, k == 7, xkeys + [f"wu{s}"], [ku])
            P.act(sg[fc % 2][:], pg[:, 0:C], AF.Silu, [kg], [f"sg{fc % 2}"])
            P.tt("dve", actT[s][:, fc, :], sg[fc % 2][:], pu[:, 0:C], ALU.mult, [f"sg{fc % 2}", ku], [f"actT{s}_{fc}"])
        akeys = [f"actT{s}_{fc}" for fc in range(4)]
        for sb in range(NSB):
            ys = ycount % 2
            ycount += 1
            for hf in range(2):
                py = pb[4 + hf]
                for fc in range(4):
                    P.mm(py[:], actT[s][:, fc, sb * 128:(sb + 1) * 128], wds[s][:, fc, hf * 512:(hf + 1) * 512], fc == 0, fc == 3, akeys + [f"wd{s}"], [f"pb{4 + hf}"])
                P.cp("act" if hf == 0 else "dve", yb[ys][:, hf * 512:(hf + 1) * 512], py[:], [f"pb{4 + hf}"], [f"yb{ys}"])
            r0 = e_ * C + sb * 128
            P.st("sp", y_d.ap()[r0:r0 + 128, :], yb[ys][:], [f"yb{ys}"], f"yst{ys}", [f"y_{e_}_{sb}"])

    y0 = [P.sbuf([128, D], F32, f"y0_{i}") for i in range(2)]
    y1 = [P.sbuf([128, D], F32, f"y1_{i}") for i in range(2)]
    outs = []
    for i in range(NTL):
        s = i % 2
        hk = f"ht{s}"
        P.ld("sp", htile[s][:], h1_d.ap()[i * 128:(i + 1) * 128, :], [hk], r=[f"h1d{i}"])
        for kk, yt in ((0, y0), (1, y1)):
            P.dma("pool", (lambda i, kk, yt, s: lambda e: e.indirect_dma_start(
                out=yt[s][:], out_offset=None, in_=y_d.ap(),
                in_offset=bass.IndirectOffsetOnAxis(ap=dest[:, i, kk:kk + 1], axis=0)))(i, kk, yt, s),
                y_keys + [f"dest{i}"], [f"y{kk}_{s}"], semkey=f"gath{kk}{s}")
        r = rt[s]
        rk = f"rt{s}"
        P.ts("pool", r[:], htile[s][:], ALPHA, None, ALU.mult, None, [hk], [rk])
        P.stt(r[:], y0[s][:], gates[:, i, 0:1], r[:], ALU.mult, ALU.add, [f"y0_{s}", f"gate{i}", rk], [rk])
        P.stt(r[:], y1[s][:], gates[:, i, 1:2], r[:], ALU.mult, ALU.add, [f"y1_{s}", f"gate{i}", rk], [rk])
        layer_norm(r[:], 1, r[:], [rk], rk)
        outs.append(P.st("sp", hdst_fn(i), r[:], [rk], f"ost{s}"))
    return outs


def build_mega(S, C, depth=DEPTH, NEXP=NE):
    T = S // 2
    NTL = T // 128
    nc = bass.Bass("TRN2", target_bir_lowering=False)
    P = Prog(nc)
    NA = (depth + 1) // 2
    NP = depth // 2
    inp = lambda name, shape, dt=F32: nc.dram_tensor(name, list(shape), dt, kind="ExternalInput")
    x_all = inp("x_all", [S, D])
    h0 = inp("h0", [T, D])
    oidx_d = inp("oidx", [128, NTL * 2], I32)
    hidx_d = inp("hidx", [128, 1], I32)
    wq_d = inp("wq", [NA, D, 512])
    wk_d = inp("wk", [NA, D, 512])
    wv_d = inp("wv", [NA, D, 512])
    wo_d = inp("w_o", [NA, D, D])
    lqk_d = inp("lqk", [NA, 4, 64])
    linit_d = inp("linit", [NA, 1])
    subg_d = inp("subg", [NA, 128])
    cs_d = inp("cs", [128, (S // 128) * 16])
    tri_d = inp("trimask", [128, 128])
    ident_d = inp("ident", [128, 128])
    ustr_d = inp("ustrict", [128, 128])
    ecoff_d = inp("ecoff", [1, 32])
    if NP:
        win_d = inp("w_in", [NP, D, D])
        wgrp_d = inp("w_grp", [NP, 4, 256, 256])
        lsT_d = inp("lsT", [NP, 128, 8])
        wout_d = inp("w_out", [NP, D, D])
        am0_d = inp("am0", [4, 128, 128])
        amd_d = inp("amd", [4, 128, 128])
        amo_d = inp("amo", [4, 128, 128])
    lng_d = inp("ln_g", [depth, 2, D])
    lnb_d = inp("ln_b", [depth, 2, D])
    wrt_d = inp("w_rt", [depth, D, 36])
    brt_d = inp("b_rt", [depth, 36])
    wg_d = inp("w_gate", [depth, NEXP, D, DE])
    wu_d = inp("w_up", [depth, NEXP, D, DE])
    wd_d = inp("w_down", [depth, NEXP, DE, D])
    out_d = nc.dram_tensor("out", [T, D], F32, kind="ExternalOutput")
    o_loc = nc.dram_tensor("o_loc", [S, 512], F32)
    o_all = nc.dram_tensor("o_all", [2 * S, 512], F32)
    hcur = nc.dram_tensor("hcur", [T, D], F32)
    h_all = nc.dram_tensor("h_all", [S, D], F32)
    halo_all = nc.dram_tensor("halo_all", [384, D], F32)
    scr = dict(h1=nc.dram_tensor("h1_scr", [T, D], F32), xin=nc.dram_tensor("xin_scr", [NE * C, D], BF16),
               y=nc.dram_tensor("y_scr", [NE * C, D], F32), o_all=o_all, halo_all=halo_all)
    pb = [P.psum([128, 512], F32, f"pb{i}") for i in range(8)]
    groups = [[0, 1], [2, 3], [4, 5], [6, 7]]
    CH_O = min(1024, S)
    CH_H = min(512, T)

    zf = P.sbuf([128, D], F32, "zf")
    P.op("pool", lambda e: e.memset(zf[:], 0.0), (), ["zf"])
    P.st("sp", halo_all.ap()[256:384, :], zf[:], ["zf"], "hz")
    P.fence()

    outs = []
    for i in range(depth):
        j = i // 2
        last = (i == depth - 1)
        W = dict(ident=ident_d.ap(), ustrict=ustr_d.ap(), ecoff=ecoff_d.ap(), b_rt=brt_d.ap()[i:i + 1, :], w_rt=wrt_d.ap()[i],
                 ln_g=lng_d.ap()[i], ln_b=lnb_d.ap()[i], w_gate=wg_d.ap()[i], w_up=wu_d.ap()[i], w_down=wd_d.ap()[i])
        hsrc = (lambda t: h0.ap()[t * 128:(t + 1) * 128, :]) if i == 0 else (lambda t: hcur.ap()[t * 128:(t + 1) * 128, :])
        hdst = (lambda t: out_d.ap()[t * 128:(t + 1) * 128, :]) if last else (lambda t: hcur.ap()[t * 128:(t + 1) * 128, :])
        if i % 2 == 0:
            if i == 0:
                src = x_all
                src_row = lambda tt: tt * 128
            else:
                for q in range(T // CH_H):
                    P.coll((lambda q: lambda e: e.collective_compute(
                        "AllGather", ALU.bypass, replica_groups=groups, ins=[hcur.ap()[q * CH_H:(q + 1) * CH_H, :]],
                        outs=[h_all.ap()[q * 2 * CH_H:(q + 1) * 2 * CH_H, :]]))(q), [], [], "cc")
                P.fence()
                src = h_all

                def src_row(tt):
                    r_, l_ = divmod(tt * 128, T)
                    return (l_ // CH_H) * 2 * CH_H + r_ * CH_H + l_ % CH_H
            attn_phase(P, nc, pb, S, src, src_row, wq_d.ap()[j], wk_d.ap()[j], wv_d.ap()[j], cs_d, lqk_d.ap()[j], linit_d.ap()[j:j + 1, :],
                       subg_d.ap()[j:j + 1, :], ident_d, tri_d, o_loc)
            P.fence()
            for q in range(S // CH_O):
                P.coll((lambda q: lambda e: e.collective_compute(
                    "AllGather", ALU.bypass, replica_groups=groups, ins=[o_loc.ap()[q * CH_O:(q + 1) * CH_O, :]],
                    outs=[o_all.ap()[q * 2 * CH_O:(q + 1) * 2 * CH_O, :]]))(q), [], [], "cc")
            P.fence()
            W.update(w_o=wo_d.ap()[j], oidx=oidx_d.ap())
            outs = tail_phase(P, nc, pb, "attn", T, C, NEXP, hsrc, hdst, W, scr, last)
        else:
            P.coll(lambda e: e.collective_compute("AllGather", ALU.bypass, replica_groups=groups,
                                                  ins=[hcur.ap()[T - 128:T, :]], outs=[halo_all.ap()[0:256, :]]), [], [], "cc")
            P.fence()
            W.update(w_in=win_d.ap()[j], w_grp=wgrp_d.ap()[j], lsT=lsT_d.ap()[j], w_out=wout_d.ap()[j],
                     am0=am0_d.ap(), amd=amd_d.ap(), amo=amo_d.ap(), hidx=hidx_d.ap())
            outs = tail_phase(P, nc, pb, "pool", T, C, NEXP, hsrc, hdst, W, scr, last)
        P.fence()
    P.emit(final_wait_ops=outs)
    return nc


_PROGS = {}
S_FULL = 8192
T_CORE = 4096
CAP = 384


def _prog(key):
    if key not in _PROGS:
        if key == "attn":
            _PROGS[key] = build_attn(S_FULL)
        elif key == "tail_attn":
            _PROGS[key] = build_tail("attn", T_CORE, CAP)
        else:
            _PROGS[key] = build_tail("pool", T_CORE, CAP)
    return _PROGS[key]


def _c(a):
    return np.ascontiguousarray(a, dtype=np.float32)


def mega_in_maps(inputs, S, C, depth):
    f32 = np.float32
    x = np.asarray(inputs["x"], dtype=f32)
    B = x.shape[0]
    T = S // 2
    NTL = T // 128
    NA = (depth + 1) // 2
    NP = depth // 2
    hc = host_consts(C)
    ac = attn_consts(S)
    wqkv = np.asarray(inputs["attn_w_qkv"], dtype=f32)[:NA]
    common = dict(
        w_o=_c(np.asarray(inputs["attn_w_o"])[:NA]),
        lqk=_c(np.stack([np.asarray(inputs[k])[:NA] for k in ("attn_lq1", "attn_lk1", "attn_lq2", "attn_lk2")], axis=1)),
        linit=np.array([[0.8 - 0.6 * math.exp(-0.3 * (2 * j))] for j in range(NA)], f32),
        subg=_c(np.asarray(inputs["attn_sub_g"])[:NA]),
        ln_g=_c(np.asarray(inputs["ln_g"])[:depth]), ln_b=_c(np.asarray(inputs["ln_b"])[:depth]),
        w_rt=_c(np.concatenate([np.asarray(inputs["moe_w_grp_router"])[:depth], np.asarray(inputs["moe_w_exp_router"])[:depth]], axis=2)),
        b_rt=_c(np.concatenate([np.asarray(inputs["moe_b_grp_router"])[:depth], np.asarray(inputs["moe_b_exp_router"])[:depth]], axis=1)),
        w_gate=_c(np.asarray(inputs["moe_w_gate"])[:depth]), w_up=_c(np.asarray(inputs["moe_w_up"])[:depth]),
        w_down=_c(np.asarray(inputs["moe_w_down"])[:depth]),
        cs=ac["cs"], trimask=ac["trimask"], **hc)
    if NP:
        pc = pool_consts(False)
        common.update(
            w_in=_c(np.asarray(inputs["pool_w_in"])[:NP]), w_grp=_c(np.asarray(inputs["pool_w_grp"])[:NP]),
            lsT=_c(np.asarray(inputs["pool_scale"], dtype=f32)[:NP].reshape(NP, 8, 128).transpose(0, 2, 1)),
            w_out=_c(np.asarray(inputs["pool_w_out"])[:NP]), amd=pc["amd"], amo=pc["amo"])
    p = np.arange(128, dtype=np.int64)[:, None]
    ch_o = min(1024, S)
    in_maps = []
    for c in range(NCORES):
        b, r = c // 2, c % 2
        oidx = np.zeros((128, NTL * 2), np.int32)
        for i in range(NTL):
            for g in range(2):
                t_ = r * T + i * 128 + p[:, 0]
                oidx[:, i * 2 + g] = (t_ // ch_o) * 2 * ch_o + g * ch_o + t_ % ch_o
        hidx = (p + (0 if r == 1 else 256)).astype(np.int32)
        m = dict(
            x_all=_c(x[b]), h0=_c(x[b, r * T:(r + 1) * T]), oidx=oidx, hidx=hidx,
            wq=_c(wqkv[:, :, r * 512:(r + 1) * 512]), wk=_c(wqkv[:, :, 1024 + r * 512:1024 + (r + 1) * 512]),
            wv=_c(wqkv[:, :, 2048 + r * 512:2048 + (r + 1) * 512]), **common)
        if NP:
            m["am0"] = pool_consts(r == 0)["am0"]
        in_maps.append(m)
    return in_maps


_MEGA = {}


def kernel(**inputs):
    x = np.asarray(inputs["x"])
    B, S, _ = x.shape
    if "m" not in _MEGA:
        _MEGA["m"] = build_mega(S, CAP)
    in_maps = mega_in_maps(inputs, S, CAP, DEPTH)
    res = run_bass_kernel_spmd(_MEGA["m"], in_maps, core_ids=list(range(NCORES)))
    T = S // 2
    out = np.empty((B, S, D), np.float32)
    for c in range(NCORES):
        out[c // 2, (c % 2) * T:(c % 2 + 1) * T] = res.results[c]["out"]
    return out


def kernel_unfused(**inputs):
    f32 = np.float32
    x = np.asarray(inputs["x"], dtype=f32)
    B, S, _ = x.shape
    h = x.reshape(B * S, D)
    cores = list(range(NCORES))
    hc = host_consts(CAP)
    ac = attn_consts(S)
    for i in range(DEPTH):
        j = i // 2
        tail_common = dict(
            ln_g=_c(inputs["ln_g"][i]), ln_b=_c(inputs["ln_b"][i]),
            w_rt=_c(np.concatenate([inputs["moe_w_grp_router"][i], inputs["moe_w_exp_router"][i]], axis=1)),
            b_rt=_c(np.concatenate([inputs["moe_b_grp_router"][i], inputs["moe_b_exp_router"][i]], axis=0).reshape(1, 36)),
            w_gate=_c(inputs["moe_w_gate"][i]), w_up=_c(inputs["moe_w_up"][i]), w_down=_c(inputs["moe_w_down"][i]),
            **hc)
        if i % 2 == 0:
            linit = 0.8 - 0.6 * math.exp(-0.3 * i)
            wqkv = np.asarray(inputs["attn_w_qkv"][j], dtype=f32)
            in_maps = []
            for c in cores:
                b, hg = c // 2, c % 2
                in_maps.append(dict(
                    xT=_c(h[b * S:(b + 1) * S].T),
                    wq=_c(wqkv[:, hg * 512:(hg + 1) * 512]),
                    wk=_c(wqkv[:, 1024 + hg * 512:1024 + (hg + 1) * 512]),
                    wv=_c(wqkv[:, 2048 + hg * 512:2048 + (hg + 1) * 512]),
                    lq1=_c(inputs["attn_lq1"][j]).reshape(1, 64), lk1=_c(inputs["attn_lk1"][j]).reshape(1, 64),
                    lq2=_c(inputs["attn_lq2"][j]).reshape(1, 64), lk2=_c(inputs["attn_lk2"][j]).reshape(1, 64),
                    linit=np.array([[linit]], f32), subg=_c(inputs["attn_sub_g"][j]).reshape(1, 128), **ac))
            res = run_bass_kernel_spmd(_prog("attn"), in_maps, core_ids=cores)
            o = np.empty((B * S, D), f32)
            for c in cores:
                b, hg = c // 2, c % 2
                o[b * S:(b + 1) * S, hg * 512:(hg + 1) * 512] = res.results[c]["o"]
            in_maps = []
            for c in cores:
                rows = slice(c * T_CORE, (c + 1) * T_CORE)
                in_maps.append(dict(h=_c(h[rows]), oT=_c(o[rows].T), w_o=_c(inputs["attn_w_o"][j]), **tail_common))
            res = run_bass_kernel_spmd(_prog("tail_attn"), in_maps, core_ids=cores)
        else:
            in_maps = []
            for c in cores:
                rows = slice(c * T_CORE, (c + 1) * T_CORE)
                first = (c % 2 == 0)
                halo = np.zeros((128, D), f32) if first else _c(h[c * T_CORE - 128:c * T_CORE])
                in_maps.append(dict(
                    h=_c(h[rows]), halo=halo, w_in=_c(inputs["pool_w_in"][j]), w_grp=_c(inputs["pool_w_grp"][j]),
                    lsT=_c(np.asarray(inputs["pool_scale"][j], dtype=f32).reshape(8, 128).T),
                    w_out=_c(inputs["pool_w_out"][j]), **pool_consts(first), **tail_common))
            res = run_bass_kernel_spmd(_prog("tail_pool"), in_maps, core_ids=cores)
        h = np.concatenate([res.results[c]["out"] for c in cores], axis=0)
    return h.reshape(B, S, D).astype(f32)
```

```python
import contextlib
import math
import numpy as np
import concourse.bass as bass
import concourse.mybir as mybir
from concourse.bass_utils import run_bass_kernel_spmd

F32 = mybir.dt.float32
BF16 = mybir.dt.bfloat16
I32 = mybir.dt.int32
ALU = mybir.AluOpType
AF = mybir.ActivationFunctionType
AX = mybir.AxisListType

D = 1024
NE = 32
DE = 512
NCORES = 8
DEPTH = 4
ALPHA = (2 * DEPTH) ** 0.25
LN_EPS = 1e-5
SUBLN_EPS = 1e-5
POOL_WINDOWS = (2, 4, 8, 16)
ENGINES = ("pe", "act", "dve", "pool", "sp")
EPOCH = 20000
SB_BASE = 16512
SB_TOP = 229344


class Op:
    __slots__ = ("eng", "fn", "waits", "sig", "is_dma", "semkey", "inc")


class Prog:
    def __init__(self, nc):
        self.nc = nc
        self.ops = []
        self.last_writer = {}
        self.readers = {}
        self.stack = contextlib.ExitStack()
        self.uid = 0
        self.sb_off = SB_BASE

    def sbuf(self, shape, dtype, name=None):
        self.uid += 1
        nbytes = int(np.prod(shape[1:])) * (4 if dtype in (F32, I32) else 2)
        off = (self.sb_off + 63) // 64 * 64
        assert off + nbytes <= SB_TOP, f"SBUF overflow allocating {name} {shape}: {off}+{nbytes}"
        self.sb_off = off + nbytes
        return self.nc.alloc_sbuf_tensor_at(f"s{self.uid}_" + (name or "t"), list(shape), dtype, offset=off)

    def reset_sbuf(self):
        self.sb_off = SB_BASE

    def fence(self):
        last = {}
        dmas = {}
        for o in self.ops:
            if o.fn is None:
                continue
            if o.is_dma:
                dmas[o.semkey] = o
            else:
                last[o.eng] = o
        targets = list(last.values()) + list(dmas.values())
        for t in targets:
            t.sig = True
        for eng in ENGINES:
            op = Op()
            op.eng, op.fn, op.is_dma, op.semkey, op.sig, op.inc = eng, None, False, None, False, 1
            op.waits = [t for t in targets if not (t.eng == eng and not t.is_dma)]
            self.ops.append(op)
        self.last_writer.clear()
        self.readers.clear()

    def psum(self, shape, dtype=F32, name=None):
        self.uid += 1
        return self.stack.enter_context(self.nc.psum_tensor("p_" + (name or f"ps{self.uid}"), list(shape), dtype))

    def _add(self, eng, fn, reads, writes, is_dma=False, semkey=None):
        op = Op()
        op.eng, op.fn, op.is_dma, op.semkey = eng, fn, is_dma, semkey
        op.sig = False
        op.inc = 16 if is_dma else 1
        excl = [k for k in reads if k.startswith("pb")]
        if excl:
            reads = [k for k in reads if not k.startswith("pb")]
            writes = list(writes) + [k for k in excl if k not in writes]
        deps = []
        for k in reads:
            w = self.last_writer.get(k)
            if w is not None:
                deps.append(w)
        for k in writes:
            w = self.last_writer.get(k)
            if w is not None:
                deps.append(w)
            deps.extend(self.readers.get(k, ()))
        seen = set()
        op.waits = []
        for d in deps:
            if id(d) in seen or d is op:
                continue
            seen.add(id(d))
            if d.eng == "pe" and eng == "pe" and not d.is_dma and not is_dma:
                continue
            op.waits.append(d)
            d.sig = True
        for k in reads:
            self.readers.setdefault(k, []).append(op)
        for k in writes:
            self.last_writer[k] = op
            self.readers[k] = []
        self.ops.append(op)
        return op

    def op(self, eng, fn, reads=(), writes=()):
        return self._add(eng, fn, reads, writes)

    def dma(self, eng, fn, reads=(), writes=(), semkey=None):
        o = self._add(eng, fn, reads, writes, is_dma=True, semkey=semkey)
        o.sig = True
        return o

    def coll(self, fn, reads, writes, semkey, inc=1):
        o = self._add("pool", fn, reads, writes, is_dma=True, semkey=semkey)
        o.sig = True
        o.inc = inc
        return o

    def mm(self, out, lhsT, rhs, start, stop, r, w):
        return self.op("pe", lambda e: e.matmul(out, lhsT, rhs, start=start, stop=stop), r, w)

    def tr(self, out, in_, ident, r, w):
        return self.op("pe", lambda e: e.transpose(out, in_, ident), r, w)

    def act(self, out, in_, func, r, w, bias=None, scale=1.0, accum=None):
        def f(e):
            kw = {}
            if bias is not None:
                kw["bias"] = bias
            if accum is not None:
                kw["accum_out"] = accum
            return e.activation(out=out, in_=in_, func=func, scale=scale, **kw)
        return self.op("act", f, r, w)

    def tt(self, eng, out, in0, in1, op, r, w):
        return self.op(eng, lambda e: e.tensor_tensor(out=out, in0=in0, in1=in1, op=op), r, w)

    def ts(self, eng, out, in0, s1, s2, op0, op1, r, w):
        if s2 is None:
            return self.op(eng, lambda e: e.tensor_scalar(out=out, in0=in0, scalar1=s1, scalar2=None, op0=op0), r, w)
        return self.op(eng, lambda e: e.tensor_scalar(out=out, in0=in0, scalar1=s1, scalar2=s2, op0=op0, op1=op1), r, w)

    def stt(self, out, in0, scalar, in1, op0, op1, r, w, accum=None):
        def f(e):
            if accum is not None:
                return e.scalar_tensor_tensor(out=out, in0=in0, scalar=scalar, in1=in1, op0=op0, op1=op1, accum_out=accum)
            return e.scalar_tensor_tensor(out=out, in0=in0, scalar=scalar, in1=in1, op0=op0, op1=op1)
        return self.op("dve", f, r, w)

    def cp(self, eng, out, in_, r, w):
        if eng == "act":
            return self.op("act", lambda e: e.copy(out=out, in_=in_), r, w)
        return self.op(eng, lambda e: e.tensor_copy(out=out, in_=in_), r, w)

    def red(self, out, in_, op, r, w):
        return self.op("dve", lambda e: e.tensor_reduce(out=out, in_=in_, axis=AX.X, op=op), r, w)

    def ld(self, eng, out, in_, w, semkey=None, r=()):
        return self.dma(eng, lambda e: e.dma_start(out=out, in_=in_), r, w, semkey=semkey or w[0])

    def st(self, eng, out, in_, r, semkey, w=()):
        return self.dma(eng, lambda e: e.dma_start(out=out, in_=in_), r, w, semkey=semkey)

    def emit(self, final_wait_ops=()):
        nc = self.nc
        sem_of = {}
        cnt = {}
        semnames = []
        eng_sig_count = {e: 0 for e in ENGINES}
        for o in self.ops:
            if not o.sig:
                continue
            if o.is_dma:
                name = "d_" + str(o.semkey)
                cnt[name] = cnt.get(name, 0) + o.inc
                sem_of[id(o)] = (name, cnt[name])
            else:
                n = eng_sig_count[o.eng]
                name = f"c_{o.eng}_{n // EPOCH}"
                eng_sig_count[o.eng] = n + 1
                sem_of[id(o)] = (name, n % EPOCH + 1)
            if name not in semnames:
                semnames.append(name)
        sems = {}
        for name in semnames:
            sems[name] = self.stack.enter_context(nc.semaphore(name))
        self.nsems = len(semnames)
        per_eng = {e: [] for e in ENGINES}
        for o in self.ops:
            per_eng[o.eng].append(o)
        finals = list(final_wait_ops)

        def run(engname, eng):
            waited = {}
            for o in per_eng[engname]:
                for d in o.waits:
                    name, val = sem_of[id(d)]
                    if waited.get(name, 0) >= val:
                        continue
                    waited[name] = val
                    eng.wait_ge(sems[name], val)
                if o.fn is None:
                    continue
                ins = o.fn(eng)
                if o.sig:
                    name, val = sem_of[id(o)]
                    ins.then_inc(sems[name], o.inc)
            if engname == "sp":
                for d in finals:
                    name, val = sem_of[id(d)]
                    if waited.get(name, 0) >= val:
                        continue
                    waited[name] = val
                    eng.wait_ge(sems[name], val)

        with nc.Block() as block:
            @block.tensor
            def _(e):
                run("pe", e)

            @block.scalar
            def _(e):
                run("act", e)

            @block.vector
            def _(e):
                run("dve", e)

            @block.gpsimd
            def _(e):
                run("pool", e)

            @block.sync
            def _(e):
                run("sp", e)
        self.stack.close()


def build_tail(kind, T, C, NEXP=NE):
    nc = bass.Bass("TRN2", target_bir_lowering=False)
    P = Prog(nc)
    NTL = T // 128
    NSB = C // 128
    inp = lambda name, shape: nc.dram_tensor(name, list(shape), F32, kind="ExternalInput")
    h_d = inp("h", [T, D])
    if kind == "attn":
        oT_d = inp("oT", [D, T])
        wo_d = inp("w_o", [D, D])
    else:
        halo_d = inp("halo", [128, D])
        am0_d = inp("am0", [4, 128, 128])
        amd_d = inp("amd", [4, 128, 128])
        amo_d = inp("amo", [4, 128, 128])
        win_d = inp("w_in", [D, D])
        wgrp_d = inp("w_grp", [4, 256, 256])
        lsT_d = inp("lsT", [128, 8])
        wout_d = inp("w_out", [D, D])
    lng_d = inp("ln_g", [2, D])
    lnb_d = inp("ln_b", [2, D])
    wrt_d = inp("w_rt", [D, 36])
    brt_d = inp("b_rt", [1, 36])
    wg_d = inp("w_gate", [NEXP, D, DE])
    wu_d = inp("w_up", [NEXP, D, DE])
    wd_d = inp("w_down", [NEXP, DE, D])
    ident_d = inp("ident", [128, 128])
    ustr_d = inp("ustrict", [128, 128])
    ecoff_d = inp("ecoff", [1, 32])
    out_d = nc.dram_tensor("out", [T, D], F32, kind="ExternalOutput")
    h1_d = nc.dram_tensor("h1_scr", [T, D], F32)
    xin_d = nc.dram_tensor("xin_scr", [NE * C, D], BF16)
    y_d = nc.dram_tensor("y_scr", [NE * C, D], F32)

    ident = P.sbuf([128, 128], F32, "ident")
    identb = P.sbuf([128, 128], BF16, "identb")
    ustr = P.sbuf([128, 128], BF16, "ustr")
    ones = P.sbuf([128, 128], BF16, "ones")
    ecoff = P.sbuf([128, 32], F32, "ecoff")
    brt = P.sbuf([128, 36], F32, "brt")
    wrt = P.sbuf([128, 8, 36], F32, "wrt")
    lng = P.sbuf([128, 2, D], F32, "lng")
    lnb = P.sbuf([128, 2, D], F32, "lnb")
    epst = P.sbuf([128, 1], F32, "epst")
    Scnt = P.sbuf([128, 32], BF16, "Scnt")
    gates = P.sbuf([128, NTL, 2], F32, "gates")
    dest = P.sbuf([128, NTL, 2], I32, "dest")

    P.ld("sp", ident[:], ident_d.ap(), ["ident"])
    P.ld("pool", identb[:], ident_d.ap(), ["identb"])
    P.ld("pool", ustr[:], ustr_d.ap(), ["ustr"])
    P.op("pool", lambda e: e.memset(ones[:], 1.0), (), ["ones"])
    P.op("pool", lambda e: e.memset(epst[:], LN_EPS), (), ["epst"])
    P.op("pool", lambda e: e.memset(Scnt[:], 0.0), (), ["Scnt"])
    P.ld("sp", ecoff[:], ecoff_d.ap().partition_broadcast(128), ["ecoff"])
    P.ld("sp", brt[:], brt_d.ap().partition_broadcast(128), ["brt"])
    P.ld("sp", wrt[:], wrt_d.ap().rearrange("(k p) n -> p k n", p=128), ["wrt"])
    for j in range(2):
        P.ld("sp", lng[:, j, :], lng_d.ap()[j:j + 1, :].partition_broadcast(128), [f"lng{j}"])
        P.ld("sp", lnb[:, j, :], lnb_d.ap()[j:j + 1, :].partition_broadcast(128), [f"lnb{j}"])

    zt = P.sbuf([128, D], BF16, "zt")
    P.op("pool", lambda e: e.memset(zt[:], 0.0), (), ["zt"])
    nz = (NE * C) // 128
    for z in range(nz):
        P.dma("sp", (lambda z: lambda e: e.dma_start(out=xin_d.ap()[z * 128:(z + 1) * 128, :], in_=zt[:]))(z), ["zt"], ["xinz"] if z == nz - 1 else [f"xinz{z}"], semkey="xinz")
    pb = [P.psum([128, 512], F32, f"pb{i}") for i in range(6)]
    pbb = [P.psum([128, 1024], BF16, f"pbb{i}") for i in range(2)]

    if kind == "attn":
        wo = P.sbuf([128, 8, D], BF16, "wo")
        P.ld("pool", wo[:], wo_d.ap().rearrange("(k p) n -> p k n", p=128), ["wo"])
        OCH = min(512, T)
        oT = [P.sbuf([128, 8, OCH], BF16, f"oT{i}") for i in range(2)]
    else:
        win = P.sbuf([128, 8, D], BF16, "win")
        wout = P.sbuf([128, 8, D], BF16, "wout")
        wgrp = P.sbuf([128, 4, 2, 256], BF16, "wgrp")
        lsT = P.sbuf([128, 8], F32, "lsT")
        am0 = P.sbuf([128, 4, 128], BF16, "am0")
        amd = P.sbuf([128, 4, 128], BF16, "amd")
        amo = P.sbuf([128, 4, 128], BF16, "amo")
        P.ld("pool", win[:], win_d.ap().rearrange("(k p) n -> p k n", p=128), ["win"])
        P.ld("pool", wout[:], wout_d.ap().rearrange("(k p) n -> p k n", p=128), ["wout"])
        P.ld("pool", wgrp[:], wgrp_d.ap().rearrange("g (k p) n -> p g k n", p=128), ["wgrp"])
        P.ld("sp", lsT[:], lsT_d.ap(), ["lsT"])
        P.ld("pool", am0[:], am0_d.ap().rearrange("w p n -> p w n"), ["am0"])
        P.ld("pool", amd[:], amd_d.ap().rearrange("w p n -> p w n"), ["amd"])
        P.ld("pool", amo[:], amo_d.ap().rearrange("w p n -> p w n"), ["amo"])
        ubuf = [P.sbuf([128, D], BF16, f"u{i}") for i in range(3)]
        hTb = P.sbuf([128, 8, 128], BF16, "hTb")
        pT = P.sbuf([128, 8, 128], BF16, "pT")
        qT = P.sbuf([128, 8, 128], BF16, "qT")

    htile = [P.sbuf([128, D], F32, f"ht{i}") for i in range(2)]
    rt = [P.sbuf([128, D], F32, f"rt{i}") for i in range(2)]
    h1b = [P.sbuf([128, D], BF16, f"h1b{i}") for i in range(2)]
    h1T = P.sbuf([128, 8, 128], F32, "h1T")
    stats = P.sbuf([128, 2, 6], F32, "stats")
    mv = P.sbuf([128, 2], F32, "mv")
    rstd = P.sbuf([128, 1], F32, "rstd")
    nmr = P.sbuf([128, 1], F32, "nmr")
    L = P.sbuf([128, 36], F32, "L")
    gmax = P.sbuf([128, 1], F32, "gmax")
    ngmax = P.sbuf([128, 1], F32, "ngmax")
    G1 = P.sbuf([128, 4], F32, "G1")
    ge = P.sbuf([128, 4], F32, "ge")
    gsum = P.sbuf([128, 1], F32, "gsum")
    pen = P.sbuf([128, 4], F32, "pen")
    Lm = P.sbuf([128, 32], F32, "Lm")
    Lm2 = P.sbuf([128, 32], F32, "Lm2")
    m1 = P.sbuf([128, 1], F32, "m1")
    m2 = P.sbuf([128, 1], F32, "m2")
    OH1 = P.sbuf([128, 32], F32, "OH1")
    OH2 = P.sbuf([128, 32], F32, "OH2")
    OHc = P.sbuf([128, 32], BF16, "OHc")
    dd = P.sbuf([128, 1], F32, "dd")
    ex = P.sbuf([128, 1], F32, "ex")
    den = P.sbuf([128, 1], F32, "den")
    Pf = P.sbuf([128, 32], F32, "Pf")
    tmp32 = P.sbuf([128, 32], F32, "tmp32")
    dflt = P.sbuf([128, 2], F32, "dflt")

    def layer_norm(src, j, dst, keys_r, key_w):
        sv = src.rearrange("p (c f) -> p c f", c=2)
        for c in range(2):
            P.op("dve", (lambda c: lambda e: e.bn_stats(out=stats[:, c, :], in_=sv[:, c, :]))(c), keys_r, [f"stats{c}"])
        P.op("dve", lambda e: e.bn_aggr(out=mv[:], in_=stats[:]), ["stats0", "stats1"], ["mv"])
        P.act(rstd[:], mv[:, 1:2], AF.Sqrt, ["mv", "epst"], ["rstd"], bias=epst[:, 0:1])
        P.op("dve", lambda e: e.reciprocal(out=rstd[:], in_=rstd[:]), ["rstd"], ["rstd"])
        P.stt(nmr[:], mv[:, 0:1], -1.0, rstd[:], ALU.mult, ALU.mult, ["mv", "rstd"], ["nmr"])
        P.act(dst, src, AF.Identity, list(keys_r) + ["rstd", "nmr"], [key_w], bias=nmr[:, 0:1], scale=rstd[:, 0:1])
        P.tt("dve", dst, dst, lng[:, j, :], ALU.mult, [key_w, f"lng{j}"], [key_w])
        P.tt("pool", dst, dst, lnb[:, j, :], ALU.add, [key_w, f"lnb{j}"], [key_w])

    def pool_u(i, slot, hslot):
        hs = htile[hslot]
        hk = f"ht{hslot}"
        if i < 0:
            P.ld("sp", hs[:], halo_d.ap(), [hk])
        for k in range(8):
            P.tr(pb[2 + k // 4][:, (k % 4) * 128:(k % 4 + 1) * 128], hs[:, k * 128:(k + 1) * 128], ident[:], [hk, "ident"], [f"pb{2 + k // 4}"])
        for hf in range(2):
            P.cp("act" if hf == 0 else "dve", hTb[:, hf * 4:(hf + 1) * 4, :], pb[2 + hf][:].rearrange("p (k t) -> p k t", k=4), [f"pb{2 + hf}"], [f"hTb{hf}"])
        for hf in range(2):
            for k in range(8):
                P.mm(pb[hf][:], hTb[:, k, :], win[:, k, hf * 512:(hf + 1) * 512], k == 0, k == 7, [f"hTb{k // 4}", "win"], [f"pb{hf}"])
        for hf in range(2):
            P.cp("act" if hf == 0 else "dve", ubuf[slot][:, hf * 512:(hf + 1) * 512], pb[hf][:], [f"pb{hf}"], [f"u{slot}_{hf}"])

    if kind == "pool":
        pool_u(-1, 2, 1)

    for i in range(NTL):
        s = i % 2
        hk = f"ht{s}"
        P.ld("sp", htile[s][:], h_d.ap()[i * 128:(i + 1) * 128, :], [hk])
        if kind == "attn":
            ch = (i * 128) // OCH
            if (i * 128) % OCH == 0:
                P.ld("pool", oT[ch % 2][:], oT_d.ap()[:, ch * OCH:(ch + 1) * OCH].rearrange("(k p) t -> p k t", p=128), [f"oT{ch % 2}"])
            off = i * 128 - ch * OCH
            for hf in range(2):
                for k in range(8):
                    P.mm(pb[hf][:], oT[ch % 2][:, k, off:off + 128], wo[:, k, hf * 512:(hf + 1) * 512], k == 0, k == 7, [f"oT{ch % 2}", "wo"], [f"pb{hf}"])
        else:
            us = i % 3
            up = (i - 1) % 3
            pool_u(i, us, s)
            A = am0 if i == 0 else amd
            Ak = "am0" if i == 0 else "amd"
            for j in range(8):
                g = j // 2
                o_ = pb[4 + j // 4][:, (j % 4) * 128:(j % 4 + 1) * 128]
                P.mm(o_, ubuf[us][:, j * 128:(j + 1) * 128], A[:, g, :], True, False, [f"u{us}_{j // 4}", Ak], [f"pb{4 + j // 4}"])
                P.mm(o_, ubuf[up][:, j * 128:(j + 1) * 128], amo[:, g, :], False, True, [f"u{up}_{j // 4}", "amo"], [f"pb{4 + j // 4}"])
            for hf in range(2):
                P.cp("act" if hf == 0 else "dve", pT[:, hf * 4:(hf + 1) * 4, :], pb[4 + hf][:].rearrange("p (k t) -> p k t", k=4), [f"pb{4 + hf}"], [f"pT{hf}"])
            for j in range(8):
                g = j // 2
                o_ = pb[4 + j // 4][:, (j % 4) * 128:(j % 4 + 1) * 128]
                for kk in range(2):
                    P.mm(o_, wgrp[:, g, kk, (j % 2) * 128:(j % 2 + 1) * 128], pT[:, 2 * g + kk, :], kk == 0, kk == 1, [f"pT{(2 * g + kk) // 4}", "wgrp"], [f"pb{4 + j // 4}"])
            for j in range(8):
                P.act(qT[:, j, :], pb[4 + j // 4][:, (j % 4) * 128:(j % 4 + 1) * 128], AF.Identity, [f"pb{4 + j // 4}", "lsT"], [f"qT{j}"], scale=lsT[:, j:j + 1])
            for hf in range(2):
                for k in range(8):
                    P.mm(pb[hf][:], qT[:, k, :], wout[:, k, hf * 512:(hf + 1) * 512], k == 0, k == 7, [f"qT{k}", "wout"], [f"pb{hf}"])
        r = rt[s]
        rk = f"rt{s}"
        for hf in range(2):
            P.stt(r[:, hf * 512:(hf + 1) * 512], htile[s][:, hf * 512:(hf + 1) * 512], ALPHA, pb[hf][:], ALU.mult, ALU.add, [hk, f"pb{hf}"], [rk])
        layer_norm(r[:], 0, r[:], [rk], rk)
        P.st("sp", h1_d.ap()[i * 128:(i + 1) * 128, :], r[:], [rk], f"h1st{s}", [f"h1d{i}"])
        P.cp("act", h1b[s][:], r[:], [rk], [f"h1b{s}"])
        for k in range(8):
            P.tr(pb[2 + k // 4][:, (k % 4) * 128:(k % 4 + 1) * 128], r[:, k * 128:(k + 1) * 128], ident[:], [rk, "ident"], [f"pb{2 + k // 4}"])
        for hf in range(2):
            P.cp("act" if hf == 0 else "dve", h1T[:, hf * 4:(hf + 1) * 4, :], pb[2 + hf][:].rearrange("p (k t) -> p k t", k=4), [f"pb{2 + hf}"], [f"h1T{hf}"])
        for k in range(8):
            P.mm(pb[4][:, 0:36], h1T[:, k, :], wrt[:, k, :], k == 0, k == 7, [f"h1T{k // 4}", "wrt"], ["pb4"])
        P.tt("dve", L[:], pb[4][:, 0:36], brt[:], ALU.add, ["pb4", "brt"], ["L"])
        P.red(gmax[:], L[:, 0:4], ALU.max, ["L"], ["gmax"])
        P.tt("dve", G1[:], L[:, 0:4], gmax[:, 0:1].to_broadcast([128, 4]), ALU.is_equal, ["L", "gmax"], ["G1"])
        P.ts("dve", ngmax[:], gmax[:], -1.0, None, ALU.mult, None, ["gmax"], ["ngmax"])
        P.act(ge[:], L[:, 0:4], AF.Exp, ["L", "ngmax"], ["ge", "gsum"], bias=ngmax[:, 0:1], accum=gsum[:, 0:1])
        P.ts("dve", pen[:], G1[:], -1.0, 1e30, ALU.add, ALU.mult, ["G1"], ["pen"])
        P.tt("dve", Lm[:].rearrange("p (g e) -> p g e", g=4), L[:, 4:36].rearrange("p (g e) -> p g e", g=4),
             pen[:].unsqueeze(2).to_broadcast([128, 4, 8]), ALU.add, ["L", "pen"], ["Lm"])
        P.red(m1[:], Lm[:], ALU.max, ["Lm"], ["m1"])
        P.tt("dve", OH1[:], Lm[:], m1[:, 0:1].to_broadcast([128, 32]), ALU.is_equal, ["Lm", "m1"], ["OH1"])
        P.stt(Lm2[:], OH1[:], -1e30, Lm[:], ALU.mult, ALU.add, ["OH1", "Lm"], ["Lm2"])
        P.red(m2[:], Lm2[:], ALU.max, ["Lm2"], ["m2"])
        P.tt("dve", OH2[:], Lm2[:], m2[:, 0:1].to_broadcast([128, 32]), ALU.is_equal, ["Lm2", "m2"], ["OH2"])
        P.tt("dve", dd[:], m2[:], m1[:], ALU.subtract, ["m1", "m2"], ["dd"])
        P.act(ex[:], dd[:], AF.Exp, ["dd"], ["ex"])
        P.stt(den[:], ex[:], 1.0, gsum[:], ALU.add, ALU.mult, ["ex", "gsum"], ["den"])
        P.op("dve", (lambda i: lambda e: e.reciprocal(out=gates[:, i, 0:1], in_=den[:]))(i), ["den"], [f"gate{i}"])
        P.tt("dve", gates[:, i, 1:2], gates[:, i, 0:1], ex[:], ALU.mult, [f"gate{i}", "ex"], [f"gate{i}"])
        P.tt("dve", OHc[:], OH1[:], OH2[:], ALU.add, ["OH1", "OH2"], ["OHc"])
        P.mm(pb[5][:, 0:32], ustr[:], OHc[:], True, False, ["ustr", "OHc"], ["pb5"])
        P.mm(pb[5][:, 0:32], ones[:], Scnt[:], False, True, ["ones", "Scnt"], ["pb5"])
        P.tt("dve", Pf[:], pb[5][:, 0:32], ecoff[:], ALU.add, ["pb5", "ecoff"], ["Pf"])
        P.tt("dve", Scnt[:], Scnt[:], OHc[:], ALU.add, ["Scnt", "OHc"], ["Scnt"])
        P.stt(tmp32[:], OH1[:], 1.0, Pf[:], ALU.mult, ALU.mult, ["OH1", "Pf"], ["tmp32", "dflt0"], accum=dflt[:, 0:1])
        P.stt(tmp32[:], OH2[:], 1.0, Pf[:], ALU.mult, ALU.mult, ["OH2", "Pf"], ["tmp32", "dflt1"], accum=dflt[:, 1:2])
        P.cp("dve", dest[:, i, :], dflt[:], ["dflt0", "dflt1"], [f"dest{i}"])
        for kk in range(2):
            P.dma("pool", (lambda i, kk, s: lambda e: e.indirect_dma_start(
                out=xin_d.ap(), out_offset=bass.IndirectOffsetOnAxis(ap=dest[:, i, kk:kk + 1], axis=0),
                in_=h1b[s][:], in_offset=None))(i, kk, s), [f"h1b{s}", f"dest{i}", "xinz"], [f"xinw{i}_{kk}"], semkey=f"scat{s}{kk}")

    wgs = [P.sbuf([128, 8, DE], BF16, f"wg{i}") for i in range(2)]
    wus = [P.sbuf([128, 8, DE], BF16, f"wu{i}") for i in range(2)]
    wds = [P.sbuf([128, 4, D], BF16, f"wd{i}") for i in range(2)]
    xin = [P.sbuf([128, NSB, D], BF16, f"xin{i}") for i in range(2)]
    xT = [P.sbuf([128, 8, C], BF16, f"xT{i}") for i in range(2)]
    actT = [P.sbuf([128, 4, C], BF16, f"actT{i}") for i in range(2)]
    sg = [P.sbuf([128, C], F32, f"sg{i}") for i in range(2)]
    yb = [P.sbuf([128, D], F32, f"yb{i}") for i in range(2)]
    ycount = 0
    xin_keys = [f"xinw{i}_{kk}" for i in range(NTL) for kk in range(2)]
    y_keys = [f"y_{e_}_{sb}" for e_ in range(NEXP) for sb in range(NSB)]
    for e_ in range(NEXP):
        s = e_ % 2
        P.ld("pool", wgs[s][:], wg_d.ap()[e_].rearrange("(k p) n -> p k n", p=128), [f"wg{s}"])
        P.ld("pool", wus[s][:], wu_d.ap()[e_].rearrange("(k p) n -> p k n", p=128), [f"wu{s}"])
        P.ld("pool", wds[s][:], wd_d.ap()[e_].rearrange("(k p) n -> p k n", p=128), [f"wd{s}"])
        P.ld("sp", xin[s][:], xin_d.ap()[e_ * C:(e_ + 1) * C, :].rearrange("(b p) n -> p b n", p=128), [f"xin{s}"], r=xin_keys)
        for sb in range(NSB):
            pbt = pbb[sb % 2]
            for k in range(8):
                P.tr(pbt[:, k * 128:(k + 1) * 128], xin[s][:, sb, k * 128:(k + 1) * 128], identb[:], [f"xin{s}", "identb"], [f"pbb{sb % 2}"])
            P.cp("dve" if sb % 2 else "act", xT[s][:, :, sb * 128:(sb + 1) * 128], pbt[:].rearrange("p (k t) -> p k t", k=8), [f"pbb{sb % 2}"], [f"xT{s}_{sb}"])
        xkeys = [f"xT{s}_{sb}" for sb in range(NSB)]
        for fc in range(4):
            pg = pb[(fc % 2) * 2]
            pu = pb[(fc % 2) * 2 + 1]
            kg, ku = f"pb{(fc % 2) * 2}", f"pb{(fc % 2) * 2 + 1}"
            for k in range(8):
                P.mm(pg[:, 0:C], wgs[s][:, k, fc * 128:(fc + 1) * 128], xT[s][:, k, :], k == 0, k == 7, xkeys + [f"wg{s}"], [kg])
            for k in range(8):
                P.mm(pu[:, 0:C], wus[s][:, k, fc * 128:(fc + 1) * 128], xT[s][:, k, :], k == 0, k == 7, xkeys + [f"wu{s}"], [ku])
            P.act(sg[fc % 2][:], pg[:, 0:C], AF.Silu, [kg], [f"sg{fc % 2}"])
            P.tt("dve", actT[s][:, fc, :], sg[fc % 2][:], pu[:, 0:C], ALU.mult, [f"sg{fc % 2}", ku], [f"actT{s}_{fc}"])
        akeys = [f"actT{s}_{fc}" for fc in range(4)]
        for sb in range(NSB):
            ys = ycount % 2
            ycount += 1
            for hf in range(2):
                py = pb[4 + hf]
                for fc in range(4):
                    P.mm(py[:], actT[s][:, fc, sb * 128:(sb + 1) * 128], wds[s][:, fc, hf * 512:(hf + 1) * 512], fc == 0, fc == 3, akeys + [f"wd{s}"], [f"pb{4 + hf}"])
                P.cp("act" if hf == 0 else "dve", yb[ys][:, hf * 512:(hf + 1) * 512], py[:], [f"pb{4 + hf}"], [f"yb{ys}"])
            r0 = e_ * C + sb * 128
            P.st("sp", y_d.ap()[r0:r0 + 128, :], yb[ys][:], [f"yb{ys}"], f"yst{ys}", [f"y_{e_}_{sb}"])

    y0 = [P.sbuf([128, D], F32, f"y0_{i}") for i in range(2)]
    y1 = [P.sbuf([128, D], F32, f"y1_{i}") for i in range(2)]
    outs = []
    for i in range(NTL):
        s = i % 2
        hk = f"ht{s}"
        P.ld("sp", htile[s][:], h1_d.ap()[i * 128:(i + 1) * 128, :], [hk], r=[f"h1d{i}"])
        for kk, yt in ((0, y0), (1, y1)):
            P.dma("pool", (lambda i, kk, yt, s: lambda e: e.indirect_dma_start(
                out=yt[s][:], out_offset=None, in_=y_d.ap(),
                in_offset=bass.IndirectOffsetOnAxis(ap=dest[:, i, kk:kk + 1], axis=0)))(i, kk, yt, s),
                y_keys + [f"dest{i}"], [f"y{kk}_{s}"], semkey=f"gath{kk}{s}")
        r = rt[s]
        rk = f"rt{s}"
        P.ts("pool", r[:], htile[s][:], ALPHA, None, ALU.mult, None, [hk], [rk])
        P.stt(r[:], y0[s][:], gates[:, i, 0:1], r[:], ALU.mult, ALU.add, [f"y0_{s}", f"gate{i}", rk], [rk])
        P.stt(r[:], y1[s][:], gates[:, i, 1:2], r[:], ALU.mult, ALU.add, [f"y1_{s}", f"gate{i}", rk], [rk])
        layer_norm(r[:], 1, r[:], [rk], rk)
        outs.append(P.st("sp", out_d.ap()[i * 128:(i + 1) * 128, :], r[:], [rk], f"ost{s}"))
    P.emit(final_wait_ops=outs)
    return nc


def host_consts(C):
    ident = np.eye(128, dtype=np.float32)
    ustrict = (np.arange(128)[:, None] < np.arange(128)[None, :]).astype(np.float32)
    ecoff = (np.arange(32, dtype=np.float32) * C).reshape(1, 32)
    return dict(ident=ident, ustrict=ustrict, ecoff=ecoff)


def pool_consts(first):
    am0 = np.zeros((4, 128, 128), np.float32)
    amd = np.zeros((4, 128, 128), np.float32)
    amo = np.zeros((4, 128, 128), np.float32)
    sp = np.arange(128)[:, None]
    s = np.arange(128)[None, :]
    for g, w in enumerate(POOL_WINDOWS):
        band = ((sp <= s) & (sp > s - w)).astype(np.float32)
        amd[g] = band / w - np.eye(128, dtype=np.float32)
        cnt = np.minimum(s + 1, w).astype(np.float32)
        am0[g] = band / cnt - np.eye(128, dtype=np.float32)
        amo[g] = ((sp - 128 > s - w)).astype(np.float32) / w
    return dict(am0=am0 if first else amd.copy(), amd=amd, amo=amo)


def build_attn(S, dbg=0):
    nc = bass.Bass("TRN2", target_bir_lowering=False)
    P = Prog(nc)
    NT = S // 128
    NCH = S // 512
    inp = lambda name, shape: nc.dram_tensor(name, list(shape), F32, kind="ExternalInput")
    xT_d = inp("xT", [D, S])
    wq_d = inp("wq", [D, 512])
    wk_d = inp("wk", [D, 512])
    wv_d = inp("wv", [D, 512])
    cs_d = inp("cs", [128, (S // 128) * 16])
    lq1_d = inp("lq1", [1, 64])
    lk1_d = inp("lk1", [1, 64])
    lq2_d = inp("lq2", [1, 64])
    lk2_d = inp("lk2", [1, 64])
    linit_d = inp("linit", [1, 1])
    subg_d = inp("subg", [1, 128])
    ident_d = inp("ident", [128, 128])
    tri_d = inp("trimask", [128, 128])
    o_d = nc.dram_tensor("o", [S, 512], F32, kind="ExternalOutput")

    identb = P.sbuf([128, 128], BF16, "identb")
    trib = P.sbuf([128, 128], BF16, "trib")
    cs = P.sbuf([128, NT, 16], F32, "cs")
    lqk = P.sbuf([128, 4, 64], F32, "lqk")
    linit = P.sbuf([128, 1], F32, "linit")
    subg = P.sbuf([128, 128], F32, "subg")
    subgs = P.sbuf([128, 128], F32, "subgs")
    junk64 = P.sbuf([128, 64], F32, "junk64")
    ssum = P.sbuf([128, 2], F32, "ssum")
    esum = P.sbuf([128, 2], F32, "esum")
    nlam = P.sbuf([128, 1], F32, "nlam")
    om = P.sbuf([128, 1], F32, "om")
    epst = P.sbuf([128, 1], F32, "epst")
    P.ld("pool", identb[:], ident_d.ap(), ["identb"])
    P.ld("pool", trib[:], tri_d.ap(), ["trib"])
    P.ld("sp", cs[:].rearrange("p t c -> p (t c)"), cs_d.ap(), ["cs"])
    for n, dd_ in enumerate((lq1_d, lk1_d, lq2_d, lk2_d)):
        P.ld("sp", lqk[:, n, :], dd_.ap().partition_broadcast(128), [f"lqk{n}"])
    P.ld("sp", linit[:], linit_d.ap().partition_broadcast(128), ["linit"])
    P.ld("sp", subg[:], subg_d.ap().partition_broadcast(128), ["subg"])
    P.op("pool", lambda e: e.memset(epst[:], SUBLN_EPS), (), ["epst"])
    P.stt(junk64[:], lqk[:, 0, :], 1.0, lqk[:, 1, :], ALU.mult, ALU.mult, ["lqk0", "lqk1"], ["junk64", "ssum0"], accum=ssum[:, 0:1])
    P.stt(junk64[:], lqk[:, 2, :], 1.0, lqk[:, 3, :], ALU.mult, ALU.mult, ["lqk2", "lqk3"], ["junk64", "ssum1"], accum=ssum[:, 1:2])
    P.act(esum[:], ssum[:], AF.Exp, ["ssum0", "ssum1"], ["esum"])
    P.tt("dve", nlam[:], esum[:, 1:2], esum[:, 0:1], ALU.subtract, ["esum"], ["nlam"])
    P.tt("dve", nlam[:], nlam[:], linit[:], ALU.subtract, ["nlam", "linit"], ["nlam"])
    P.ts("dve", om[:], linit[:], -1.0, 1.0, ALU.mult, ALU.add, ["linit"], ["om"])
    P.ts("dve", subgs[:], subg[:], om[:, 0:1], None, ALU.mult, None, ["subg", "om"], ["subgs"])

    pb = [P.psum([128, 512], F32, f"pb{i}") for i in range(7)]
    pbb = P.psum([128, 1024], BF16, "pbb")

    QKT = P.sbuf([128, 4, S], BF16, "QKT")
    Vaug = P.sbuf([128, NT, 2, 129], BF16, "Vaug")
    wq = P.sbuf([128, 8, 256], BF16, "wq")
    wk = P.sbuf([128, 8, 256], BF16, "wk")
    wv = P.sbuf([128, 8, 256], BF16, "wv")
    xTb = [P.sbuf([128, 8, 512], BF16, f"xT{i}") for i in range(2)]
    qksb = [P.sbuf([128, 512], BF16, f"qksb{i}") for i in range(2)]
    tA = P.sbuf([128, 8, 8], F32, "tA")
    tB = P.sbuf([128, 8, 8], F32, "tB")
    ET = [[P.sbuf([128, 512], BF16, f"ET{m}_{i}") for i in range(3)] for m in range(2)]
    ocp = [P.sbuf([128, 3, 512], F32, f"ocp{i}") for i in range(2)]
    rl = P.sbuf([128, 2], F32, "rl")
    t0 = P.sbuf([128, 128], F32, "t0")
    av = P.sbuf([128, 128], F32, "av")
    junk = P.sbuf([128, 128], F32, "junk")
    ss = P.sbuf([128, 1], F32, "ss")
    rstd = P.sbuf([128, 1], F32, "rstd")
    ot = [P.sbuf([128, 128], F32, f"ot{i}") for i in range(2)]
    P.op("pool", lambda e: e.memset(Vaug[:], 1.0), (), ["Vaug_init"])
    outs = []
    ocount = [0]

    for hp in range(2):
        if dbg == 4:
            break
        for w_, wd_, nm in ((wq, wq_d, "wq"), (wk, wk_d, "wk"), (wv, wv_d, "wv")):
            P.ld("pool", w_[:], wd_.ap()[:, hp * 256:(hp + 1) * 256].rearrange("(k p) n -> p k n", p=128), [nm])
        for tt in range(NT):
            c = tt // 4
            if tt % 4 == 0:
                P.ld("pool", xTb[c % 2][:], xT_d.ap()[:, c * 512:(c + 1) * 512].rearrange("(k p) t -> p k t", p=128), [f"xT{c % 2}"])
            xs = xTb[c % 2]
            xk = f"xT{c % 2}"
            off = (tt % 4) * 128
            s = tt % 2
            pqk = pb[s]
            pv = pb[2 + s]
            for k in range(8):
                P.mm(pqk[:, 0:256], xs[:, k, off:off + 128], wq[:, k, :], k == 0, False, [xk, "wq"], [f"pb{s}"])
            for k in range(8):
                P.mm(pqk[:, 256:512], xs[:, k, off:off + 128], wk[:, k, :], False, k == 7, [xk, "wk"], [f"pb{s}"])
            for k in range(8):
                P.mm(pv[:, 0:256], xs[:, k, off:off + 128], wv[:, k, :], k == 0, k == 7, [xk, "wv"], [f"pb{2 + s}"])
            if dbg == 5:
                continue
            qv = pqk[:].rearrange("p (g d) -> p g d", g=8)
            qs_ = qksb[s][:].rearrange("p (g d) -> p g d", g=8)
            P.cp("act", qs_[:, :, 16:64], qv[:, :, 16:64], [f"pb{s}"], [f"qkrest{s}"])
            if dbg == 7:
                continue
            cosb = cs[:, tt, 0:8].unsqueeze(1).to_broadcast([128, 8, 8])
            sinb = cs[:, tt, 8:16].unsqueeze(1).to_broadcast([128, 8, 8])
            P.tt("dve", tA[:], qv[:, :, 0:8], cosb, ALU.mult, [f"pb{s}", "cs"], ["tA"])
            P.tt("dve", tB[:], qv[:, :, 8:16], sinb, ALU.mult, [f"pb{s}", "cs"], ["tB"])
            if dbg == 8:
                continue
            P.tt("dve", qs_[:, :, 0:8], tA[:], tB[:], ALU.subtract, ["tA", "tB"], [f"qkrot{s}a"])
            P.tt("dve", tA[:], qv[:, :, 0:8], sinb, ALU.mult, [f"pb{s}", "cs"], ["tA"])
            P.tt("dve", tB[:], qv[:, :, 8:16], cosb, ALU.mult, [f"pb{s}", "cs"], ["tB"])
            P.tt("dve", qs_[:, :, 8:16], tA[:], tB[:], ALU.add, ["tA", "tB"], [f"qkrot{s}b"])
            if dbg == 6:
                continue
            for blk in range(4):
                P.tr(pbb[:, s * 512 + blk * 128:s * 512 + (blk + 1) * 128], qksb[s][:, blk * 128:(blk + 1) * 128], identb[:],
                     [f"qkrest{s}", f"qkrot{s}a", f"qkrot{s}b", "identb"], [f"pbb{s}"])
            P.cp("dve" if s else "act", QKT[:, :, tt * 128:(tt + 1) * 128], pbb[:, s * 512:(s + 1) * 512].rearrange("p (b t) -> p b t", b=4),
                 [f"pbb{s}"], [f"QKT{tt}"])
            P.cp("act" if s else "dve", Vaug[:, tt, :, 0:128], pv[:, 0:256].rearrange("p (h d) -> p h d", h=2),
                 [f"pb{2 + s}", "Vaug_init"], [f"V{tt}"])

        if dbg == 1:
            break
        steps = [(hh, j, kt) for hh in range(2) for j in range(NCH) for kt in range(4 * j + 4)]

        def emit_qk(n):
            hh, j, kt = steps[n]
            d = kt - 4 * j
            qs = 128 * max(d, 0)
            for m in range(2):
                bank = (n % 2) * 2 + m
                pS = pb[bank]
                rd = [f"QKT{t}" for t in range(4 * j + qs // 128, 4 * j + 4)] + [f"QKT{kt}"]
                P.mm(pS[:, qs:512], QKT[m * 64:(m + 1) * 64, 2 + hh, kt * 128:(kt + 1) * 128],
                     QKT[m * 64:(m + 1) * 64, hh, j * 512 + qs:(j + 1) * 512], True, d < 0, rd, [f"pb{bank}"])
                if d >= 0:
                    P.mm(pS[:, qs:qs + 128], identb[:], trib[:], False, True, ["identb", "trib"], [f"pb{bank}"])

        def emit_exp(n):
            hh, j, kt = steps[n]
            d = kt - 4 * j
            qs = 128 * max(d, 0)
            for m in range(2):
                bank = (n % 2) * 2 + m
                P.act(ET[m][n % 3][:, qs:512], pb[bank][:, qs:512], AF.Exp, [f"pb{bank}"], [f"ET{m}_{n % 3}"], scale=0.125)

        def emit_pv(n):
            hh, j, kt = steps[n]
            d = kt - 4 * j
            for t in range(8):
                m, qsub = t // 4, t % 4
                if qsub < d:
                    continue
                bank = 4 + t // 3
                col = (t % 3) * 129
                first = (kt == 0) and (t % 3 == 0)
                P.op("pe", (lambda bank, col, m, n, qsub, kt, hh, first: lambda e: e.matmul(
                    pb[bank][:, col:col + 129], ET[m][n % 3][:, qsub * 128:(qsub + 1) * 128], Vaug[:, kt, hh, :],
                    start=first, stop=False, skip_group_check=True))(bank, col, m, n, qsub, kt, hh, first),
                    [f"ET{m}_{n % 3}", f"V{kt}"], [f"pb{bank}"])

        def emit_final(n):
            hh, j, kt = steps[n]
            oc = ocp[ocount[0] % 2]
            ock = f"ocp{ocount[0] % 2}"
            ocount[0] += 1
            for b3 in range(3):
                ncol = 387 if b3 < 2 else 258
                P.cp("act" if b3 == 1 else "dve", oc[:, b3, 0:ncol], pb[4 + b3][:, 0:ncol], [f"pb{4 + b3}"], [f"{ock}_{b3}"])
            for qsub in range(4):
                t0_, t1_ = qsub, 4 + qsub
                O0 = oc[:, t0_ // 3, (t0_ % 3) * 129:(t0_ % 3) * 129 + 129]
                O1 = oc[:, t1_ // 3, (t1_ % 3) * 129:(t1_ % 3) * 129 + 129]
                k0, k1 = f"{ock}_{t0_ // 3}", f"{ock}_{t1_ // 3}"
                P.op("dve", (lambda O0: lambda e: e.reciprocal(out=rl[:, 0:1], in_=O0[:, 128:129]))(O0), [k0], ["rl0"])
                P.op("dve", (lambda O1: lambda e: e.reciprocal(out=rl[:, 1:2], in_=O1[:, 128:129]))(O1), [k1], ["rl1"])
                P.tt("dve", rl[:, 1:2], rl[:, 1:2], nlam[:], ALU.mult, ["rl1", "nlam"], ["rl1"])
                P.ts("dve", t0[:], O0[:, 0:128], rl[:, 0:1], None, ALU.mult, None, [k0, "rl0"], ["t0"])
                P.stt(av[:], O1[:, 0:128], rl[:, 1:2], t0[:], ALU.mult, ALU.add, [k1, "rl1", "t0"], ["av"])
                P.stt(junk[:], av[:], 1.0, av[:], ALU.mult, ALU.mult, ["av"], ["junk", "ss"], accum=ss[:, 0:1])
                P.act(rstd[:], ss[:], AF.Sqrt, ["ss", "epst"], ["rstd"], bias=epst[:, 0:1], scale=1.0 / 128.0)
                P.op("dve", lambda e: e.reciprocal(out=rstd[:], in_=rstd[:]), ["rstd"], ["rstd"])
                osl = (j * 4 + qsub) % 2
                P.stt(ot[osl][:], av[:], rstd[:, 0:1], subgs[:], ALU.mult, ALU.mult, ["av", "rstd", "subgs"], [f"ot{osl}"])
                r0 = (j * 4 + qsub) * 128
                hcol = (hp * 2 + hh) * 128
                outs.append(P.st("sp", o_d.ap()[r0:r0 + 128, hcol:hcol + 128], ot[osl][:], [f"ot{osl}"], f"ost{osl}"))

        nsteps = len(steps)
        emit_qk(0)
        for n in range(nsteps):
            emit_exp(n)
            if n + 1 < nsteps:
                emit_qk(n + 1)
            if dbg != 2:
                emit_pv(n)
            hh, j, kt = steps[n]
            if kt == 4 * j + 3 and dbg not in (2, 3):
                emit_final(n)
    P.emit(final_wait_ops=outs)
    return nc


def attn_consts(S):
    inv = (500000.0 ** (-np.arange(0, 16, 2, dtype=np.float32) / 16.0)).astype(np.float32)
    ang = np.arange(S, dtype=np.float32)[:, None] * inv[None, :]
    cs = np.concatenate([np.cos(ang), np.sin(ang)], axis=1).astype(np.float32)
    cs = np.ascontiguousarray(cs.reshape(S // 128, 128, 16).transpose(1, 0, 2).reshape(128, (S // 128) * 16))
    k = np.arange(128)[:, None]
    q = np.arange(128)[None, :]
    tri = np.where(q >= k, 0.0, -30000.0).astype(np.float32)
    return dict(cs=cs, trimask=tri, ident=np.eye(128, dtype=np.float32))


def bfv(pbank):
    return pbank[:].bitcast(BF16)


def attn_phase(P, nc, pb, S, src_d, src_row, wq_a, wk_a, wv_a, cs_d, lqk_a, linit_a, subg_a, ident_d, tri_d, o_d):
    P.reset_sbuf()
    NT = S // 128
    NCH = S // 512
    identb = P.sbuf([128, 128], BF16, "identb")
    trib = P.sbuf([128, 128], BF16, "trib")
    cs = P.sbuf([128, NT, 16], F32, "cs")
    lqk = P.sbuf([128, 4, 64], F32, "lqk")
    linit = P.sbuf([128, 1], F32, "linit")
    subg = P.sbuf([128, 128], F32, "subg")
    subgs = P.sbuf([128, 128], F32, "subgs")
    junk64 = P.sbuf([128, 64], F32, "junk64")
    ssum = P.sbuf([128, 2], F32, "ssum")
    esum = P.sbuf([128, 2], F32, "esum")
    nlam = P.sbuf([128, 1], F32, "nlam")
    om = P.sbuf([128, 1], F32, "om")
    epst = P.sbuf([128, 1], F32, "epst")
    P.ld("pool", identb[:], ident_d.ap(), ["identb"])
    P.ld("pool", trib[:], tri_d.ap(), ["trib"])
    P.ld("sp", cs[:].rearrange("p t c -> p (t c)"), cs_d.ap(), ["cs"])
    for n in range(4):
        P.ld("sp", lqk[:, n, :], lqk_a[n:n + 1, :].partition_broadcast(128), [f"lqk{n}"])
    P.ld("sp", linit[:], linit_a.partition_broadcast(128), ["linit"])
    P.ld("sp", subg[:], subg_a.partition_broadcast(128), ["subg"])
    P.op("pool", lambda e: e.memset(epst[:], SUBLN_EPS), (), ["epst"])
    P.stt(junk64[:], lqk[:, 0, :], 1.0, lqk[:, 1, :], ALU.mult, ALU.mult, ["lqk0", "lqk1"], ["junk64", "ssum0"], accum=ssum[:, 0:1])
    P.stt(junk64[:], lqk[:, 2, :], 1.0, lqk[:, 3, :], ALU.mult, ALU.mult, ["lqk2", "lqk3"], ["junk64", "ssum1"], accum=ssum[:, 1:2])
    P.act(esum[:], ssum[:], AF.Exp, ["ssum0", "ssum1"], ["esum"])
    P.tt("dve", nlam[:], esum[:, 1:2], esum[:, 0:1], ALU.subtract, ["esum"], ["nlam"])
    P.tt("dve", nlam[:], nlam[:], linit[:], ALU.subtract, ["nlam", "linit"], ["nlam"])
    P.ts("dve", om[:], linit[:], -1.0, 1.0, ALU.mult, ALU.add, ["linit"], ["om"])
    P.ts("dve", subgs[:], subg[:], om[:, 0:1], None, ALU.mult, None, ["subg", "om"], ["subgs"])

    QKT = P.sbuf([128, 4, S], BF16, "QKT")
    Vaug = P.sbuf([128, NT, 2, 129], BF16, "Vaug")
    wq = P.sbuf([128, 8, 256], BF16, "wq")
    wk = P.sbuf([128, 8, 256], BF16, "wk")
    wv = P.sbuf([128, 8, 256], BF16, "wv")
    xt = [P.sbuf([128, D], BF16, f"xt{i}") for i in range(2)]
    xTt = [P.sbuf([128, 8, 128], BF16, f"xTt{i}") for i in range(2)]
    qksb = [P.sbuf([128, 512], BF16, f"qksb{i}") for i in range(2)]
    tA = P.sbuf([128, 8, 8], F32, "tA")
    tB = P.sbuf([128, 8, 8], F32, "tB")
    ET = [[P.sbuf([128, 512], BF16, f"ET{m}_{i}") for i in range(3)] for m in range(2)]
    ocp = [P.sbuf([128, 3, 512], F32, f"ocp{i}") for i in range(2)]
    rl = P.sbuf([128, 2], F32, "rl")
    t0 = P.sbuf([128, 128], F32, "t0")
    av = P.sbuf([128, 128], F32, "av")
    junk = P.sbuf([128, 128], F32, "junk")
    ss = P.sbuf([128, 1], F32, "ss")
    rstd = P.sbuf([128, 1], F32, "rstd")
    ot = [P.sbuf([128, 128], F32, f"ot{i}") for i in range(2)]
    P.op("pool", lambda e: e.memset(Vaug[:], 1.0), (), ["Vaug_init"])
    ocount = [0]
    pbbq = bfv(pb[7])
    pbbx = bfv(pb[6])

    for hp in range(2):
        for w_, wa_, nm in ((wq, wq_a, "wq"), (wk, wk_a, "wk"), (wv, wv_a, "wv")):
            P.ld("pool", w_[:], wa_[:, hp * 256:(hp + 1) * 256].rearrange("(k p) n -> p k n", p=128), [nm])
        for tt in range(NT):
            s = tt % 2
            P.ld("pool", xt[s][:], src_d.ap()[src_row(tt):src_row(tt) + 128, :], [f"xt{s}"])
            for k in range(8):
                P.tr(pbbx[:, k * 128:(k + 1) * 128], xt[s][:, k * 128:(k + 1) * 128], identb[:], [f"xt{s}", "identb"], ["pb6"])
            P.cp("act", xTt[s][:, 0:4, :], pbbx[:, 0:512].rearrange("p (k t) -> p k t", k=4), ["pb6"], [f"xTt{s}a"])
            P.cp("dve", xTt[s][:, 4:8, :], pbbx[:, 512:1024].rearrange("p (k t) -> p k t", k=4), ["pb6"], [f"xTt{s}b"])
            xk = [f"xTt{s}a", f"xTt{s}b"]
            pqk = pb[s]
            pv = pb[2 + s]
            for k in range(8):
                P.mm(pqk[:, 0:256], xTt[s][:, k, :], wq[:, k, :], k == 0, False, xk + ["wq"], [f"pb{s}"])
            for k in range(8):
                P.mm(pqk[:, 256:512], xTt[s][:, k, :], wk[:, k, :], False, k == 7, xk + ["wk"], [f"pb{s}"])
            for k in range(8):
                P.mm(pv[:, 0:256], xTt[s][:, k, :], wv[:, k, :], k == 0, k == 7, xk + ["wv"], [f"pb{2 + s}"])
            qv = pqk[:].rearrange("p (g d) -> p g d", g=8)
            qs_ = qksb[s][:].rearrange("p (g d) -> p g d", g=8)
            P.cp("act", qs_[:, :, 16:64], qv[:, :, 16:64], [f"pb{s}"], [f"qkrest{s}"])
            cosb = cs[:, tt, 0:8].unsqueeze(1).to_broadcast([128, 8, 8])
            sinb = cs[:, tt, 8:16].unsqueeze(1).to_broadcast([128, 8, 8])
            P.tt("dve", tA[:], qv[:, :, 0:8], cosb, ALU.mult, [f"pb{s}", "cs"], ["tA"])
            P.tt("dve", tB[:], qv[:, :, 8:16], sinb, ALU.mult, [f"pb{s}", "cs"], ["tB"])
            P.tt("dve", qs_[:, :, 0:8], tA[:], tB[:], ALU.subtract, ["tA", "tB"], [f"qkrot{s}a"])
            P.tt("dve", tA[:], qv[:, :, 0:8], sinb, ALU.mult, [f"pb{s}", "cs"], ["tA"])
            P.tt("dve", tB[:], qv[:, :, 8:16], cosb, ALU.mult, [f"pb{s}", "cs"], ["tB"])
            P.tt("dve", qs_[:, :, 8:16], tA[:], tB[:], ALU.add, ["tA", "tB"], [f"qkrot{s}b"])
            for blk in range(4):
                P.tr(pbbq[:, s * 512 + blk * 128:s * 512 + (blk + 1) * 128], qksb[s][:, blk * 128:(blk + 1) * 128], identb[:],
                     [f"qkrest{s}", f"qkrot{s}a", f"qkrot{s}b", "identb"], ["pb7"])
            P.cp("dve" if s else "act", QKT[:, :, tt * 128:(tt + 1) * 128], pbbq[:, s * 512:(s + 1) * 512].rearrange("p (b t) -> p b t", b=4),
                 ["pb7"], [f"QKT{tt}"])
            P.cp("act" if s else "dve", Vaug[:, tt, :, 0:128], pv[:, 0:256].rearrange("p (h d) -> p h d", h=2),
                 [f"pb{2 + s}", "Vaug_init"], [f"V{tt}"])

        steps = [(hh, j, kt) for hh in range(2) for j in range(NCH) for kt in range(4 * j + 4)]

        def emit_qk(n):
            hh, j, kt = steps[n]
            d = kt - 4 * j
            qs = 128 * max(d, 0)
            for m in range(2):
                bank = (n % 2) * 2 + m
                pS = pb[bank]
                rd = [f"QKT{t}" for t in range(4 * j + qs // 128, 4 * j + 4)] + [f"QKT{kt}"]
                P.mm(pS[:, qs:512], QKT[m * 64:(m + 1) * 64, 2 + hh, kt * 128:(kt + 1) * 128],
                     QKT[m * 64:(m + 1) * 64, hh, j * 512 + qs:(j + 1) * 512], True, d < 0, rd, [f"pb{bank}"])
                if d >= 0:
                    P.mm(pS[:, qs:qs + 128], identb[:], trib[:], False, True, ["identb", "trib"], [f"pb{bank}"])

        def emit_exp(n):
            hh, j, kt = steps[n]
            d = kt - 4 * j
            qs = 128 * max(d, 0)
            for m in range(2):
                bank = (n % 2) * 2 + m
                P.act(ET[m][n % 3][:, qs:512], pb[bank][:, qs:512], AF.Exp, [f"pb{bank}"], [f"ET{m}_{n % 3}"], scale=0.125)

        def emit_pv(n):
            hh, j, kt = steps[n]
            d = kt - 4 * j
            for t in range(8):
                m, qsub = t // 4, t % 4
                if qsub < d:
                    continue
                bank = 4 + t // 3
                col = (t % 3) * 129
                first = (kt == 0) and (t % 3 == 0)
                P.op("pe", (lambda bank, col, m, n, qsub, kt, hh, first: lambda e: e.matmul(
                    pb[bank][:, col:col + 129], ET[m][n % 3][:, qsub * 128:(qsub + 1) * 128], Vaug[:, kt, hh, :],
                    start=first, stop=False, skip_group_check=True))(bank, col, m, n, qsub, kt, hh, first),
                    [f"ET{m}_{n % 3}", f"V{kt}"], [f"pb{bank}"])

        def emit_final(n):
            hh, j, kt = steps[n]
            oc = ocp[ocount[0] % 2]
            ock = f"ocp{ocount[0] % 2}"
            ocount[0] += 1
            for b3 in range(3):
                ncol = 387 if b3 < 2 else 258
                P.cp("act" if b3 == 1 else "dve", oc[:, b3, 0:ncol], pb[4 + b3][:, 0:ncol], [f"pb{4 + b3}"], [f"{ock}_{b3}"])
            for qsub in range(4):
                t0_, t1_ = qsub, 4 + qsub
                O0 = oc[:, t0_ // 3, (t0_ % 3) * 129:(t0_ % 3) * 129 + 129]
                O1 = oc[:, t1_ // 3, (t1_ % 3) * 129:(t1_ % 3) * 129 + 129]
                k0, k1 = f"{ock}_{t0_ // 3}", f"{ock}_{t1_ // 3}"
                P.op("dve", (lambda O0: lambda e: e.reciprocal(out=rl[:, 0:1], in_=O0[:, 128:129]))(O0), [k0], ["rl0"])
                P.op("dve", (lambda O1: lambda e: e.reciprocal(out=rl[:, 1:2], in_=O1[:, 128:129]))(O1), [k1], ["rl1"])
                P.tt("dve", rl[:, 1:2], rl[:, 1:2], nlam[:], ALU.mult, ["rl1", "nlam"], ["rl1"])
                P.ts("dve", t0[:], O0[:, 0:128], rl[:, 0:1], None, ALU.mult, None, [k0, "rl0"], ["t0"])
                P.stt(av[:], O1[:, 0:128], rl[:, 1:2], t0[:], ALU.mult, ALU.add, [k1, "rl1", "t0"], ["av"])
                P.stt(junk[:], av[:], 1.0, av[:], ALU.mult, ALU.mult, ["av"], ["junk", "ss"], accum=ss[:, 0:1])
                P.act(rstd[:], ss[:], AF.Sqrt, ["ss", "epst"], ["rstd"], bias=epst[:, 0:1], scale=1.0 / 128.0)
                P.op("dve", lambda e: e.reciprocal(out=rstd[:], in_=rstd[:]), ["rstd"], ["rstd"])
                osl = (j * 4 + qsub) % 2
                P.stt(ot[osl][:], av[:], rstd[:, 0:1], subgs[:], ALU.mult, ALU.mult, ["av", "rstd", "subgs"], [f"ot{osl}"])
                r0 = (j * 4 + qsub) * 128
                hcol = (hp * 2 + hh) * 128
                P.st("sp", o_d.ap()[r0:r0 + 128, hcol:hcol + 128], ot[osl][:], [f"ot{osl}"], f"ost{osl}")

        nsteps = len(steps)
        emit_qk(0)
        for n in range(nsteps):
            emit_exp(n)
            if n + 1 < nsteps:
                emit_qk(n + 1)
            emit_pv(n)
            hh, j, kt = steps[n]
            if kt == 4 * j + 3:
                emit_final(n)


def tail_phase(P, nc, pb, kind, T, C, NEXP, hsrc_fn, hdst_fn, W, scr, final):
    P.reset_sbuf()
    NTL = T // 128
    NSB = C // 128
    h1_d, xin_d, y_d = scr["h1"], scr["xin"], scr["y"]
    ident = P.sbuf([128, 128], F32, "ident")
    identb = P.sbuf([128, 128], BF16, "identb")
    ustr = P.sbuf([128, 128], BF16, "ustr")
    ones = P.sbuf([128, 128], BF16, "ones")
    ecoff = P.sbuf([128, 32], F32, "ecoff")
    brt = P.sbuf([128, 36], F32, "brt")
    wrt = P.sbuf([128, 8, 36], F32, "wrt")
    lng = P.sbuf([128, 2, D], F32, "lng")
    lnb = P.sbuf([128, 2, D], F32, "lnb")
    epst = P.sbuf([128, 1], F32, "epst")
    Scnt = P.sbuf([128, 32], BF16, "Scnt")
    gates = P.sbuf([128, NTL, 2], F32, "gates")
    dest = P.sbuf([128, NTL, 2], I32, "dest")
    P.ld("sp", ident[:], W["ident"], ["ident"])
    P.ld("pool", identb[:], W["ident"], ["identb"])
    P.ld("pool", ustr[:], W["ustrict"], ["ustr"])
    P.op("pool", lambda e: e.memset(ones[:], 1.0), (), ["ones"])
    P.op("pool", lambda e: e.memset(epst[:], LN_EPS), (), ["epst"])
    P.op("pool", lambda e: e.memset(Scnt[:], 0.0), (), ["Scnt"])
    P.ld("sp", ecoff[:], W["ecoff"].partition_broadcast(128), ["ecoff"])
    P.ld("sp", brt[:], W["b_rt"].partition_broadcast(128), ["brt"])
    P.ld("sp", wrt[:], W["w_rt"].rearrange("(k p) n -> p k n", p=128), ["wrt"])
    for j in range(2):
        P.ld("sp", lng[:, j, :], W["ln_g"][j:j + 1, :].partition_broadcast(128), [f"lng{j}"])
        P.ld("sp", lnb[:, j, :], W["ln_b"][j:j + 1, :].partition_broadcast(128), [f"lnb{j}"])
    zt = P.sbuf([128, D], BF16, "zt")
    P.op("pool", lambda e: e.memset(zt[:], 0.0), (), ["zt"])
    nz = (NE * C) // 128
    for z in range(nz):
        P.dma("sp", (lambda z: lambda e: e.dma_start(out=xin_d.ap()[z * 128:(z + 1) * 128, :], in_=zt[:]))(z), ["zt"], ["xinz"] if z == nz - 1 else [f"xinz{z}"], semkey="xinz")
    pbb = bfv(pb[6])

    if kind == "attn":
        wo = P.sbuf([128, 8, D], BF16, "wo")
        P.ld("pool", wo[:], W["w_o"].rearrange("(k p) n -> p k n", p=128), ["wo"])
        oidx = P.sbuf([128, NTL * 2], I32, "oidx")
        P.ld("sp", oidx[:], W["oidx"], ["oidx"])
        og = [P.sbuf([128, D], F32, f"og{i}") for i in range(2)]
        ob = P.sbuf([128, D], BF16, "ob")
        oTt = P.sbuf([128, 8, 128], BF16, "oTt")
    else:
        win = P.sbuf([128, 8, D], BF16, "win")
        wout = P.sbuf([128, 8, D], BF16, "wout")
        wgrp = P.sbuf([128, 4, 2, 256], BF16, "wgrp")
        lsT = P.sbuf([128, 8], F32, "lsT")
        am0 = P.sbuf([128, 4, 128], BF16, "am0")
        amd = P.sbuf([128, 4, 128], BF16, "amd")
        amo = P.sbuf([128, 4, 128], BF16, "amo")
        hidx = P.sbuf([128, 1], I32, "hidx")
        P.ld("sp", hidx[:], W["hidx"], ["hidx"])
        P.ld("pool", win[:], W["w_in"].rearrange("(k p) n -> p k n", p=128), ["win"])
        P.ld("pool", wout[:], W["w_out"].rearrange("(k p) n -> p k n", p=128), ["wout"])
        P.ld("pool", wgrp[:], W["w_grp"].rearrange("g (k p) n -> p g k n", p=128), ["wgrp"])
        P.ld("sp", lsT[:], W["lsT"], ["lsT"])
        P.ld("pool", am0[:], W["am0"].rearrange("w p n -> p w n"), ["am0"])
        P.ld("pool", amd[:], W["amd"].rearrange("w p n -> p w n"), ["amd"])
        P.ld("pool", amo[:], W["amo"].rearrange("w p n -> p w n"), ["amo"])
        ubuf = [P.sbuf([128, D], BF16, f"u{i}") for i in range(3)]
        hTb = P.sbuf([128, 8, 128], BF16, "hTb")
        pT = P.sbuf([128, 8, 128], BF16, "pT")
        qT = P.sbuf([128, 8, 128], BF16, "qT")

    htile = [P.sbuf([128, D], F32, f"ht{i}") for i in range(2)]
    rt = [P.sbuf([128, D], F32, f"rt{i}") for i in range(2)]
    h1b = [P.sbuf([128, D], BF16, f"h1b{i}") for i in range(2)]
    h1T = P.sbuf([128, 8, 128], F32, "h1T")
    stats = P.sbuf([128, 2, 6], F32, "stats")
    mv = P.sbuf([128, 2], F32, "mv")
    rstd = P.sbuf([128, 1], F32, "rstd")
    nmr = P.sbuf([128, 1], F32, "nmr")
    L = P.sbuf([128, 36], F32, "L")
    gmax = P.sbuf([128, 1], F32, "gmax")
    ngmax = P.sbuf([128, 1], F32, "ngmax")
    G1 = P.sbuf([128, 4], F32, "G1")
    ge = P.sbuf([128, 4], F32, "ge")
    gsum = P.sbuf([128, 1], F32, "gsum")
    pen = P.sbuf([128, 4], F32, "pen")
    Lm = P.sbuf([128, 32], F32, "Lm")
    Lm2 = P.sbuf([128, 32], F32, "Lm2")
    m1 = P.sbuf([128, 1], F32, "m1")
    m2 = P.sbuf([128, 1], F32, "m2")
    OH1 = P.sbuf([128, 32], F32, "OH1")
    OH2 = P.sbuf([128, 32], F32, "OH2")
    OHc = P.sbuf([128, 32], BF16, "OHc")
    dd = P.sbuf([128, 1], F32, "dd")
    ex = P.sbuf([128, 1], F32, "ex")
    den = P.sbuf([128, 1], F32, "den")
    Pf = P.sbuf([128, 32], F32, "Pf")
    tmp32 = P.sbuf([128, 32], F32, "tmp32")
    dflt = P.sbuf([128, 2], F32, "dflt")

    def layer_norm(src, j, dst, keys_r, key_w):
        sv = src.rearrange("p (c f) -> p c f", c=2)
        for c in range(2):
            P.op("dve", (lambda c: lambda e: e.bn_stats(out=stats[:, c, :], in_=sv[:, c, :]))(c), keys_r, [f"stats{c}"])
        P.op("dve", lambda e: e.bn_aggr(out=mv[:], in_=stats[:]), ["stats0", "stats1"], ["mv"])
        P.act(rstd[:], mv[:, 1:2], AF.Sqrt, ["mv", "epst"], ["rstd"], bias=epst[:, 0:1])
        P.op("dve", lambda e: e.reciprocal(out=rstd[:], in_=rstd[:]), ["rstd"], ["rstd"])
        P.stt(nmr[:], mv[:, 0:1], -1.0, rstd[:], ALU.mult, ALU.mult, ["mv", "rstd"], ["nmr"])
        P.act(dst, src, AF.Identity, list(keys_r) + ["rstd", "nmr"], [key_w], bias=nmr[:, 0:1], scale=rstd[:, 0:1])
        P.tt("dve", dst, dst, lng[:, j, :], ALU.mult, [key_w, f"lng{j}"], [key_w])
        P.tt("dve", dst, dst, lnb[:, j, :], ALU.add, [key_w, f"lnb{j}"], [key_w])

    def pool_u(i, slot, hslot):
        hs = htile[hslot]
        hk = f"ht{hslot}"
        if i < 0:
            P.dma("pool", lambda e: e.indirect_dma_start(out=hs[:], out_offset=None, in_=scr["halo_all"].ap(),
                                                         in_offset=bass.IndirectOffsetOnAxis(ap=hidx[:, 0:1], axis=0)),
                  ["hidx"], [hk], semkey=hk)
        for k in range(8):
            P.tr(pb[2 + k // 4][:, (k % 4) * 128:(k % 4 + 1) * 128], hs[:, k * 128:(k + 1) * 128], ident[:], [hk, "ident"], [f"pb{2 + k // 4}"])
        for hf in range(2):
            P.cp("act" if hf == 0 else "dve", hTb[:, hf * 4:(hf + 1) * 4, :], pb[2 + hf][:].rearrange("p (k t) -> p k t", k=4), [f"pb{2 + hf}"], [f"hTb{hf}"])
        for hf in range(2):
            for k in range(8):
                P.mm(pb[hf][:], hTb[:, k, :], win[:, k, hf * 512:(hf + 1) * 512], k == 0, k == 7, [f"hTb{k // 4}", "win"], [f"pb{hf}"])
        for hf in range(2):
            P.cp("act" if hf == 0 else "dve", ubuf[slot][:, hf * 512:(hf + 1) * 512], pb[hf][:], [f"pb{hf}"], [f"u{slot}_{hf}"])

    if kind == "pool":
        pool_u(-1, 2, 1)

    def s1_loads(i):
        s = i % 2
        P.ld("sp", htile[s][:], hsrc_fn(i), [f"ht{s}"])
        if kind == "attn":
            for g in range(2):
                P.dma("pool", (lambda i, g, s: lambda e: e.indirect_dma_start(
                    out=og[s][:, g * 512:(g + 1) * 512], out_offset=None, in_=scr["o_all"].ap(),
                    in_offset=bass.IndirectOffsetOnAxis(ap=oidx[:, i * 2 + g:i * 2 + g + 1], axis=0)))(i, g, s),
                    ["oidx"], [f"og{s}_{g}"], semkey=f"og{s}_{g}")

    s1_loads(0)
    for i in range(NTL):
        s = i % 2
        hk = f"ht{s}"
        if i + 1 < NTL:
            s1_loads(i + 1)
        if kind == "attn":
            P.cp("act", ob[:], og[s][:], [f"og{s}_0", f"og{s}_1"], ["ob"])
            for k in range(8):
                P.tr(pbb[:, k * 128:(k + 1) * 128], ob[:, k * 128:(k + 1) * 128], identb[:], ["ob", "identb"], ["pb6"])
            P.cp("dve", oTt[:].rearrange("p k t -> p (k t)"), pbb, ["pb6"], ["oTt"])
            for hf in range(2):
                for k in range(8):
                    P.mm(pb[hf][:], oTt[:, k, :], wo[:, k, hf * 512:(hf + 1) * 512], k == 0, k == 7, ["oTt", "wo"], [f"pb{hf}"])
        else:
            us = i % 3
            up = (i - 1) % 3
            pool_u(i, us, s)
            A = am0 if i == 0 else amd
            Ak = "am0" if i == 0 else "amd"
            for j in range(8):
                g = j // 2
                o_ = pb[4 + j // 4][:, (j % 4) * 128:(j % 4 + 1) * 128]
                P.mm(o_, ubuf[us][:, j * 128:(j + 1) * 128], A[:, g, :], True, False, [f"u{us}_{j // 4}", Ak], [f"pb{4 + j // 4}"])
                P.mm(o_, ubuf[up][:, j * 128:(j + 1) * 128], amo[:, g, :], False, True, [f"u{up}_{j // 4}", "amo"], [f"pb{4 + j // 4}"])
            for hf in range(2):
                P.cp("act" if hf == 0 else "dve", pT[:, hf * 4:(hf + 1) * 4, :], pb[4 + hf][:].rearrange("p (k t) -> p k t", k=4), [f"pb{4 + hf}"], [f"pT{hf}"])
            for j in range(8):
                g = j // 2
                o_ = pb[4 + j // 4][:, (j % 4) * 128:(j % 4 + 1) * 128]
                for kk in range(2):
                    P.mm(o_, wgrp[:, g, kk, (j % 2) * 128:(j % 2 + 1) * 128], pT[:, 2 * g + kk, :], kk == 0, kk == 1, [f"pT{(2 * g + kk) // 4}", "wgrp"], [f"pb{4 + j // 4}"])
            for j in range(8):
                P.act(qT[:, j, :], pb[4 + j // 4][:, (j % 4) * 128:(j % 4 + 1) * 128], AF.Identity, [f"pb{4 + j // 4}", "lsT"], [f"qT{j}"], scale=lsT[:, j:j + 1])
            for hf in range(2):
                for k in range(8):
                    P.mm(pb[hf][:], qT[:, k, :], wout[:, k, hf * 512:(hf + 1) * 512], k == 0, k == 7, [f"qT{k}", "wout"], [f"pb{hf}"])
        r = rt[s]
        rk = f"rt{s}"
        for hf in range(2):
            P.stt(r[:, hf * 512:(hf + 1) * 512], htile[s][:, hf * 512:(hf + 1) * 512], ALPHA, pb[hf][:], ALU.mult, ALU.add, [hk, f"pb{hf}"], [rk])
        layer_norm(r[:], 0, r[:], [rk], rk)
        P.st("sp", h1_d.ap()[i * 128:(i + 1) * 128, :], r[:], [rk], f"h1st{s}", [f"h1d{i}"])
        P.cp("act", h1b[s][:], r[:], [rk], [f"h1b{s}"])
        for k in range(8):
            P.tr(pb[2 + k // 4][:, (k % 4) * 128:(k % 4 + 1) * 128], r[:, k * 128:(k + 1) * 128], ident[:], [rk, "ident"], [f"pb{2 + k // 4}"])
        for hf in range(2):
            P.cp("act" if hf == 0 else "dve", h1T[:, hf * 4:(hf + 1) * 4, :], pb[2 + hf][:].rearrange("p (k t) -> p k t", k=4), [f"pb{2 + hf}"], [f"h1T{hf}"])
        for k in range(8):
            P.mm(pb[4][:, 0:36], h1T[:, k, :], wrt[:, k, :], k == 0, k == 7, [f"h1T{k // 4}", "wrt"], ["pb4"])
        P.tt("dve", L[:], pb[4][:, 0:36], brt[:], ALU.add, ["pb4", "brt"], ["L"])
        P.red(gmax[:], L[:, 0:4], ALU.max, ["L"], ["gmax"])
        P.tt("dve", G1[:], L[:, 0:4], gmax[:, 0:1].to_broadcast([128, 4]), ALU.is_equal, ["L", "gmax"], ["G1"])
        P.ts("dve", ngmax[:], gmax[:], -1.0, None, ALU.mult, None, ["gmax"], ["ngmax"])
        P.act(ge[:], L[:, 0:4], AF.Exp, ["L", "ngmax"], ["ge", "gsum"], bias=ngmax[:, 0:1], accum=gsum[:, 0:1])
        P.ts("dve", pen[:], G1[:], -1.0, 1e30, ALU.add, ALU.mult, ["G1"], ["pen"])
        P.tt("dve", Lm[:].rearrange("p (g e) -> p g e", g=4), L[:, 4:36].rearrange("p (g e) -> p g e", g=4),
             pen[:].unsqueeze(2).to_broadcast([128, 4, 8]), ALU.add, ["L", "pen"], ["Lm"])
        P.red(m1[:], Lm[:], ALU.max, ["Lm"], ["m1"])
        P.tt("dve", OH1[:], Lm[:], m1[:, 0:1].to_broadcast([128, 32]), ALU.is_equal, ["Lm", "m1"], ["OH1"])
        P.stt(Lm2[:], OH1[:], -1e30, Lm[:], ALU.mult, ALU.add, ["OH1", "Lm"], ["Lm2"])
        P.red(m2[:], Lm2[:], ALU.max, ["Lm2"], ["m2"])
        P.tt("dve", OH2[:], Lm2[:], m2[:, 0:1].to_broadcast([128, 32]), ALU.is_equal, ["Lm2", "m2"], ["OH2"])
        P.tt("dve", dd[:], m2[:], m1[:], ALU.subtract, ["m1", "m2"], ["dd"])
        P.act(ex[:], dd[:], AF.Exp, ["dd"], ["ex"])
        P.stt(den[:], ex[:], 1.0, gsum[:], ALU.add, ALU.mult, ["ex", "gsum"], ["den"])
        P.op("dve", (lambda i: lambda e: e.reciprocal(out=gates[:, i, 0:1], in_=den[:]))(i), ["den"], [f"gate{i}"])
        P.tt("dve", gates[:, i, 1:2], gates[:, i, 0:1], ex[:], ALU.mult, [f"gate{i}", "ex"], [f"gate{i}"])
        P.tt("dve", OHc[:], OH1[:], OH2[:], ALU.add, ["OH1", "OH2"], ["OHc"])
        P.mm(pb[5][:, 0:32], ustr[:], OHc[:], True, False, ["ustr", "OHc"], ["pb5"])
        P.mm(pb[5][:, 0:32], ones[:], Scnt[:], False, True, ["ones", "Scnt"], ["pb5"])
        P.tt("dve", Pf[:], pb[5][:, 0:32], ecoff[:], ALU.add, ["pb5", "ecoff"], ["Pf"])
        P.tt("dve", Scnt[:], Scnt[:], OHc[:], ALU.add, ["Scnt", "OHc"], ["Scnt"])
        P.stt(tmp32[:], OH1[:], 1.0, Pf[:], ALU.mult, ALU.mult, ["OH1", "Pf"], ["tmp32", "dflt0"], accum=dflt[:, 0:1])
        P.stt(tmp32[:], OH2[:], 1.0, Pf[:], ALU.mult, ALU.mult, ["OH2", "Pf"], ["tmp32", "dflt1"], accum=dflt[:, 1:2])
        P.cp("dve", dest[:, i, :], dflt[:], ["dflt0", "dflt1"], [f"dest{i}"])
        for kk in range(2):
            P.dma("pool", (lambda i, kk, s: lambda e: e.indirect_dma_start(
                out=xin_d.ap(), out_offset=bass.IndirectOffsetOnAxis(ap=dest[:, i, kk:kk + 1], axis=0),
                in_=h1b[s][:], in_offset=None))(i, kk, s), [f"h1b{s}", f"dest{i}", "xinz"], [f"xinw{i}_{kk}"], semkey=f"scat{s}{kk}")

    wgs = [P.sbuf([128, 8, DE], BF16, f"wg{i}") for i in range(2)]
    wus = [P.sbuf([128, 8, DE], BF16, f"wu{i}") for i in range(2)]
    wds = [P.sbuf([128, 4, D], BF16, f"wd{i}") for i in range(2)]
    xin = [P.sbuf([128, NSB, D], BF16, f"xin{i}") for i in range(2)]
    xT = [P.sbuf([128, 8, C], BF16, f"xT{i}") for i in range(2)]
    actT = [P.sbuf([128, 4, C], BF16, f"actT{i}") for i in range(2)]
    sg = [P.sbuf([128, C], F32, f"sg{i}") for i in range(2)]
    yb = [P.sbuf([128, D], F32, f"yb{i}") for i in range(2)]
    ycount = 0
    xin_keys = [f"xinw{i}_{kk}" for i in range(NTL) for kk in range(2)]
    y_keys = [f"y_{e_}_{sb}" for e_ in range(NEXP) for sb in range(NSB)]
    for e_ in range(NEXP):
        s = e_ % 2
        P.ld("pool", wgs[s][:], W["w_gate"][e_].rearrange("(k p) n -> p k n", p=128), [f"wg{s}"])
        P.ld("pool", wus[s][:], W["w_up"][e_].rearrange("(k p) n -> p k n", p=128), [f"wu{s}"])
        P.ld("pool", wds[s][:], W["w_down"][e_].rearrange("(k p) n -> p k n", p=128), [f"wd{s}"])
        if e_ == 0:
            P.ld("sp", xin[0][:], xin_d.ap()[0:C, :].rearrange("(b p) n -> p b n", p=128), ["xin0"], r=xin_keys)
        if e_ + 1 < NEXP:
            P.ld("sp", xin[1 - s][:], xin_d.ap()[(e_ + 1) * C:(e_ + 2) * C, :].rearrange("(b p) n -> p b n", p=128), [f"xin{1 - s}"], r=xin_keys)
        for sb in range(NSB):
            for k in range(8):
                P.tr(pbb[:, k * 128:(k + 1) * 128], xin[s][:, sb, k * 128:(k + 1) * 128], identb[:], [f"xin{s}", "identb"], ["pb6"])
            P.cp("dve" if sb % 2 else "act", xT[s][:, :, sb * 128:(sb + 1) * 128], pbb.rearrange("p (k t) -> p k t", k=8), ["pb6"], [f"xT{s}_{sb}"])
        xkeys = [f"xT{s}_{sb}" for sb in range(NSB)]
        for fc in range(4):
            pg = pb[(fc % 2) * 2]
            pu = pb[(fc % 2) * 2 + 1]
            kg, ku = f"pb{(fc % 2) * 2}", f"pb{(fc % 2) * 2 + 1}"
            for k in range(8):
                P.mm(pg[:, 0:C], wgs[s][:, k, fc * 128:(fc + 1) * 128], xT[s][:, k, :], k == 0, k == 7, xkeys + [f"wg{s}"], [kg])
            for k in range(8):
                P.mm(pu[:, 0:C], wus[s][:, k, fc * 128:(fc + 1) * 128], xT[s][:, k, :], k == 0, k == 7, xkeys + [f"wu{s}"], [ku])
            P.act(sg[fc % 2][:], pg[:, 0:C], AF.Silu, [kg], [f"sg{fc % 2}"])
            P.tt("dve", actT[s][:, fc, :], sg[fc % 2][:], pu[:, 0:C], ALU.mult, [f"sg{fc % 2}", ku], [f"actT{s}_{fc}"])
        akeys = [f"actT{s}_{fc}" for fc in range(4)]
        for sb in range(NSB):
            ys = ycount % 2
            ycount += 1
            for hf in range(2):
                py = pb[4 + hf]
                for fc in range(4):
                    P.mm(py[:], actT[s][:, fc, sb * 128:(sb + 1) * 128], wds[s][:, fc, hf * 512:(hf + 1) * 512], fc == 0, fc == 3, akeys + [f"wd{s}"], [f"pb{4 + hf}"])
                P.cp("act" if hf == 0 else "dve", yb[ys][:, hf * 512:(hf + 1) * 512], py[:], [f"pb{4 + hf}"], [f"yb{ys}"])
            r0 = e_ * C + sb * 128
            P.st("sp", y_d.ap()[r0:r0 + 128, :], yb[ys][:], [f"yb{ys}"], f"yst{ys}", [f"y_{e_}_{sb}"])

    y0 = [P.sbuf([128, D], F32, f"y0_{i}") for i in range(2)]
    y1 = [P.sbuf([128, D], F32, f"y1_{i}") for i in range(2)]
    outs = []
    def s3_loads(i):
        s = i % 2
        P.ld("sp", htile[s][:], h1_d.ap()[i * 128:(i + 1) * 128, :], [f"ht{s}"], r=[f"h1d{i}"])
        for kk, yt in ((0, y0), (1, y1)):
            P.dma("pool", (lambda i, kk, yt, s: lambda e: e.indirect_dma_start(
                out=yt[s][:], out_offset=None, in_=y_d.ap(),
                in_offset=bass.IndirectOffsetOnAxis(ap=dest[:, i, kk:kk + 1], axis=0)))(i, kk, yt, s),
                y_keys + [f"dest{i}"], [f"y{kk}_{s}"], semkey=f"gath{kk}{s}")

    s3_loads(0)
    for i in range(NTL):
        s = i % 2
        hk = f"ht{s}"
        if i + 1 < NTL:
            s3_loads(i + 1)
        r = rt[s]
        rk = f"rt{s}"
        P.act(r[:], htile[s][:], AF.Identity, [hk], [rk], scale=ALPHA)
        P.stt(r[:], y0[s][:], gates[:, i, 0:1], r[:], ALU.mult, ALU.add, [f"y0_{s}", f"gate{i}", rk], [rk])
        P.stt(r[:], y1[s][:], gates[:, i, 1:2], r[:], ALU.mult, ALU.add, [f"y1_{s}", f"gate{i}", rk], [rk])
        layer_norm(r[:], 1, r[:], [rk], rk)
        outs.append(P.st("sp", hdst_fn(i), r[:], [rk], f"ost{s}"))
    return outs


def build_mega(S, C, depth=DEPTH, NEXP=NE):
    T = S // 2
    NTL = T // 128
    nc = bass.Bass("TRN2", target_bir_lowering=False)
    P = Prog(nc)
    NA = (depth + 1) // 2
    NP = depth // 2
    inp = lambda name, shape, dt=F32: nc.dram_tensor(name, list(shape), dt, kind="ExternalInput")
    x_all = inp("x_all", [S, D])
    h0 = inp("h0", [T, D])
    oidx_d = inp("oidx", [128, NTL * 2], I32)
    hidx_d = inp("hidx", [128, 1], I32)
    wq_d = inp("wq", [NA, D, 512])
    wk_d = inp("wk", [NA, D, 512])
    wv_d = inp("wv", [NA, D, 512])
    wo_d = inp("w_o", [NA, D, D])
    lqk_d = inp("lqk", [NA, 4, 64])
    linit_d = inp("linit", [NA, 1])
    subg_d = inp("subg", [NA, 128])
    cs_d = inp("cs", [128, (S // 128) * 16])
    tri_d = inp("trimask", [128, 128])
    ident_d = inp("ident", [128, 128])
    ustr_d = inp("ustrict", [128, 128])
    ecoff_d = inp("ecoff", [1, 32])
    if NP:
        win_d = inp("w_in", [NP, D, D])
        wgrp_d = inp("w_grp", [NP, 4, 256, 256])
        lsT_d = inp("lsT", [NP, 128, 8])
        wout_d = inp("w_out", [NP, D, D])
        am0_d = inp("am0", [4, 128, 128])
        amd_d = inp("amd", [4, 128, 128])
        amo_d = inp("amo", [4, 128, 128])
    lng_d = inp("ln_g", [depth, 2, D])
    lnb_d = inp("ln_b", [depth, 2, D])
    wrt_d = inp("w_rt", [depth, D, 36])
    brt_d = inp("b_rt", [depth, 36])
    wg_d = inp("w_gate", [depth, NEXP, D, DE])
    wu_d = inp("w_up", [depth, NEXP, D, DE])
    wd_d = inp("w_down", [depth, NEXP, DE, D])
    out_d = nc.dram_tensor("out", [T, D], F32, kind="ExternalOutput")
    o_loc = nc.dram_tensor("o_loc", [S, 512], F32)
    o_all = nc.dram_tensor("o_all", [2 * S, 512], F32)
    hcur = nc.dram_tensor("hcur", [T, D], F32)
    h_all = nc.dram_tensor("h_all", [S, D], F32)
    halo_all = nc.dram_tensor("halo_all", [384, D], F32)
    scr = dict(h1=nc.dram_tensor("h1_scr", [T, D], F32), xin=nc.dram_tensor("xin_scr", [NE * C, D], BF16),
               y=nc.dram_tensor("y_scr", [NE * C, D], F32), o_all=o_all, halo_all=halo_all)
    pb = [P.psum([128, 512], F32, f"pb{i}") for i in range(8)]
    groups = [[0, 1], [2, 3], [4, 5], [6, 7]]
    CH_O = min(1024, S)
    CH_H = min(512, T)

    zf = P.sbuf([128, D], F32, "zf")
    P.op("pool", lambda e: e.memset(zf[:], 0.0), (), ["zf"])
    P.st("sp", halo_all.ap()[256:384, :], zf[:], ["zf"], "hz")
    P.fence()

    outs = []
    for i in range(depth):
        j = i // 2
        last = (i == depth - 1)
        W = dict(ident=ident_d.ap(), ustrict=ustr_d.ap(), ecoff=ecoff_d.ap(), b_rt=brt_d.ap()[i:i + 1, :], w_rt=wrt_d.ap()[i],
                 ln_g=lng_d.ap()[i], ln_b=lnb_d.ap()[i], w_gate=wg_d.ap()[i], w_up=wu_d.ap()[i], w_down=wd_d.ap()[i])
        hsrc = (lambda t: h0.ap()[t * 128:(t + 1) * 128, :]) if i == 0 else (lambda t: hcur.ap()[t * 128:(t + 1) * 128, :])
        hdst = (lambda t: out_d.ap()[t * 128:(t + 1) * 128, :]) if last else (lambda t: hcur.ap()[t * 128:(t + 1) * 128, :])
        if i % 2 == 0:
            if i == 0:
                src = x_all
                src_row = lambda tt: tt * 128
            else:
                for q in range(T // CH_H):
                    P.coll((lambda q: lambda e: e.collective_compute(
                        "AllGather", ALU.bypass, replica_groups=groups, ins=[hcur.ap()[q * CH_H:(q + 1) * CH_H, :]],
                        outs=[h_all.ap()[q * 2 * CH_H:(q + 1) * 2 * CH_H, :]]))(q), [], [], "cc")
                P.fence()
                src = h_all

                def src_row(tt):
                    r_, l_ = divmod(tt * 128, T)
                    return (l_ // CH_H) * 2 * CH_H + r_ * CH_H + l_ % CH_H
            attn_phase(P, nc, pb, S, src, src_row, wq_d.ap()[j], wk_d.ap()[j], wv_d.ap()[j], cs_d, lqk_d.ap()[j], linit_d.ap()[j:j + 1, :],
                       subg_d.ap()[j:j + 1, :], ident_d, tri_d, o_loc)
            P.fence()
            for q in range(S // CH_O):
                P.coll((lambda q: lambda e: e.collective_compute(
                    "AllGather", ALU.bypass, replica_groups=groups, ins=[o_loc.ap()[q * CH_O:(q + 1) * CH_O, :]],
                    outs=[o_all.ap()[q * 2 * CH_O:(q + 1) * 2 * CH_O, :]]))(q), [], [], "cc")
            P.fence()
            W.update(w_o=wo_d.ap()[j], oidx=oidx_d.ap())
            outs = tail_phase(P, nc, pb, "attn", T, C, NEXP, hsrc, hdst, W, scr, last)
        else:
            P.coll(lambda e: e.collective_compute("AllGather", ALU.bypass, replica_groups=groups,
                                                  ins=[hcur.ap()[T - 128:T, :]], outs=[halo_all.ap()[0:256, :]]), [], [], "cc")
            P.fence()
            W.update(w_in=win_d.ap()[j], w_grp=wgrp_d.ap()[j], lsT=lsT_d.ap()[j], w_out=wout_d.ap()[j],
                     am0=am0_d.ap(), amd=amd_d.ap(), amo=amo_d.ap(), hidx=hidx_d.ap())
            outs = tail_phase(P, nc, pb, "pool", T, C, NEXP, hsrc, hdst, W, scr, last)
        P.fence()
    P.emit(final_wait_ops=outs)
    return nc


_PROGS = {}
S_FULL = 8192
T_CORE = 4096
CAP = 384


def _prog(key):
    if key not in _PROGS:
        if key == "attn":
            _PROGS[key] = build_attn(S_FULL)
        elif key == "tail_attn":
            _PROGS[key] = build_tail("attn", T_CORE, CAP)
        else:
            _PROGS[key] = build_tail("pool", T_CORE, CAP)
    return _PROGS[key]


def _c(a):
    return np.ascontiguousarray(a, dtype=np.float32)


def mega_in_maps(inputs, S, C, depth):
    f32 = np.float32
    x = np.asarray(inputs["x"], dtype=f32)
    B = x.shape[0]
    T = S // 2
    NTL = T // 128
    NA = (depth + 1) // 2
    NP = depth // 2
    hc = host_consts(C)
    ac = attn_consts(S)
    wqkv = np.asarray(inputs["attn_w_qkv"], dtype=f32)[:NA]
    common = dict(
        w_o=_c(np.asarray(inputs["attn_w_o"])[:NA]),
        lqk=_c(np.stack([np.asarray(inputs[k])[:NA] for k in ("attn_lq1", "attn_lk1", "attn_lq2", "attn_lk2")], axis=1)),
        linit=np.array([[0.8 - 0.6 * math.exp(-0.3 * (2 * j))] for j in range(NA)], f32),
        subg=_c(np.asarray(inputs["attn_sub_g"])[:NA]),
        ln_g=_c(np.asarray(inputs["ln_g"])[:depth]), ln_b=_c(np.asarray(inputs["ln_b"])[:depth]),
        w_rt=_c(np.concatenate([np.asarray(inputs["moe_w_grp_router"])[:depth], np.asarray(inputs["moe_w_exp_router"])[:depth]], axis=2)),
        b_rt=_c(np.concatenate([np.asarray(inputs["moe_b_grp_router"])[:depth], np.asarray(inputs["moe_b_exp_router"])[:depth]], axis=1)),
        w_gate=_c(np.asarray(inputs["moe_w_gate"])[:depth]), w_up=_c(np.asarray(inputs["moe_w_up"])[:depth]),
        w_down=_c(np.asarray(inputs["moe_w_down"])[:depth]),
        cs=ac["cs"], trimask=ac["trimask"], **hc)
    if NP:
        pc = pool_consts(False)
        common.update(
            w_in=_c(np.asarray(inputs["pool_w_in"])[:NP]), w_grp=_c(np.asarray(inputs["pool_w_grp"])[:NP]),
            lsT=_c(np.asarray(inputs["pool_scale"], dtype=f32)[:NP].reshape(NP, 8, 128).transpose(0, 2, 1)),
            w_out=_c(np.asarray(inputs["pool_w_out"])[:NP]), amd=pc["amd"], amo=pc["amo"])
    p = np.arange(128, dtype=np.int64)[:, None]
    ch_o = min(1024, S)
    in_maps = []
    for c in range(NCORES):
        b, r = c // 2, c % 2
        oidx = np.zeros((128, NTL * 2), np.int32)
        for i in range(NTL):
            for g in range(2):
                t_ = r * T + i * 128 + p[:, 0]
                oidx[:, i * 2 + g] = (t_ // ch_o) * 2 * ch_o + g * ch_o + t_ % ch_o
        hidx = (p + (0 if r == 1 else 256)).astype(np.int32)
        m = dict(
            x_all=_c(x[b]), h0=_c(x[b, r * T:(r + 1) * T]), oidx=oidx, hidx=hidx,
            wq=_c(wqkv[:, :, r * 512:(r + 1) * 512]), wk=_c(wqkv[:, :, 1024 + r * 512:1024 + (r + 1) * 512]),
            wv=_c(wqkv[:, :, 2048 + r * 512:2048 + (r + 1) * 512]), **common)
        if NP:
            m["am0"] = pool_consts(r == 0)["am0"]
        in_maps.append(m)
    return in_maps


_MEGA = {}


def kernel(**inputs):
    x = np.asarray(inputs["x"])
    B, S, _ = x.shape
    if "m" not in _MEGA:
        _MEGA["m"] = build_mega(S, CAP)
    in_maps = mega_in_maps(inputs, S, CAP, DEPTH)
    res = run_bass_kernel_spmd(_MEGA["m"], in_maps, core_ids=list(range(NCORES)))
    T = S // 2
    out = np.empty((B, S, D), np.float32)
    for c in range(NCORES):
        out[c // 2, (c % 2) * T:(c % 2 + 1) * T] = res.results[c]["out"]
    return out


def kernel_unfused(**inputs):
    f32 = np.float32
    x = np.asarray(inputs["x"], dtype=f32)
    B, S, _ = x.shape
    h = x.reshape(B * S, D)
    cores = list(range(NCORES))
    hc = host_consts(CAP)
    ac = attn_consts(S)
    for i in range(DEPTH):
        j = i // 2
        tail_common = dict(
            ln_g=_c(inputs["ln_g"][i]), ln_b=_c(inputs["ln_b"][i]),
            w_rt=_c(np.concatenate([inputs["moe_w_grp_router"][i], inputs["moe_w_exp_router"][i]], axis=1)),
            b_rt=_c(np.concatenate([inputs["moe_b_grp_router"][i], inputs["moe_b_exp_router"][i]], axis=0).reshape(1, 36)),
            w_gate=_c(inputs["moe_w_gate"][i]), w_up=_c(inputs["moe_w_up"][i]), w_down=_c(inputs["moe_w_down"][i]),
            **hc)
        if i % 2 == 0:
            linit = 0.8 - 0.6 * math.exp(-0.3 * i)
            wqkv = np.asarray(inputs["attn_w_qkv"][j], dtype=f32)
            in_maps = []
            for c in cores:
                b, hg = c // 2, c % 2
                in_maps.append(dict(
                    xT=_c(h[b * S:(b + 1) * S].T),
                    wq=_c(wqkv[:, hg * 512:(hg + 1) * 512]),
                    wk=_c(wqkv[:, 1024 + hg * 512:1024 + (hg + 1) * 512]),
                    wv=_c(wqkv[:, 2048 + hg * 512:2048 + (hg + 1) * 512]),
                    lq1=_c(inputs["attn_lq1"][j]).reshape(1, 64), lk1=_c(inputs["attn_lk1"][j]).reshape(1, 64),
                    lq2=_c(inputs["attn_lq2"][j]).reshape(1, 64), lk2=_c(inputs["attn_lk2"][j]).reshape(1, 64),
                    linit=np.array([[linit]], f32), subg=_c(inputs["attn_sub_g"][j]).reshape(1, 128), **ac))
            res = run_bass_kernel_spmd(_prog("attn"), in_maps, core_ids=cores)
            o = np.empty((B * S, D), f32)
            for c in cores:
                b, hg = c // 2, c % 2
                o[b * S:(b + 1) * S, hg * 512:(hg + 1) * 512] = res.results[c]["o"]
            in_maps = []
            for c in cores:
                rows = slice(c * T_CORE, (c + 1) * T_CORE)
                in_maps.append(dict(h=_c(h[rows]), oT=_c(o[rows].T), w_o=_c(inputs["attn_w_o"][j]), **tail_common))
            res = run_bass_kernel_spmd(_prog("tail_attn"), in_maps, core_ids=cores)
        else:
            in_maps = []
            for c in cores:
                rows = slice(c * T_CORE, (c + 1) * T_CORE)
                first = (c % 2 == 0)
                halo = np.zeros((128, D), f32) if first else _c(h[c * T_CORE - 128:c * T_CORE])
                in_maps.append(dict(
                    h=_c(h[rows]), halo=halo, w_in=_c(inputs["pool_w_in"][j]), w_grp=_c(inputs["pool_w_grp"][j]),
                    lsT=_c(np.asarray(inputs["pool_scale"][j], dtype=f32).reshape(8, 128).T),
                    w_out=_c(inputs["pool_w_out"][j]), **pool_consts(first), **tail_common))
            res = run_bass_kernel_spmd(_prog("tail_pool"), in_maps, core_ids=cores)
        h = np.concatenate([res.results[c]["out"] for c in cores], axis=0)
    return h.reshape(B, S, D).astype(f32)
```

```python
import contextlib
import math
import numpy as np
import concourse.bass as bass
import concourse.mybir as mybir
from concourse.bass_utils import run_bass_kernel_spmd

F32 = mybir.dt.float32
BF16 = mybir.dt.bfloat16
I32 = mybir.dt.int32
ALU = mybir.AluOpType
AF = mybir.ActivationFunctionType
AX = mybir.AxisListType

D = 1024
NE = 32
DE = 512
NCORES = 8
DEPTH = 4
ALPHA = (2 * DEPTH) ** 0.25
LN_EPS = 1e-5
SUBLN_EPS = 1e-5
POOL_WINDOWS = (2, 4, 8, 16)
ENGINES = ("pe", "act", "dve", "pool", "sp")
EPOCH = 20000
SB_BASE = 16512
SB_TOP = 229344


class Op:
    __slots__ = ("eng", "fn", "waits", "sig", "is_dma", "semkey", "inc")


class Prog:
    def __init__(self, nc):
        self.nc = nc
        self.ops = []
        self.last_writer = {}
        self.readers = {}
        self.stack = contextlib.ExitStack()
        self.uid = 0
        self.sb_off = SB_BASE

    def sbuf(self, shape, dtype, name=None):
        self.uid += 1
        nbytes = int(np.prod(shape[1:])) * (4 if dtype in (F32, I32) else 2)
        off = (self.sb_off + 63) // 64 * 64
        assert off + nbytes <= SB_TOP, f"SBUF overflow allocating {name} {shape}: {off}+{nbytes}"
        self.sb_off = off + nbytes
        return self.nc.alloc_sbuf_tensor_at(f"s{self.uid}_" + (name or "t"), list(shape), dtype, offset=off)

    def reset_sbuf(self):
        self.sb_off = SB_BASE

    def fence(self):
        last = {}
        dmas = {}
        for o in self.ops:
            if o.fn is None:
                continue
            if o.is_dma:
                dmas[o.semkey] = o
            else:
                last[o.eng] = o
        targets = list(last.values()) + list(dmas.values())
        for t in targets:
            t.sig = True
        for eng in ENGINES:
            op = Op()
            op.eng, op.fn, op.is_dma, op.semkey, op.sig, op.inc = eng, None, False, None, False, 1
            op.waits = [t for t in targets if not (t.eng == eng and not t.is_dma)]
            self.ops.append(op)
        self.last_writer.clear()
        self.readers.clear()

    def psum(self, shape, dtype=F32, name=None):
        self.uid += 1
        return self.stack.enter_context(self.nc.psum_tensor("p_" + (name or f"ps{self.uid}"), list(shape), dtype))

    def _add(self, eng, fn, reads, writes, is_dma=False, semkey=None):
        op = Op()
        op.eng, op.fn, op.is_dma, op.semkey = eng, fn, is_dma, semkey
        op.sig = False
        op.inc = 16 if is_dma else 1
        excl = [k for k in reads if k.startswith("pb")]
        if excl:
            reads = [k for k in reads if not k.startswith("pb")]
            writes = list(writes) + [k for k in excl if k not in writes]
        deps = []
        for k in reads:
            w = self.last_writer.get(k)
            if w is not None:
                deps.append(w)
        for k in writes:
            w = self.last_writer.get(k)
            if w is not None:
                deps.append(w)
            deps.extend(self.readers.get(k, ()))
        seen = set()
        op.waits = []
        for d in deps:
            if id(d) in seen or d is op:
                continue
            seen.add(id(d))
            if d.eng == "pe" and eng == "pe" and not d.is_dma and not is_dma:
                continue
            op.waits.append(d)
            d.sig = True
        for k in reads:
            self.readers.setdefault(k, []).append(op)
        for k in writes:
            self.last_writer[k] = op
            self.readers[k] = []
        self.ops.append(op)
        return op

    def op(self, eng, fn, reads=(), writes=()):
        return self._add(eng, fn, reads, writes)

    def dma(self, eng, fn, reads=(), writes=(), semkey=None):
        o = self._add(eng, fn, reads, writes, is_dma=True, semkey=semkey)
        o.sig = True
        return o

    def coll(self, fn, reads, writes, semkey, inc=1):
        o = self._add("pool", fn, reads, writes, is_dma=True, semkey=semkey)
        o.sig = True
        o.inc = inc
        return o

    def mm(self, out, lhsT, rhs, start, stop, r, w):
        return self.op("pe", lambda e: e.matmul(out, lhsT, rhs, start=start, stop=stop), r, w)

    def tr(self, out, in_, ident, r, w):
        return self.op("pe", lambda e: e.transpose(out, in_, ident), r, w)

    def act(self, out, in_, func, r, w, bias=None, scale=1.0, accum=None):
        def f(e):
            kw = {}
            if bias is not None:
                kw["bias"] = bias
            if accum is not None:
                kw["accum_out"] = accum
            return e.activation(out=out, in_=in_, func=func, scale=scale, **kw)
        return self.op("act", f, r, w)

    def tt(self, eng, out, in0, in1, op, r, w):
        return self.op(eng, lambda e: e.tensor_tensor(out=out, in0=in0, in1=in1, op=op), r, w)

    def ts(self, eng, out, in0, s1, s2, op0, op1, r, w):
        if s2 is None:
            return self.op(eng, lambda e: e.tensor_scalar(out=out, in0=in0, scalar1=s1, scalar2=None, op0=op0), r, w)
        return self.op(eng, lambda e: e.tensor_scalar(out=out, in0=in0, scalar1=s1, scalar2=s2, op0=op0, op1=op1), r, w)

    def stt(self, out, in0, scalar, in1, op0, op1, r, w, accum=None):
        def f(e):
            if accum is not None:
                return e.scalar_tensor_tensor(out=out, in0=in0, scalar=scalar, in1=in1, op0=op0, op1=op1, accum_out=accum)
            return e.scalar_tensor_tensor(out=out, in0=in0, scalar=scalar, in1=in1, op0=op0, op1=op1)
        return self.op("dve", f, r, w)

    def cp(self, eng, out, in_, r, w):
        if eng == "act":
            return self.op("act", lambda e: e.copy(out=out, in_=in_), r, w)
        return self.op(eng, lambda e: e.tensor_copy(out=out, in_=in_), r, w)

    def red(self, out, in_, op, r, w):
        return self.op("dve", lambda e: e.tensor_reduce(out=out, in_=in_, axis=AX.X, op=op), r, w)

    def ld(self, eng, out, in_, w, semkey=None, r=()):
        return self.dma(eng, lambda e: e.dma_start(out=out, in_=in_), r, w, semkey=semkey or w[0])

    def st(self, eng, out, in_, r, semkey, w=()):
        return self.dma(eng, lambda e: e.dma_start(out=out, in_=in_), r, w, semkey=semkey)

    def emit(self, final_wait_ops=()):
        nc = self.nc
        sem_of = {}
        cnt = {}
        semnames = []
        eng_sig_count = {e: 0 for e in ENGINES}
        for o in self.ops:
            if not o.sig:
                continue
            if o.is_dma:
                name = "d_" + str(o.semkey)
                cnt[name] = cnt.get(name, 0) + o.inc
                sem_of[id(o)] = (name, cnt[name])
            else:
                n = eng_sig_count[o.eng]
                name = f"c_{o.eng}_{n // EPOCH}"
                eng_sig_count[o.eng] = n + 1
                sem_of[id(o)] = (name, n % EPOCH + 1)
            if name not in semnames:
                semnames.append(name)
        sems = {}
        for name in semnames:
            sems[name] = self.stack.enter_context(nc.semaphore(name))
        self.nsems = len(semnames)
        per_eng = {e: [] for e in ENGINES}
        for o in self.ops:
            per_eng[o.eng].append(o)
        finals = list(final_wait_ops)

        def run(engname, eng):
            waited = {}
            for o in per_eng[engname]:
                for d in o.waits:
                    name, val = sem_of[id(d)]
                    if waited.get(name, 0) >= val:
                        continue
                    waited[name] = val
                    eng.wait_ge(sems[name], val)
                if o.fn is None:
                    continue
                ins = o.fn(eng)
                if o.sig:
                    name, val = sem_of[id(o)]
                    ins.then_inc(sems[name], o.inc)
            if engname == "sp":
                for d in finals:
                    name, val = sem_of[id(d)]
                    if waited.get(name, 0) >= val:
                        continue
                    waited[name] = val
                    eng.wait_ge(sems[name], val)

        with nc.Block() as block:
            @block.tensor
            def _(e):
                run("pe", e)

            @block.scalar
            def _(e):
                run("act", e)

            @block.vector
            def _(e):
                run("dve", e)

            @block.gpsimd
            def _(e):
                run("pool", e)

            @block.sync
            def _(e):
                run("sp", e)
        self.stack.close()


def build_tail(kind, T, C, NEXP=NE):
    nc = bass.Bass("TRN2", target_bir_lowering=False)
    P = Prog(nc)
    NTL = T // 128
    NSB = C // 128
    inp = lambda name, shape: nc.dram_tensor(name, list(shape), F32, kind="ExternalInput")
    h_d = inp("h", [T, D])
    if kind == "attn":
        oT_d = inp("oT", [D, T])
        wo_d = inp("w_o", [D, D])
    else:
        halo_d = inp("halo", [128, D])
        am0_d = inp("am0", [4, 128, 128])
        amd_d = inp("amd", [4, 128, 128])
        amo_d = inp("amo", [4, 128, 128])
        win_d = inp("w_in", [D, D])
        wgrp_d = inp("w_grp", [4, 256, 256])
        lsT_d = inp("lsT", [128, 8])
        wout_d = inp("w_out", [D, D])
    lng_d = inp("ln_g", [2, D])
    lnb_d = inp("ln_b", [2, D])
    wrt_d = inp("w_rt", [D, 36])
    brt_d = inp("b_rt", [1, 36])
    wg_d = inp("w_gate", [NEXP, D, DE])
    wu_d = inp("w_up", [NEXP, D, DE])
    wd_d = inp("w_down", [NEXP, DE, D])
    ident_d = inp("ident", [128, 128])
    ustr_d = inp("ustrict", [128, 128])
    ecoff_d = inp("ecoff", [1, 32])
    out_d = nc.dram_tensor("out", [T, D], F32, kind="ExternalOutput")
    h1_d = nc.dram_tensor("h1_scr", [T, D], F32)
    xin_d = nc.dram_tensor("xin_scr", [NE * C, D], BF16)
    y_d = nc.dram_tensor("y_scr", [NE * C, D], F32)

    ident = P.sbuf([128, 128], F32, "ident")
    identb = P.sbuf([128, 128], BF16, "identb")
    ustr = P.sbuf([128, 128], BF16, "ustr")
    ones = P.sbuf([128, 128], BF16, "ones")
    ecoff = P.sbuf([128, 32], F32, "ecoff")
    brt = P.sbuf([128, 36], F32, "brt")
    wrt = P.sbuf([128, 8, 36], F32, "wrt")
    lng = P.sbuf([128, 2, D], F32, "lng")
    lnb = P.sbuf([128, 2, D], F32, "lnb")
    epst = P.sbuf([128, 1], F32, "epst")
    Scnt = P.sbuf([128, 32], BF16, "Scnt")
    gates = P.sbuf([128, NTL, 2], F32, "gates")
    dest = P.sbuf([128, NTL, 2], I32, "dest")

    P.ld("sp", ident[:], ident_d.ap(), ["ident"])
    P.ld("pool", identb[:], ident_d.ap(), ["identb"])
    P.ld("pool", ustr[:], ustr_d.ap(), ["ustr"])
    P.op("pool", lambda e: e.memset(ones[:], 1.0), (), ["ones"])
    P.op("pool", lambda e: e.memset(epst[:], LN_EPS), (), ["epst"])
    P.op("pool", lambda e: e.memset(Scnt[:], 0.0), (), ["Scnt"])
    P.ld("sp", ecoff[:], ecoff_d.ap().partition_broadcast(128), ["ecoff"])
    P.ld("sp", brt[:], brt_d.ap().partition_broadcast(128), ["brt"])
    P.ld("sp", wrt[:], wrt_d.ap().rearrange("(k p) n -> p k n", p=128), ["wrt"])
    for j in range(2):
        P.ld("sp", lng[:, j, :], lng_d.ap()[j:j + 1, :].partition_broadcast(128), [f"lng{j}"])
        P.ld("sp", lnb[:, j, :], lnb_d.ap()[j:j + 1, :].partition_broadcast(128), [f"lnb{j}"])

    zt = P.sbuf([128, D], BF16, "zt")
    P.op("pool", lambda e: e.memset(zt[:], 0.0), (), ["zt"])
    nz = (NE * C) // 128
    for z in range(nz):
        P.dma("sp", (lambda z: lambda e: e.dma_start(out=xin_d.ap()[z * 128:(z + 1) * 128, :], in_=zt[:]))(z), ["zt"], ["xinz"] if z == nz - 1 else [f"xinz{z}"], semkey="xinz")
    pb = [P.psum([128, 512], F32, f"pb{i}") for i in range(6)]
    pbb = [P.psum([128, 1024], BF16, f"pbb{i}") for i in range(2)]

    if kind == "attn":
        wo = P.sbuf([128, 8, D], BF16, "wo")
        P.ld("pool", wo[:], wo_d.ap().rearrange("(k p) n -> p k n", p=128), ["wo"])
        OCH = min(512, T)
        oT = [P.sbuf([128, 8, OCH], BF16, f"oT{i}") for i in range(2)]
    else:
        win = P.sbuf([128, 8, D], BF16, "win")
        wout = P.sbuf([128, 8, D], BF16, "wout")
        wgrp = P.sbuf([128, 4, 2, 256], BF16, "wgrp")
        lsT = P.sbuf([128, 8], F32, "lsT")
        am0 = P.sbuf([128, 4, 128], BF16, "am0")
        amd = P.sbuf([128, 4, 128], BF16, "amd")
        amo = P.sbuf([128, 4, 128], BF16, "amo")
        P.ld("pool", win[:], win_d.ap().rearrange("(k p) n -> p k n", p=128), ["win"])
        P.ld("pool", wout[:], wout_d.ap().rearrange("(k p) n -> p k n", p=128), ["wout"])
        P.ld("pool", wgrp[:], wgrp_d.ap().rearrange("g (k p) n -> p g k n", p=128), ["wgrp"])
        P.ld("sp", lsT[:], lsT_d.ap(), ["lsT"])
        P.ld("pool", am0[:], am0_d.ap().rearrange("w p n -> p w n"), ["am0"])
        P.ld("pool", amd[:], amd_d.ap().rearrange("w p n -> p w n"), ["amd"])
        P.ld("pool", amo[:], amo_d.ap().rearrange("w p n -> p w n"), ["amo"])
        ubuf = [P.sbuf([128, D], BF16, f"u{i}") for i in range(3)]
        hTb = P.sbuf([128, 8, 128], BF16, "hTb")
        pT = P.sbuf([128, 8, 128], BF16, "pT")
        qT = P.sbuf([128, 8, 128], BF16, "qT")

    htile = [P.sbuf([128, D], F32, f"ht{i}") for i in range(2)]
    rt = [P.sbuf([128, D], F32, f"rt{i}") for i in range(2)]
    h1b = [P.sbuf([128, D], BF16, f"h1b{i}") for i in range(2)]
    h1T = P.sbuf([128, 8, 128], F32, "h1T")
    stats = P.sbuf([128, 2, 6], F32, "stats")
    mv = P.sbuf([128, 2], F32, "mv")
    rstd = P.sbuf([128, 1], F32, "rstd")
    nmr = P.sbuf([128, 1], F32, "nmr")
    L = P.sbuf([128, 36], F32, "L")
    gmax = P.sbuf([128, 1], F32, "gmax")
    ngmax = P.sbuf([128, 1], F32, "ngmax")
    G1 = P.sbuf([128, 4], F32, "G1")
    ge = P.sbuf([128, 4], F32, "ge")
    gsum = P.sbuf([128, 1], F32, "gsum")
    pen = P.sbuf([128, 4], F32, "pen")
    Lm = P.sbuf([128, 32], F32, "Lm")
    Lm2 = P.sbuf([128, 32], F32, "Lm2")
    m1 = P.sbuf([128, 1], F32, "m1")
    m2 = P.sbuf([128, 1], F32, "m2")
    OH1 = P.sbuf([128, 32], F32, "OH1")
    OH2 = P.sbuf([128, 32], F32, "OH2")
    OHc = P.sbuf([128, 32], BF16, "OHc")
    dd = P.sbuf([128, 1], F32, "dd")
    ex = P.sbuf([128, 1], F32, "ex")
    den = P.sbuf([128, 1], F32, "den")
    Pf = P.sbuf([128, 32], F32, "Pf")
    tmp32 = P.sbuf([128, 32], F32, "tmp32")
    dflt = P.sbuf([128, 2], F32, "dflt")

    def layer_norm(src, j, dst, keys_r, key_w):
        sv = src.rearrange("p (c f) -> p c f", c=2)
        for c in range(2):
            P.op("dve", (lambda c: lambda e: e.bn_stats(out=stats[:, c, :], in_=sv[:, c, :]))(c), keys_r, [f"stats{c}"])
        P.op("dve", lambda e: e.bn_aggr(out=mv[:], in_=stats[:]), ["stats0", "stats1"], ["mv"])
        P.act(rstd[:], mv[:, 1:2], AF.Sqrt, ["mv", "epst"], ["rstd"], bias=epst[:, 0:1])
        P.op("dve", lambda e: e.reciprocal(out=rstd[:], in_=rstd[:]), ["rstd"], ["rstd"])
        P.stt(nmr[:], mv[:, 0:1], -1.0, rstd[:], ALU.mult, ALU.mult, ["mv", "rstd"], ["nmr"])
        P.act(dst, src, AF.Identity, list(keys_r) + ["rstd", "nmr"], [key_w], bias=nmr[:, 0:1], scale=rstd[:, 0:1])
        P.tt("dve", dst, dst, lng[:, j, :], ALU.mult, [key_w, f"lng{j}"], [key_w])
        P.tt("pool", dst, dst, lnb[:, j, :], ALU.add, [key_w, f"lnb{j}"], [key_w])

    def pool_u(i, slot, hslot):
        hs = htile[hslot]
        hk = f"ht{hslot}"
        if i < 0:
            P.ld("sp", hs[:], halo_d.ap(), [hk])
        for k in range(8):
            P.tr(pb[2 + k // 4][:, (k % 4) * 128:(k % 4 + 1) * 128], hs[:, k * 128:(k + 1) * 128], ident[:], [hk, "ident"], [f"pb{2 + k // 4}"])
        for hf in range(2):
            P.cp("act" if hf == 0 else "dve", hTb[:, hf * 4:(hf + 1) * 4, :], pb[2 + hf][:].rearrange("p (k t) -> p k t", k=4), [f"pb{2 + hf}"], [f"hTb{hf}"])
        for hf in range(2):
            for k in range(8):
                P.mm(pb[hf][:], hTb[:, k, :], win[:, k, hf * 512:(hf + 1) * 512], k == 0, k == 7, [f"hTb{k // 4}", "win"], [f"pb{hf}"])
        for hf in range(2):
            P.cp("act" if hf == 0 else "dve", ubuf[slot][:, hf * 512:(hf + 1) * 512], pb[hf][:], [f"pb{hf}"], [f"u{slot}_{hf}"])

    if kind == "pool":
        pool_u(-1, 2, 1)

    for i in range(NTL):
        s = i % 2
        hk = f"ht{s}"
        P.ld("sp", htile[s][:], h_d.ap()[i * 128:(i + 1) * 128, :], [hk])
        if kind == "attn":
            ch = (i * 128) // OCH
            if (i * 128) % OCH == 0:
                P.ld("pool", oT[ch % 2][:], oT_d.ap()[:, ch * OCH:(ch + 1) * OCH].rearrange("(k p) t -> p k t", p=128), [f"oT{ch % 2}"])
            off = i * 128 - ch * OCH
            for hf in range(2):
                for k in range(8):
                    P.mm(pb[hf][:], oT[ch % 2][:, k, off:off + 128], wo[:, k, hf * 512:(hf + 1) * 512], k == 0, k == 7, [f"oT{ch % 2}", "wo"], [f"pb{hf}"])
        else:
            us = i % 3
            up = (i - 1) % 3
            pool_u(i, us, s)
            A = am0 if i == 0 else amd
            Ak = "am0" if i == 0 else "amd"
            for j in range(8):
                g = j // 2
                o_ = pb[4 + j // 4][:, (j % 4) * 128:(j % 4 + 1) * 128]
                P.mm(o_, ubuf[us][:, j * 128:(j + 1) * 128], A[:, g, :], True, False, [f"u{us}_{j // 4}", Ak], [f"pb{4 + j // 4}"])
                P.mm(o_, ubuf[up][:, j * 128:(j + 1) * 128], amo[:, g, :], False, True, [f"u{up}_{j // 4}", "amo"], [f"pb{4 + j // 4}"])
            for hf in range(2):
                P.cp("act" if hf == 0 else "dve", pT[:, hf * 4:(hf + 1) * 4, :], pb[4 + hf][:].rearrange("p (k t) -> p k t", k=4), [f"pb{4 + hf}"], [f"pT{hf}"])
            for j in range(8):
                g = j // 2
                o_ = pb[4 + j // 4][:, (j % 4) * 128:(j % 4 + 1) * 128]
                for kk in range(2):
                    P.mm(o_, wgrp[:, g, kk, (j % 2) * 128:(j % 2 + 1) * 128], pT[:, 2 * g + kk, :], kk == 0, kk == 1, [f"pT{(2 * g + kk) // 4}", "wgrp"], [f"pb{4 + j // 4}"])
            for j in range(8):
                P.act(qT[:, j, :], pb[4 + j // 4][:, (j % 4) * 128:(j % 4 + 1) * 128], AF.Identity, [f"pb{4 + j // 4}", "lsT"], [f"qT{j}"], scale=lsT[:, j:j + 1])
            for hf in range(2):
                for k in range(8):
                    P.mm(pb[hf][:], qT[:, k, :], wout[:, k, hf * 512:(hf + 1) * 512], k == 0, k == 7, [f"qT{k}", "wout"], [f"pb{hf}"])
        r = rt[s]
        rk = f"rt{s}"
        for hf in range(2):
            P.stt(r[:, hf * 512:(hf + 1) * 512], htile[s][:, hf * 512:(hf + 1) * 512], ALPHA, pb[hf][:], ALU.mult, ALU.add, [hk, f"pb{hf}"], [rk])
        layer_norm(r[:], 0, r[:], [rk], rk)
        P.st("sp", h1_d.ap()[i * 128:(i + 1) * 128, :], r[:], [rk], f"h1st{s}", [f"h1d{i}"])
        P.cp("act", h1b[s][:], r[:], [rk], [f"h1b{s}"])
        for k in range(8):
            P.tr(pb[2 + k // 4][:, (k % 4) * 128:(k % 4 + 1) * 128], r[:, k * 128:(k + 1) * 128], ident[:], [rk, "ident"], [f"pb{2 + k // 4}"])
        for hf in range(2):
            P.cp("act" if hf == 0 else "dve", h1T[:, hf * 4:(hf + 1) * 4, :], pb[2 + hf][:].rearrange("p (k t) -> p k t", k=4), [f"pb{2 + hf}"], [f"h1T{hf}"])
        for k in range(8):
            P.mm(pb[4][:, 0:36], h1T[:, k, :], wrt[:, k, :], k == 0, k == 7, [f"h1T{k // 4}", "wrt"], ["pb4"])
        P.tt("dve", L[:], pb[4][:, 0:36], brt[:], ALU.add, ["pb4", "brt"], ["L"])
        P.red(gmax[:], L[:, 0:4], ALU.max, ["L"], ["gmax"])
        P.tt("dve", G1[:], L[:, 0:4], gmax[:, 0:1].to_broadcast([128, 4]), ALU.is_equal, ["L", "gmax"], ["G1"])
        P.ts("dve", ngmax[:], gmax[:], -1.0, None, ALU.mult, None, ["gmax"], ["ngmax"])
        P.act(ge[:], L[:, 0:4], AF.Exp, ["L", "ngmax"], ["ge", "gsum"], bias=ngmax[:, 0:1], accum=gsum[:, 0:1])
        P.ts("dve", pen[:], G1[:], -1.0, 1e30, ALU.add, ALU.mult, ["G1"], ["pen"])
        P.tt("dve", Lm[:].rearrange("p (g e) -> p g e", g=4), L[:, 4:36].rearrange("p (g e) -> p g e", g=4),
             pen[:].unsqueeze(2).to_broadcast([128, 4, 8]), ALU.add, ["L", "pen"], ["Lm"])
        P.red(m1[:], Lm[:], ALU.max, ["Lm"], ["m1"])
        P.tt("dve", OH1[:], Lm[:], m1[:, 0:1].to_broadcast([128, 32]), ALU.is_equal, ["Lm", "m1"], ["OH1"])
        P.stt(Lm2[:], OH1[:], -1e30, Lm[:], ALU.mult, ALU.add, ["OH1", "Lm"], ["Lm2"])
        P.red(m2[:], Lm2[:], ALU.max, ["Lm2"], ["m2"])
        P.tt("dve", OH2[:], Lm2[:], m2[:, 0:1].to_broadcast([128, 32]), ALU.is_equal, ["Lm2", "m2"], ["OH2"])
        P.tt("dve", dd[:], m2[:], m1[:], ALU.subtract, ["m1", "m2"], ["dd"])
        P.act(ex[:], dd[:], AF.Exp, ["dd"], ["ex"])
        P.stt(den[:], ex[:], 1.0, gsum[:], ALU.add, ALU.mult, ["ex", "gsum"], ["den"])
        P.op("dve", (lambda i: lambda e: e.reciprocal(out=gates[:, i, 0:1], in_=den[:]))(i), ["den"], [f"gate{i}"])
        P.tt("dve", gates[:, i, 1:2], gates[:, i, 0:1], ex[:], ALU.mult, [f"gate{i}", "ex"], [f"gate{i}"])
        P.tt("dve", OHc[:], OH1[:], OH2[:], ALU.add, ["OH1", "OH2"], ["OHc"])
        P.mm(pb[5][:, 0:32], ustr[:], OHc[:], True, False, ["ustr", "OHc"], ["pb5"])
        P.mm(pb[5][:, 0:32], ones[:], Scnt[:], False, True, ["ones", "Scnt"], ["pb5"])
        P.tt("dve", Pf[:], pb[5][:, 0:32], ecoff[:], ALU.add, ["pb5", "ecoff"], ["Pf"])
        P.tt("dve", Scnt[:], Scnt[:], OHc[:], ALU.add, ["Scnt", "OHc"], ["Scnt"])
        P.stt(tmp32[:], OH1[:], 1.0, Pf[:], ALU.mult, ALU.mult, ["OH1", "Pf"], ["tmp32", "dflt0"], accum=dflt[:, 0:1])
        P.stt(tmp32[:], OH2[:], 1.0, Pf[:], ALU.mult, ALU.mult, ["OH2", "Pf"], ["tmp32", "dflt1"], accum=dflt[:, 1:2])
        P.cp("dve", dest[:, i, :], dflt[:], ["dflt0", "dflt1"], [f"dest{i}"])
        for kk in range(2):
            P.dma("pool", (lambda i, kk, s: lambda e: e.indirect_dma_start(
                out=xin_d.ap(), out_offset=bass.IndirectOffsetOnAxis(ap=dest[:, i, kk:kk + 1], axis=0),
                in_=h1b[s][:], in_offset=None))(i, kk, s), [f"h1b{s}", f"dest{i}", "xinz"], [f"xinw{i}_{kk}"], semkey=f"scat{s}{kk}")

    wgs = [P.sbuf([128, 8, DE], BF16, f"wg{i}") for i in range(2)]
    wus = [P.sbuf([128, 8, DE], BF16, f"wu{i}") for i in range(2)]
    wds = [P.sbuf([128, 4, D], BF16, f"wd{i}") for i in range(2)]
    xin = [P.sbuf([128, NSB, D], BF16, f"xin{i}") for i in range(2)]
    xT = [P.sbuf([128, 8, C], BF16, f"xT{i}") for i in range(2)]
    actT = [P.sbuf([128, 4, C], BF16, f"actT{i}") for i in range(2)]
    sg = [P.sbuf([128, C], F32, f"sg{i}") for i in range(2)]
    yb = [P.sbuf([128, D], F32, f"yb{i}") for i in range(2)]
    ycount = 0
    xin_keys = [f"xinw{i}_{kk}" for i in range(NTL) for kk in range(2)]
    y_keys = [f"y_{e_}_{sb}" for e_ in range(NEXP) for sb in range(NSB)]
    for e_ in range(NEXP):
        s = e_ % 2
        P.ld("pool", wgs[s][:], wg_d.ap()[e_].rearrange("(k p) n -> p k n", p=128), [f"wg{s}"])
        P.ld("pool", wus[s][:], wu_d.ap()[e_].rearrange("(k p) n -> p k n", p=128), [f"wu{s}"])
        P.ld("pool", wds[s][:], wd_d.ap()[e_].rearrange("(k p) n -> p k n", p=128), [f"wd{s}"])
        P.ld("sp", xin[s][:], xin_d.ap()[e_ * C:(e_ + 1) * C, :].rearrange("(b p) n -> p b n", p=128), [f"xin{s}"], r=xin_keys)
        for sb in range(NSB):
            pbt = pbb[sb % 2]
            for k in range(8):
                P.tr(pbt[:, k * 128:(k + 1) * 128], xin[s][:, sb, k * 128:(k + 1) * 128], identb[:], [f"xin{s}", "identb"], [f"pbb{sb % 2}"])
            P.cp("dve" if sb % 2 else "act", xT[s][:, :, sb * 128:(sb + 1) * 128], pbt[:].rearrange("p (k t) -> p k t", k=8), [f"pbb{sb % 2}"], [f"xT{s}_{sb}"])
        xkeys = [f"xT{s}_{sb}" for sb in range(NSB)]
        for fc in range(4):
            pg = pb[(fc % 2) * 2]
            pu = pb[(fc % 2) * 2 + 1]
            kg, ku = f"pb{(fc % 2) * 2}", f"pb{(fc % 2) * 2 + 1}"
            for k in range(8):
                P.mm(pg[:, 0:C], wgs[s][:, k, fc * 128:(fc + 1) * 128], xT[s][:, k, :], k == 0, k == 7, xkeys + [f"wg{s}"], [kg])
            for k in range(8):
                P.mm(pu[:, 0:C], wus[s][:, k, fc * 128:(fc + 1) * 128], xT[s][:, k, :], k == 0, k == 7, xkeys + [f"wu{s}"], [ku])
            P.act(sg[fc % 2][:], pg[:, 0:C], AF.Silu, [kg], [f"sg{fc % 2}"])
            P.tt("dve", actT[s][:, fc, :], sg[fc % 2][:], pu[:, 0:C], ALU.mult, [f"sg{fc % 2}", ku], [f"actT{s}_{fc}"])
        akeys = [f"actT{s}_{fc}" for fc in range(4)]
        for sb in range(NSB):
            ys = ycount % 2
            ycount += 1
            for hf in range(2):
                py = pb[4 + hf]
                for fc in range(4):
                    P.mm(py[:], actT[s][:, fc, sb * 128:(sb + 1) * 128], wds[s][:, fc, hf * 512:(hf + 1) * 512], fc == 0, fc == 3, akeys + [f"wd{s}"], [f"pb{4 + hf}"])
                P.cp("act" if hf == 0 else "dve", yb[ys][:, hf * 512:(hf + 1) * 512], py[:], [f"pb{4 + hf}"], [f"yb{ys}"])
            r0 = e_ * C + sb * 128
            P.st("sp", y_d.ap()[r0:r0 + 128, :], yb[ys][:], [f"yb{ys}"], f"yst{ys}", [f"y_{e_}_{sb}"])

    y0 = [P.sbuf([128, D], F32, f"y0_{i}") for i in range(2)]
    y1 = [P.sbuf([128, D], F32, f"y1_{i}") for i in range(2)]
    outs = []
    for i in range(NTL):
        s = i % 2
        hk = f"ht{s}"
        P.ld("sp", htile[s][:], h1_d.ap()[i * 128:(i + 1) * 128, :], [hk], r=[f"h1d{i}"])
        for kk, yt in ((0, y0), (1, y1)):
            P.dma("pool", (lambda i, kk, yt, s: lambda e: e.indirect_dma_start(
                out=yt[s][:], out_offset=None, in_=y_d.ap(),
                in_offset=bass.IndirectOffsetOnAxis(ap=dest[:, i, kk:kk + 1], axis=0)))(i, kk, yt, s),
                y_keys + [f"dest{i}"], [f"y{kk}_{s}"], semkey=f"gath{kk}{s}")
        r = rt[s]
        rk = f"rt{s}"
        P.ts("pool", r[:], htile[s][:], ALPHA, None, ALU.mult, None, [hk], [rk])
        P.stt(r[:], y0[s][:], gates[:, i, 0:1], r[:], ALU.mult, ALU.add, [f"y0_{s}", f"gate{i}", rk], [rk])
        P.stt(r[:], y1[s][:], gates[:, i, 1:2], r[:], ALU.mult, ALU.add, [f"y1_{s}", f"gate{i}", rk], [rk])
        layer_norm(r[:], 1, r[:], [rk], rk)
        outs.append(P.st("sp", out_d.ap()[i * 128:(i + 1) * 128, :], r[:], [rk], f"ost{s}"))
    P.emit(final_wait_ops=outs)
    return nc


def host_consts(C):
    ident = np.eye(128, dtype=np.float32)
    ustrict = (np.arange(128)[:, None] < np.arange(128)[None, :]).astype(np.float32)
    ecoff = (np.arange(32, dtype=np.float32) * C).reshape(1, 32)
    return dict(ident=ident, ustrict=ustrict, ecoff=ecoff)


def pool_consts(first):
    am0 = np.zeros((4, 128, 128), np.float32)
    amd = np.zeros((4, 128, 128), np.float32)
    amo = np.zeros((4, 128, 128), np.float32)
    sp = np.arange(128)[:, None]
    s = np.arange(128)[None, :]
    for g, w in enumerate(POOL_WINDOWS):
        band = ((sp <= s) & (sp > s - w)).astype(np.float32)
        amd[g] = band / w - np.eye(128, dtype=np.float32)
        cnt = np.minimum(s + 1, w).astype(np.float32)
        am0[g] = band / cnt - np.eye(128, dtype=np.float32)
        amo[g] = ((sp - 128 > s - w)).astype(np.float32) / w
    return dict(am0=am0 if first else amd.copy(), amd=amd, amo=amo)


def build_attn(S, dbg=0):
    nc = bass.Bass("TRN2", target_bir_lowering=False)
    P = Prog(nc)
    NT = S // 128
    NCH = S // 512
    inp = lambda name, shape: nc.dram_tensor(name, list(shape), F32, kind="ExternalInput")
    xT_d = inp("xT", [D, S])
    wq_d = inp("wq", [D, 512])
    wk_d = inp("wk", [D, 512])
    wv_d = inp("wv", [D, 512])
    cs_d = inp("cs", [128, (S // 128) * 16])
    lq1_d = inp("lq1", [1, 64])
    lk1_d = inp("lk1", [1, 64])
    lq2_d = inp("lq2", [1, 64])
    lk2_d = inp("lk2", [1, 64])
    linit_d = inp("linit", [1, 1])
    subg_d = inp("subg", [1, 128])
    ident_d = inp("ident", [128, 128])
    tri_d = inp("trimask", [128, 128])
    o_d = nc.dram_tensor("o", [S, 512], F32, kind="ExternalOutput")

    identb = P.sbuf([128, 128], BF16, "identb")
    trib = P.sbuf([128, 128], BF16, "trib")
    cs = P.sbuf([128, NT, 16], F32, "cs")
    lqk = P.sbuf([128, 4, 64], F32, "lqk")
    linit = P.sbuf([128, 1], F32, "linit")
    subg = P.sbuf([128, 128], F32, "subg")
    subgs = P.sbuf([128, 128], F32, "subgs")
    junk64 = P.sbuf([128, 64], F32, "junk64")
    ssum = P.sbuf([128, 2], F32, "ssum")
    esum = P.sbuf([128, 2], F32, "esum")
    nlam = P.sbuf([128, 1], F32, "nlam")
    om = P.sbuf([128, 1], F32, "om")
    epst = P.sbuf([128, 1], F32, "epst")
    P.ld("pool", identb[:], ident_d.ap(), ["identb"])
    P.ld("pool", trib[:], tri_d.ap(), ["trib"])
    P.ld("sp", cs[:].rearrange("p t c -> p (t c)"), cs_d.ap(), ["cs"])
    for n, dd_ in enumerate((lq1_d, lk1_d, lq2_d, lk2_d)):
        P.ld("sp", lqk[:, n, :], dd_.ap().partition_broadcast(128), [f"lqk{n}"])
    P.ld("sp", linit[:], linit_d.ap().partition_broadcast(128), ["linit"])
    P.ld("sp", subg[:], subg_d.ap().partition_broadcast(128), ["subg"])
    P.op("pool", lambda e: e.memset(epst[:], SUBLN_EPS), (), ["epst"])
    P.stt(junk64[:], lqk[:, 0, :], 1.0, lqk[:, 1, :], ALU.mult, ALU.mult, ["lqk0", "lqk1"], ["junk64", "ssum0"], accum=ssum[:, 0:1])
    P.stt(junk64[:], lqk[:, 2, :], 1.0, lqk[:, 3, :], ALU.mult, ALU.mult, ["lqk2", "lqk3"], ["junk64", "ssum1"], accum=ssum[:, 1:2])
    P.act(esum[:], ssum[:], AF.Exp, ["ssum0", "ssum1"], ["esum"])
    P.tt("dve", nlam[:], esum[:, 1:2], esum[:, 0:1], ALU.subtract, ["esum"], ["nlam"])
    P.tt("dve", nlam[:], nlam[:], linit[:], ALU.subtract, ["nlam", "linit"], ["nlam"])
    P.ts("dve", om[:], linit[:], -1.0, 1.0, ALU.mult, ALU.add, ["linit"], ["om"])
    P.ts("dve", subgs[:], subg[:], om[:, 0:1], None, ALU.mult, None, ["subg", "om"], ["subgs"])

    pb = [P.psum([128, 512], F32, f"pb{i}") for i in range(7)]
    pbb = P.psum([128, 1024], BF16, "pbb")

    QKT = P.sbuf([128, 4, S], BF16, "QKT")
    Vaug = P.sbuf([128, NT, 2, 129], BF16, "Vaug")
    wq = P.sbuf([128, 8, 256], BF16, "wq")
    wk = P.sbuf([128, 8, 256], BF16, "wk")
    wv = P.sbuf([128, 8, 256], BF16, "wv")
    xTb = [P.sbuf([128, 8, 512], BF16, f"xT{i}") for i in range(2)]
    qksb = [P.sbuf([128, 512], BF16, f"qksb{i}") for i in range(2)]
    tA = P.sbuf([128, 8, 8], F32, "tA")
    tB = P.sbuf([128, 8, 8], F32, "tB")
    ET = [[P.sbuf([128, 512], BF16, f"ET{m}_{i}") for i in range(3)] for m in range(2)]
    ocp = [P.sbuf([128, 3, 512], F32, f"ocp{i}") for i in range(2)]
    rl = P.sbuf([128, 2], F32, "rl")
    t0 = P.sbuf([128, 128], F32, "t0")
    av = P.sbuf([128, 128], F32, "av")
    junk = P.sbuf([128, 128], F32, "junk")
    ss = P.sbuf([128, 1], F32, "ss")
    rstd = P.sbuf([128, 1], F32, "rstd")
    ot = [P.sbuf([128, 128], F32, f"ot{i}") for i in range(2)]
    P.op("pool", lambda e: e.memset(Vaug[:], 1.0), (), ["Vaug_init"])
    outs = []
    ocount = [0]

    for hp in range(2):
        if dbg == 4:
            break
        for w_, wd_, nm in ((wq, wq_d, "wq"), (wk, wk_d, "wk"), (wv, wv_d, "wv")):
            P.ld("pool", w_[:], wd_.ap()[:, hp * 256:(hp + 1) * 256].rearrange("(k p) n -> p k n", p=128), [nm])
        for tt in range(NT):
            c = tt // 4
            if tt % 4 == 0:
                P.ld("pool", xTb[c % 2][:], xT_d.ap()[:, c * 512:(c + 1) * 512].rearrange("(k p) t -> p k t", p=128), [f"xT{c % 2}"])
            xs = xTb[c % 2]
            xk = f"xT{c % 2}"
            off = (tt % 4) * 128
            s = tt % 2
            pqk = pb[s]
            pv = pb[2 + s]
            for k in range(8):
                P.mm(pqk[:, 0:256], xs[:, k, off:off + 128], wq[:, k, :], k == 0, False, [xk, "wq"], [f"pb{s}"])
            for k in range(8):
                P.mm(pqk[:, 256:512], xs[:, k, off:off + 128], wk[:, k, :], False, k == 7, [xk, "wk"], [f"pb{s}"])
            for k in range(8):
                P.mm(pv[:, 0:256], xs[:, k, off:off + 128], wv[:, k, :], k == 0, k == 7, [xk, "wv"], [f"pb{2 + s}"])
            if dbg == 5:
                continue
            qv = pqk[:].rearrange("p (g d) -> p g d", g=8)
            qs_ = qksb[s][:].rearrange("p (g d) -> p g d", g=8)
            P.cp("act", qs_[:, :, 16:64], qv[:, :, 16:64], [f"pb{s}"], [f"qkrest{s}"])
            if dbg == 7:
                continue
            cosb = cs[:, tt, 0:8].unsqueeze(1).to_broadcast([128, 8, 8])
            sinb = cs[:, tt, 8:16].unsqueeze(1).to_broadcast([128, 8, 8])
            P.tt("dve", tA[:], qv[:, :, 0:8], cosb, ALU.mult, [f"pb{s}", "cs"], ["tA"])
            P.tt("dve", tB[:], qv[:, :, 8:16], sinb, ALU.mult, [f"pb{s}", "cs"], ["tB"])
            if dbg == 8:
                continue
            P.tt("dve", qs_[:, :, 0:8], tA[:], tB[:], ALU.subtract, ["tA", "tB"], [f"qkrot{s}a"])
            P.tt("dve", tA[:], qv[:, :, 0:8], sinb, ALU.mult, [f"pb{s}", "cs"], ["tA"])
            P.tt("dve", tB[:], qv[:, :, 8:16], cosb, ALU.mult, [f"pb{s}", "cs"], ["tB"])
            P.tt("dve", qs_[:, :, 8:16], tA[:], tB[:], ALU.add, ["tA", "tB"], [f"qkrot{s}b"])
            if dbg == 6:
                continue
            for blk in range(4):
                P.tr(pbb[:, s * 512 + blk * 128:s * 512 + (blk + 1) * 128], qksb[s][:, blk * 128:(blk + 1) * 128], identb[:],
                     [f"qkrest{s}", f"qkrot{s}a", f"qkrot{s}b", "identb"], [f"pbb{s}"])
            P.cp("dve" if s else "act", QKT[:, :, tt * 128:(tt + 1) * 128], pbb[:, s * 512:(s + 1) * 512].rearrange("p (b t) -> p b t", b=4),
                 [f"pbb{s}"], [f"QKT{tt}"])
            P.cp("act" if s else "dve", Vaug[:, tt, :, 0:128], pv[:, 0:256].rearrange("p (h d) -> p h d", h=2),
                 [f"pb{2 + s}", "Vaug_init"], [f"V{tt}"])

        if dbg == 1:
            break
        steps = [(hh, j, kt) for hh in range(2) for j in range(NCH) for kt in range(4 * j + 4)]

        def emit_qk(n):
            hh, j, kt = steps[n]
            d = kt - 4 * j
            qs = 128 * max(d, 0)
            for m in range(2):
                bank = (n % 2) * 2 + m
                pS = pb[bank]
                rd = [f"QKT{t}" for t in range(4 * j + qs // 128, 4 * j + 4)] + [f"QKT{kt}"]
                P.mm(pS[:, qs:512], QKT[m * 64:(m + 1) * 64, 2 + hh, kt * 128:(kt + 1) * 128],
                     QKT[m * 64:(m + 1) * 64, hh, j * 512 + qs:(j + 1) * 512], True, d < 0, rd, [f"pb{bank}"])
                if d >= 0:
                    P.mm(pS[:, qs:qs + 128], identb[:], trib[:], False, True, ["identb", "trib"], [f"pb{bank}"])

        def emit_exp(n):
            hh, j, kt = steps[n]
            d = kt - 4 * j
            qs = 128 * max(d, 0)
            for m in range(2):
                bank = (n % 2) * 2 + m
                P.act(ET[m][n % 3][:, qs:512], pb[bank][:, qs:512], AF.Exp, [f"pb{bank}"], [f"ET{m}_{n % 3}"], scale=0.125)

        def emit_pv(n):
            hh, j, kt = steps[n]
            d = kt - 4 * j
            for t in range(8):
                m, qsub = t // 4, t % 4
                if qsub < d:
                    continue
                bank = 4 + t // 3
                col = (t % 3) * 129
                first = (kt == 0) and (t % 3 == 0)
                P.op("pe", (lambda bank, col, m, n, qsub, kt, hh, first: lambda e: e.matmul(
                    pb[bank][:, col:col + 129], ET[m][n % 3][:, qsub * 128:(qsub + 1) * 128], Vaug[:, kt, hh, :],
                    start=first, stop=False, skip_group_check=True))(bank, col, m, n, qsub, kt, hh, first),
                    [f"ET{m}_{n % 3}", f"V{kt}"], [f"pb{bank}"])

        def emit_final(n):
            hh, j, kt = steps[n]
            oc = ocp[ocount[0] % 2]
            ock = f"ocp{ocount[0] % 2}"
            ocount[0] += 1
            for b3 in range(3):
                ncol = 387 if b3 < 2 else 258
                P.cp("act" if b3 == 1 else "dve", oc[:, b3, 0:ncol], pb[4 + b3][:, 0:ncol], [f"pb{4 + b3}"], [f"{ock}_{b3}"])
            for qsub in range(4):
                t0_, t1_ = qsub, 4 + qsub
                O0 = oc[:, t0_ // 3, (t0_ % 3) * 129:(t0_ % 3) * 129 + 129]
                O1 = oc[:, t1_ // 3, (t1_ % 3) * 129:(t1_ % 3) * 129 + 129]
                k0, k1 = f"{ock}_{t0_ // 3}", f"{ock}_{t1_ // 3}"
                P.op("dve", (lambda O0: lambda e: e.reciprocal(out=rl[:, 0:1], in_=O0[:, 128:129]))(O0), [k0], ["rl0"])
                P.op("dve", (lambda O1: lambda e: e.reciprocal(out=rl[:, 1:2], in_=O1[:, 128:129]))(O1), [k1], ["rl1"])
                P.tt("dve", rl[:, 1:2], rl[:, 1:2], nlam[:], ALU.mult, ["rl1", "nlam"], ["rl1"])
                P.ts("dve", t0[:], O0[:, 0:128], rl[:, 0:1], None, ALU.mult, None, [k0, "rl0"], ["t0"])
                P.stt(av[:], O1[:, 0:128], rl[:, 1:2], t0[:], ALU.mult, ALU.add, [k1, "rl1", "t0"], ["av"])
                P.stt(junk[:], av[:], 1.0, av[:], ALU.mult, ALU.mult, ["av"], ["junk", "ss"], accum=ss[:, 0:1])
                P.act(rstd[:], ss[:], AF.Sqrt, ["ss", "epst"], ["rstd"], bias=epst[:, 0:1], scale=1.0 / 128.0)
                P.op("dve", lambda e: e.reciprocal(out=rstd[:], in_=rstd[:]), ["rstd"], ["rstd"])
                osl = (j * 4 + qsub) % 2
                P.stt(ot[osl][:], av[:], rstd[:, 0:1], subgs[:], ALU.mult, ALU.mult, ["av", "rstd", "subgs"], [f"ot{osl}"])
                r0 = (j * 4 + qsub) * 128
                hcol = (hp * 2 + hh) * 128
                outs.append(P.st("sp", o_d.ap()[r0:r0 + 128, hcol:hcol + 128], ot[osl][:], [f"ot{osl}"], f"ost{osl}"))

        nsteps = len(steps)
        emit_qk(0)
        for n in range(nsteps):
            emit_exp(n)
            if n + 1 < nsteps:
                emit_qk(n + 1)
            if dbg != 2:
                emit_pv(n)
            hh, j, kt = steps[n]
            if kt == 4 * j + 3 and dbg not in (2, 3):
                emit_final(n)
    P.emit(final_wait_ops=outs)
    return nc


def attn_consts(S):
    inv = (500000.0 ** (-np.arange(0, 16, 2, dtype=np.float32) / 16.0)).astype(np.float32)
    ang = np.arange(S, dtype=np.float32)[:, None] * inv[None, :]
    cs = np.concatenate([np.cos(ang), np.sin(ang)], axis=1).astype(np.float32)
    cs = np.ascontiguousarray(cs.reshape(S // 128, 128, 16).transpose(1, 0, 2).reshape(128, (S // 128) * 16))
    k = np.arange(128)[:, None]
    q = np.arange(128)[None, :]
    tri = np.where(q >= k, 0.0, -30000.0).astype(np.float32)
    return dict(cs=cs, trimask=tri, ident=np.eye(128, dtype=np.float32))


def bfv(pbank):
    return pbank[:].bitcast(BF16)


def attn_phase(P, nc, pb, S, src_d, src_row, wq_a, wk_a, wv_a, cs_d, lqk_a, linit_a, subg_a, ident_d, tri_d, o_d):
    P.reset_sbuf()
    NT = S // 128
    NCH = S // 512
    identb = P.sbuf([128, 128], BF16, "identb")
    trib = P.sbuf([128, 128], BF16, "trib")
    cs = P.sbuf([128, NT, 16], F32, "cs")
    lqk = P.sbuf([128, 4, 64], F32, "lqk")
    linit = P.sbuf([128, 1], F32, "linit")
    subg = P.sbuf([128, 128], F32, "subg")
    subgs = P.sbuf([128, 128], F32, "subgs")
    junk64 = P.sbuf([128, 64], F32, "junk64")
    ssum = P.sbuf([128, 2], F32, "ssum")
    esum = P.sbuf([128, 2], F32, "esum")
    nlam = P.sbuf([128, 1], F32, "nlam")
    om = P.sbuf([128, 1], F32, "om")
    epst = P.sbuf([128, 1], F32, "epst")
    P.ld("pool", identb[:], ident_d.ap(), ["identb"])
    P.ld("pool", trib[:], tri_d.ap(), ["trib"])
    P.ld("sp", cs[:].rearrange("p t c -> p (t c)"), cs_d.ap(), ["cs"])
    for n in range(4):
        P.ld("sp", lqk[:, n, :], lqk_a[n:n + 1, :].partition_broadcast(128), [f"lqk{n}"])
    P.ld("sp", linit[:], linit_a.partition_broadcast(128), ["linit"])
    P.ld("sp", subg[:], subg_a.partition_broadcast(128), ["subg"])
    P.op("pool", lambda e: e.memset(epst[:], SUBLN_EPS), (), ["epst"])
    P.stt(junk64[:], lqk[:, 0, :], 1.0, lqk[:, 1, :], ALU.mult, ALU.mult, ["lqk0", "lqk1"], ["junk64", "ssum0"], accum=ssum[:, 0:1])
    P.stt(junk64[:], lqk[:, 2, :], 1.0, lqk[:, 3, :], ALU.mult, ALU.mult, ["lqk2", "lqk3"], ["junk64", "ssum1"], accum=ssum[:, 1:2])
    P.act(esum[:], ssum[:], AF.Exp, ["ssum0", "ssum1"], ["esum"])
    P.tt("dve", nlam[:], esum[:, 1:2], esum[:, 0:1], ALU.subtract, ["esum"], ["nlam"])
    P.tt("dve", nlam[:], nlam[:], linit[:], ALU.subtract, ["nlam", "linit"], ["nlam"])
    P.ts("dve", om[:], linit[:], -1.0, 1.0, ALU.mult, ALU.add, ["linit"], ["om"])
    P.ts("dve", subgs[:], subg[:], om[:, 0:1], None, ALU.mult, None, ["subg", "om"], ["subgs"])

    QKT = P.sbuf([128, 4, S], BF16, "QKT")
    Vaug = P.sbuf([128, NT, 2, 129], BF16, "Vaug")
    wq = P.sbuf([128, 8, 256], BF16, "wq")
    wk = P.sbuf([128, 8, 256], BF16, "wk")
    wv = P.sbuf([128, 8, 256], BF16, "wv")
    xt = [P.sbuf([128, D], BF16, f"xt{i}") for i in range(2)]
    xTt = [P.sbuf([128, 8, 128], BF16, f"xTt{i}") for i in range(2)]
    qksb = [P.sbuf([128, 512], BF16, f"qksb{i}") for i in range(2)]
    tA = P.sbuf([128, 8, 8], F32, "tA")
    tB = P.sbuf([128, 8, 8], F32, "tB")
    ET = [[P.sbuf([128, 512], BF16, f"ET{m}_{i}") for i in range(3)] for m in range(2)]
    ocp = [P.sbuf([128, 3, 512], F32, f"ocp{i}") for i in range(2)]
    rl = P.sbuf([128, 2], F32, "rl")
    t0 = P.sbuf([128, 128], F32, "t0")
    av = P.sbuf([128, 128], F32, "av")
    junk = P.sbuf([128, 128], F32, "junk")
    ss = P.sbuf([128, 1], F32, "ss")
    rstd = P.sbuf([128, 1], F32, "rstd")
    ot = [P.sbuf([128, 128], F32, f"ot{i}") for i in range(2)]
    P.op("pool", lambda e: e.memset(Vaug[:], 1.0), (), ["Vaug_init"])
    ocount = [0]
    pbbq = bfv(pb[7])
    pbbx = bfv(pb[6])

    for hp in range(2):
        for w_, wa_, nm in ((wq, wq_a, "wq"), (wk, wk_a, "wk"), (wv, wv_a, "wv")):
            P.ld("pool", w_[:], wa_[:, hp * 256:(hp + 1) * 256].rearrange("(k p) n -> p k n", p=128), [nm])
        for tt in range(NT):
            s = tt % 2
            P.ld("pool", xt[s][:], src_d.ap()[src_row(tt):src_row(tt) + 128, :], [f"xt{s}"])
            for k in range(8):
                P.tr(pbbx[:, k * 128:(k + 1) * 128], xt[s][:, k * 128:(k + 1) * 128], identb[:], [f"xt{s}", "identb"], ["pb6"])
            P.cp("act", xTt[s][:, 0:4, :], pbbx[:, 0:512].rearrange("p (k t) -> p k t", k=4), ["pb6"], [f"xTt{s}a"])
            P.cp("dve", xTt[s][:, 4:8, :], pbbx[:, 512:1024].rearrange("p (k t) -> p k t", k=4), ["pb6"], [f"xTt{s}b"])
            xk = [f"xTt{s}a", f"xTt{s}b"]
            pqk = pb[s]
            pv = pb[2 + s]
            for k in range(8):
                P.mm(pqk[:, 0:256], xTt[s][:, k, :], wq[:, k, :], k == 0, False, xk + ["wq"], [f"pb{s}"])
            for k in range(8):
                P.mm(pqk[:, 256:512], xTt[s][:, k, :], wk[:, k, :], False, k == 7, xk + ["wk"], [f"pb{s}"])
            for k in range(8):
                P.mm(pv[:, 0:256], xTt[s][:, k, :], wv[:, k, :], k == 0, k == 7, xk + ["wv"], [f"pb{2 + s}"])
            qv = pqk[:].rearrange("p (g d) -> p g d", g=8)
            qs_ = qksb[s][:].rearrange("p (g d) -> p g d", g=8)
            P.cp("act", qs_[:, :, 16:64], qv[:, :, 16:64], [f"pb{s}"], [f"qkrest{s}"])
            cosb = cs[:, tt, 0:8].unsqueeze(1).to_broadcast([128, 8, 8])
            sinb = cs[:, tt, 8:16].unsqueeze(1).to_broadcast([128, 8, 8])
            P.tt("dve", tA[:], qv[:, :, 0:8], cosb, ALU.mult, [f"pb{s}", "cs"], ["tA"])
            P.tt("dve", tB[:], qv[:, :, 8:16], sinb, ALU.mult, [f"pb{s}", "cs"], ["tB"])
            P.tt("dve", qs_[:, :, 0:8], tA[:], tB[:], ALU.subtract, ["tA", "tB"], [f"qkrot{s}a"])
            P.tt("dve", tA[:], qv[:, :, 0:8], sinb, ALU.mult, [f"pb{s}", "cs"], ["tA"])
            P.tt("dve", tB[:], qv[:, :, 8:16], cosb, ALU.mult, [f"pb{s}", "cs"], ["tB"])
            P.tt("dve", qs_[:, :, 8:16], tA[:], tB[:], ALU.add, ["tA", "tB"], [f"qkrot{s}b"])
            for blk in range(4):
                P.tr(pbbq[:, s * 512 + blk * 128:s * 512 + (blk + 1) * 128], qksb[s][:, blk * 128:(blk + 1) * 128], identb[:],
                     [f"qkrest{s}", f"qkrot{s}a", f"qkrot{s}b", "identb"], ["pb7"])
            P.cp("dve" if s else "act", QKT[:, :, tt * 128:(tt + 1) * 128], pbbq[:, s * 512:(s + 1) * 512].rearrange("p (b t) -> p b t", b=4),
                 ["pb7"], [f"QKT{tt}"])
            P.cp("act" if s else "dve", Vaug[:, tt, :, 0:128], pv[:, 0:256].rearrange("p (h d) -> p h d", h=2),
                 [f"pb{2 + s}", "Vaug_init"], [f"V{tt}"])

        steps = [(hh, j, kt) for hh in range(2) for j in range(NCH) for kt in range(4 * j + 4)]

        def emit_qk(n):
            hh, j, kt = steps[n]
            d = kt - 4 * j
            qs = 128 * max(d, 0)
            for m in range(2):
                bank = (n % 2) * 2 + m
                pS = pb[bank]
                rd = [f"QKT{t}" for t in range(4 * j + qs // 128, 4 * j + 4)] + [f"QKT{kt}"]
                P.mm(pS[:, qs:512], QKT[m * 64:(m + 1) * 64, 2 + hh, kt * 128:(kt + 1) * 128],
                     QKT[m * 64:(m + 1) * 64, hh, j * 512 + qs:(j + 1) * 512], True, d < 0, rd, [f"pb{bank}"])
                if d >= 0:
                    P.mm(pS[:, qs:qs + 128], identb[:], trib[:], False, True, ["identb", "trib"], [f"pb{bank}"])

        def emit_exp(n):
            hh, j, kt = steps[n]
            d = kt - 4 * j
            qs = 128 * max(d, 0)
            for m in range(2):
                bank = (n % 2) * 2 + m
                P.act(ET[m][n % 3][:, qs:512], pb[bank][:, qs:512], AF.Exp, [f"pb{bank}"], [f"ET{m}_{n % 3}"], scale=0.125)

        def emit_pv(n):
            hh, j, kt = steps[n]
            d = kt - 4 * j
            for t in range(8):
                m, qsub = t // 4, t % 4
                if qsub < d:
                    continue
                bank = 4 + t // 3
                col = (t % 3) * 129
                first = (kt == 0) and (t % 3 == 0)
                P.op("pe", (lambda bank, col, m, n, qsub, kt, hh, first: lambda e: e.matmul(
                    pb[bank][:, col:col + 129], ET[m][n % 3][:, qsub * 128:(qsub + 1) * 128], Vaug[:, kt, hh, :],
                    start=first, stop=False, skip_group_check=True))(bank, col, m, n, qsub, kt, hh, first),
                    [f"ET{m}_{n % 3}", f"V{kt}"], [f"pb{bank}"])

        def emit_final(n):
            hh, j, kt = steps[n]
            oc = ocp[ocount[0] % 2]
            ock = f"ocp{ocount[0] % 2}"
            ocount[0] += 1
            for b3 in range(3):
                ncol = 387 if b3 < 2 else 258
                P.cp("act" if b3 == 1 else "dve", oc[:, b3, 0:ncol], pb[4 + b3][:, 0:ncol], [f"pb{4 + b3}"], [f"{ock}_{b3}"])
            for qsub in range(4):
                t0_, t1_ = qsub, 4 + qsub
                O0 = oc[:, t0_ // 3, (t0_ % 3) * 129:(t0_ % 3) * 129 + 129]
                O1 = oc[:, t1_ // 3, (t1_ % 3) * 129:(t1_ % 3) * 129 + 129]
                k0, k1 = f"{ock}_{t0_ // 3}", f"{ock}_{t1_ // 3}"
                P.op("dve", (lambda O0: lambda e: e.reciprocal(out=rl[:, 0:1], in_=O0[:, 128:129]))(O0), [k0], ["rl0"])
                P.op("dve", (lambda O1: lambda e: e.reciprocal(out=rl[:, 1:2], in_=O1[:, 128:129]))(O1), [k1], ["rl1"])
                P.tt("dve", rl[:, 1:2], rl[:, 1:2], nlam[:], ALU.mult, ["rl1", "nlam"], ["rl1"])
                P.ts("dve", t0[:], O0[:, 0:128], rl[:, 0:1], None, ALU.mult, None, [k0, "rl0"], ["t0"])
                P.stt(av[:], O1[:, 0:128], rl[:, 1:2], t0[:], ALU.mult, ALU.add, [k1, "rl1", "t0"], ["av"])
                P.stt(junk[:], av[:], 1.0, av[:], ALU.mult, ALU.mult, ["av"], ["junk", "ss"], accum=ss[:, 0:1])
                P.act(rstd[:], ss[:], AF.Sqrt, ["ss", "epst"], ["rstd"], bias=epst[:, 0:1], scale=1.0 / 128.0)
                P.op("dve", lambda e: e.reciprocal(out=rstd[:], in_=rstd[:]), ["rstd"], ["rstd"])
                osl = (j * 4 + qsub) % 2
                P.stt(ot[osl][:], av[:], rstd[:, 0:1], subgs[:], ALU.mult, ALU.mult, ["av", "rstd", "subgs"], [f"ot{osl}"])
                r0 = (j * 4 + qsub) * 128
                hcol = (hp * 2 + hh) * 128
                P.st("sp", o_d.ap()[r0:r0 + 128, hcol:hcol + 128], ot[osl][:], [f"ot{osl}"], f"ost{osl}")

        nsteps = len(steps)
        emit_qk(0)
        for n in range(nsteps):
            emit_exp(n)
            if n + 1 < nsteps:
                emit_qk(n + 1)
            emit_pv(n)
            hh, j, kt = steps[n]
            if kt == 4 * j + 3:
                emit_final(n)


def tail_phase(P, nc, pb, kind, T, C, NEXP, hsrc_fn, hdst_fn, W, scr, final):
    P.reset_sbuf()
    NTL = T // 128
    NSB = C // 128
    h1_d, xin_d, y_d = scr["h1"], scr["xin"], scr["y"]
    ident = P.sbuf([128, 128], F32, "ident")
    identb = P.sbuf([128, 128], BF16, "identb")
    ustr = P.sbuf([128, 128], BF16, "ustr")
    ones = P.sbuf([128, 128], BF16, "ones")
    ecoff = P.sbuf([128, 32], F32, "ecoff")
    brt = P.sbuf([128, 36], F32, "brt")
    wrt = P.sbuf([128, 8, 36], F32, "wrt")
    lng = P.sbuf([128, 2, D], F32, "lng")
    lnb = P.sbuf([128, 2, D], F32, "lnb")
    epst = P.sbuf([128, 1], F32, "epst")
    Scnt = P.sbuf([128, 32], BF16, "Scnt")
    gates = P.sbuf([128, NTL, 2], F32, "gates")
    dest = P.sbuf([128, NTL, 2], I32, "dest")
    P.ld("sp", ident[:], W["ident"], ["ident"])
    P.ld("pool", identb[:], W["ident"], ["identb"])
    P.ld("pool", ustr[:], W["ustrict"], ["ustr"])
    P.op("pool", lambda e: e.memset(ones[:], 1.0), (), ["ones"])
    P.op("pool", lambda e: e.memset(epst[:], LN_EPS), (), ["epst"])
    P.op("pool", lambda e: e.memset(Scnt[:], 0.0), (), ["Scnt"])
    P.ld("sp", ecoff[:], W["ecoff"].partition_broadcast(128), ["ecoff"])
    P.ld("sp", brt[:], W["b_rt"].partition_broadcast(128), ["brt"])
    P.ld("sp", wrt[:], W["w_rt"].rearrange("(k p) n -> p k n", p=128), ["wrt"])
    for j in range(2):
        P.ld("sp", lng[:, j, :], W["ln_g"][j:j + 1, :].partition_broadcast(128), [f"lng{j}"])
        P.ld("sp", lnb[:, j, :], W["ln_b"][j:j + 1, :].partition_broadcast(128), [f"lnb{j}"])
    zt = P.sbuf([128, D], BF16, "zt")
    P.op("pool", lambda e: e.memset(zt[:], 0.0), (), ["zt"])
    nz = (NE * C) // 128
    for z in range(nz):
        P.dma("sp", (lambda z: lambda e: e.dma_start(out=xin_d.ap()[z * 128:(z + 1) * 128, :], in_=zt[:]))(z), ["zt"], ["xinz"] if z == nz - 1 else [f"xinz{z}"], semkey="xinz")
    pbb = bfv(pb[6])
    pbb2 = bfv(pb[7])
    tcount = [0]

    if kind == "attn":
        wo = P.sbuf([128, 8, D], BF16, "wo")
        P.ld("pool", wo[:], W["w_o"].rearrange("(k p) n -> p k n", p=128), ["wo"])
        oidx = P.sbuf([128, NTL * 2], I32, "oidx")
        P.ld("sp", oidx[:], W["oidx"], ["oidx"])
        og = [P.sbuf([128, D], F32, f"og{i}") for i in range(2)]
        ob = P.sbuf([128, D], BF16, "ob")
        oTt = P.sbuf([128, 8, 128], BF16, "oTt")
    else:
        win = P.sbuf([128, 8, D], BF16, "win")
        wout = P.sbuf([128, 8, D], BF16, "wout")
        wgrp = P.sbuf([128, 4, 2, 256], BF16, "wgrp")
        lsT = P.sbuf([128, 8], F32, "lsT")
        am0 = P.sbuf([128, 4, 128], BF16, "am0")
        amd = P.sbuf([128, 4, 128], BF16, "amd")
        amo = P.sbuf([128, 4, 128], BF16, "amo")
        hidx = P.sbuf([128, 1], I32, "hidx")
        P.ld("sp", hidx[:], W["hidx"], ["hidx"])
        P.ld("pool", win[:], W["w_in"].rearrange("(k p) n -> p k n", p=128), ["win"])
        P.ld("pool", wout[:], W["w_out"].rearrange("(k p) n -> p k n", p=128), ["wout"])
        P.ld("pool", wgrp[:], W["w_grp"].rearrange("g (k p) n -> p g k n", p=128), ["wgrp"])
        P.ld("sp", lsT[:], W["lsT"], ["lsT"])
        P.ld("pool", am0[:], W["am0"].rearrange("w p n -> p w n"), ["am0"])
        P.ld("pool", amd[:], W["amd"].rearrange("w p n -> p w n"), ["amd"])
        P.ld("pool", amo[:], W["amo"].rearrange("w p n -> p w n"), ["amo"])
        ubuf = [P.sbuf([128, D], BF16, f"u{i}") for i in range(3)]
        hTb = P.sbuf([128, 8, 128], BF16, "hTb")
        pT = P.sbuf([128, 8, 128], BF16, "pT")
        qT = P.sbuf([128, 8, 128], BF16, "qT")

    htile = [P.sbuf([128, D], F32, f"ht{i}") for i in range(2)]
    rt = [P.sbuf([128, D], F32, f"rt{i}") for i in range(2)]
    h1b = [P.sbuf([128, D], BF16, f"h1b{i}") for i in range(2)]
    h1T = P.sbuf([128, 8, 128], F32, "h1T")
    stats = P.sbuf([128, 2, 6], F32, "stats")
    mv = P.sbuf([128, 2], F32, "mv")
    rstd = P.sbuf([128, 1], F32, "rstd")
    nmr = P.sbuf([128, 1], F32, "nmr")
    L = P.sbuf([128, 36], F32, "L")
    gmax = P.sbuf([128, 1], F32, "gmax")
    ngmax = P.sbuf([128, 1], F32, "ngmax")
    G1 = P.sbuf([128, 4], F32, "G1")
    ge = P.sbuf([128, 4], F32, "ge")
    gsum = P.sbuf([128, 1], F32, "gsum")
    pen = P.sbuf([128, 4], F32, "pen")
    Lm = P.sbuf([128, 32], F32, "Lm")
    Lm2 = P.sbuf([128, 32], F32, "Lm2")
    m1 = P.sbuf([128, 1], F32, "m1")
    m2 = P.sbuf([128, 1], F32, "m2")
    OH1 = P.sbuf([128, 32], F32, "OH1")
    OH2 = P.sbuf([128, 32], F32, "OH2")
    OHc = P.sbuf([128, 32], BF16, "OHc")
    dd = P.sbuf([128, 1], F32, "dd")
    ex = P.sbuf([128, 1], F32, "ex")
    den = P.sbuf([128, 1], F32, "den")
    Pf = P.sbuf([128, 32], F32, "Pf")
    tmp32 = P.sbuf([128, 32], F32, "tmp32")
    dflt = P.sbuf([128, 2], F32, "dflt")

    def layer_norm(src, j, dst, keys_r, key_w):
        sv = src.rearrange("p (c f) -> p c f", c=2)
        for c in range(2):
            P.op("dve", (lambda c: lambda e: e.bn_stats(out=stats[:, c, :], in_=sv[:, c, :]))(c), keys_r, [f"stats{c}"])
        P.op("dve", lambda e: e.bn_aggr(out=mv[:], in_=stats[:]), ["stats0", "stats1"], ["mv"])
        P.act(rstd[:], mv[:, 1:2], AF.Sqrt, ["mv", "epst"], ["rstd"], bias=epst[:, 0:1])
        P.op("dve", lambda e: e.reciprocal(out=rstd[:], in_=rstd[:]), ["rstd"], ["rstd"])
        P.stt(nmr[:], mv[:, 0:1], -1.0, rstd[:], ALU.mult, ALU.mult, ["mv", "rstd"], ["nmr"])
        P.act(dst, src, AF.Identity, list(keys_r) + ["rstd", "nmr"], [key_w], bias=nmr[:, 0:1], scale=rstd[:, 0:1])
        P.tt("dve", dst, dst, lng[:, j, :], ALU.mult, [key_w, f"lng{j}"], [key_w])
        P.tt("dve", dst, dst, lnb[:, j, :], ALU.add, [key_w, f"lnb{j}"], [key_w])

    def pool_u(i, slot, hslot):
        hs = htile[hslot]
        hk = f"ht{hslot}"
        if i < 0:
            P.dma("pool", lambda e: e.indirect_dma_start(out=hs[:], out_offset=None, in_=scr["halo_all"].ap(),
                                                         in_offset=bass.IndirectOffsetOnAxis(ap=hidx[:, 0:1], axis=0)),
                  ["hidx"], [hk], semkey=hk)
        for k in range(8):
            P.tr(pb[2 + k // 4][:, (k % 4) * 128:(k % 4 + 1) * 128], hs[:, k * 128:(k + 1) * 128], ident[:], [hk, "ident"], [f"pb{2 + k // 4}"])
        for hf in range(2):
            P.cp("act" if hf == 0 else "dve", hTb[:, hf * 4:(hf + 1) * 4, :], pb[2 + hf][:].rearrange("p (k t) -> p k t", k=4), [f"pb{2 + hf}"], [f"hTb{hf}"])
        for hf in range(2):
            for k in range(8):
                P.mm(pb[hf][:], hTb[:, k, :], win[:, k, hf * 512:(hf + 1) * 512], k == 0, k == 7, [f"hTb{k // 4}", "win"], [f"pb{hf}"])
        for hf in range(2):
            P.cp("act" if hf == 0 else "dve", ubuf[slot][:, hf * 512:(hf + 1) * 512], pb[hf][:], [f"pb{hf}"], [f"u{slot}_{hf}"])

    if kind == "pool":
        pool_u(-1, 2, 1)

    def s1_loads(i):
        s = i % 2
        P.ld("sp", htile[s][:], hsrc_fn(i), [f"ht{s}"])
        if kind == "attn":
            for g in range(2):
                P.dma("pool", (lambda i, g, s: lambda e: e.indirect_dma_start(
                    out=og[s][:, g * 512:(g + 1) * 512], out_offset=None, in_=scr["o_all"].ap(),
                    in_offset=bass.IndirectOffsetOnAxis(ap=oidx[:, i * 2 + g:i * 2 + g + 1], axis=0)))(i, g, s),
                    ["oidx"], [f"og{s}_{g}"], semkey=f"og{s}_{g}")

    s1_loads(0)
    for i in range(NTL):
        s = i % 2
        hk = f"ht{s}"
        if i + 1 < NTL:
            s1_loads(i + 1)
        if kind == "attn":
            P.cp("act", ob[:], og[s][:], [f"og{s}_0", f"og{s}_1"], ["ob"])
            for k in range(8):
                P.tr(pbb[:, k * 128:(k + 1) * 128], ob[:, k * 128:(k + 1) * 128], identb[:], ["ob", "identb"], ["pb6"])
            P.cp("dve", oTt[:].rearrange("p k t -> p (k t)"), pbb, ["pb6"], ["oTt"])
            for hf in range(2):
                for k in range(8):
                    P.mm(pb[hf][:], oTt[:, k, :], wo[:, k, hf * 512:(hf + 1) * 512], k == 0, k == 7, ["oTt", "wo"], [f"pb{hf}"])
        else:
            us = i % 3
            up = (i - 1) % 3
            pool_u(i, us, s)
            A = am0 if i == 0 else amd
            Ak = "am0" if i == 0 else "amd"
            for j in range(8):
                g = j // 2
                o_ = pb[4 + j // 4][:, (j % 4) * 128:(j % 4 + 1) * 128]
                P.mm(o_, ubuf[us][:, j * 128:(j + 1) * 128], A[:, g, :], True, False, [f"u{us}_{j // 4}", Ak], [f"pb{4 + j // 4}"])
                P.mm(o_, ubuf[up][:, j * 128:(j + 1) * 128], amo[:, g, :], False, True, [f"u{up}_{j // 4}", "amo"], [f"pb{4 + j // 4}"])
            for hf in range(2):
                P.cp("act" if hf == 0 else "dve", pT[:, hf * 4:(hf + 1) * 4, :], pb[4 + hf][:].rearrange("p (k t) -> p k t", k=4), [f"pb{4 + hf}"], [f"pT{hf}"])
            for j in range(8):
                g = j // 2
                o_ = pb[4 + j // 4][:, (j % 4) * 128:(j % 4 + 1) * 128]
                for kk in range(2):
                    P.mm(o_, wgrp[:, g, kk, (j % 2) * 128:(j % 2 + 1) * 128], pT[:, 2 * g + kk, :], kk == 0, kk == 1, [f"pT{(2 * g + kk) // 4}", "wgrp"], [f"pb{4 + j // 4}"])
            for j in range(8):
                P.act(qT[:, j, :], pb[4 + j // 4][:, (j % 4) * 128:(j % 4 + 1) * 128], AF.Identity, [f"pb{4 + j // 4}", "lsT"], [f"qT{j}"], scale=lsT[:, j:j + 1])
            for hf in range(2):
                for k in range(8):
                    P.mm(pb[hf][:], qT[:, k, :], wout[:, k, hf * 512:(hf + 1) * 512], k == 0, k == 7, [f"qT{k}", "wout"], [f"pb{hf}"])
        r = rt[s]
        rk = f"rt{s}"
        for hf in range(2):
            P.stt(r[:, hf * 512:(hf + 1) * 512], htile[s][:, hf * 512:(hf + 1) * 512], ALPHA, pb[hf][:], ALU.mult, ALU.add, [hk, f"pb{hf}"], [rk])
        layer_norm(r[:], 0, r[:], [rk], rk)
        P.st("sp", h1_d.ap()[i * 128:(i + 1) * 128, :], r[:], [rk], f"h1st{s}", [f"h1d{i}"])
        P.cp("act", h1b[s][:], r[:], [rk], [f"h1b{s}"])
        for k in range(8):
            P.tr(pb[2 + k // 4][:, (k % 4) * 128:(k % 4 + 1) * 128], r[:, k * 128:(k + 1) * 128], ident[:], [rk, "ident"], [f"pb{2 + k // 4}"])
        for hf in range(2):
            P.cp("act" if hf == 0 else "dve", h1T[:, hf * 4:(hf + 1) * 4, :], pb[2 + hf][:].rearrange("p (k t) -> p k t", k=4), [f"pb{2 + hf}"], [f"h1T{hf}"])
        for k in range(8):
            P.mm(pb[4][:, 0:36], h1T[:, k, :], wrt[:, k, :], k == 0, k == 7, [f"h1T{k // 4}", "wrt"], ["pb4"])
        P.tt("dve", L[:], pb[4][:, 0:36], brt[:], ALU.add, ["pb4", "brt"], ["L"])
        P.red(gmax[:], L[:, 0:4], ALU.max, ["L"], ["gmax"])
        P.tt("dve", G1[:], L[:, 0:4], gmax[:, 0:1].to_broadcast([128, 4]), ALU.is_equal, ["L", "gmax"], ["G1"])
        P.ts("dve", ngmax[:], gmax[:], -1.0, None, ALU.mult, None, ["gmax"], ["ngmax"])
        P.act(ge[:], L[:, 0:4], AF.Exp, ["L", "ngmax"], ["ge", "gsum"], bias=ngmax[:, 0:1], accum=gsum[:, 0:1])
        P.ts("dve", pen[:], G1[:], -1.0, 1e30, ALU.add, ALU.mult, ["G1"], ["pen"])
        P.tt("dve", Lm[:].rearrange("p (g e) -> p g e", g=4), L[:, 4:36].rearrange("p (g e) -> p g e", g=4),
             pen[:].unsqueeze(2).to_broadcast([128, 4, 8]), ALU.add, ["L", "pen"], ["Lm"])
        P.red(m1[:], Lm[:], ALU.max, ["Lm"], ["m1"])
        P.tt("dve", OH1[:], Lm[:], m1[:, 0:1].to_broadcast([128, 32]), ALU.is_equal, ["Lm", "m1"], ["OH1"])
        P.stt(Lm2[:], OH1[:], -1e30, Lm[:], ALU.mult, ALU.add, ["OH1", "Lm"], ["Lm2"])
        P.red(m2[:], Lm2[:], ALU.max, ["Lm2"], ["m2"])
        P.tt("dve", OH2[:], Lm2[:], m2[:, 0:1].to_broadcast([128, 32]), ALU.is_equal, ["Lm2", "m2"], ["OH2"])
        P.tt("dve", dd[:], m2[:], m1[:], ALU.subtract, ["m1", "m2"], ["dd"])
        P.act(ex[:], dd[:], AF.Exp, ["dd"], ["ex"])
        P.stt(den[:], ex[:], 1.0, gsum[:], ALU.add, ALU.mult, ["ex", "gsum"], ["den"])
        P.op("dve", (lambda i: lambda e: e.reciprocal(out=gates[:, i, 0:1], in_=den[:]))(i), ["den"], [f"gate{i}"])
        P.tt("dve", gates[:, i, 1:2], gates[:, i, 0:1], ex[:], ALU.mult, [f"gate{i}", "ex"], [f"gate{i}"])
        P.tt("dve", OHc[:], OH1[:], OH2[:], ALU.add, ["OH1", "OH2"], ["OHc"])
        P.mm(pb[5][:, 0:32], ustr[:], OHc[:], True, False, ["ustr", "OHc"], ["pb5"])
        P.mm(pb[5][:, 0:32], ones[:], Scnt[:], False, True, ["ones", "Scnt"], ["pb5"])
        P.tt("dve", Pf[:], pb[5][:, 0:32], ecoff[:], ALU.add, ["pb5", "ecoff"], ["Pf"])
        P.tt("dve", Scnt[:], Scnt[:], OHc[:], ALU.add, ["Scnt", "OHc"], ["Scnt"])
        P.stt(tmp32[:], OH1[:], 1.0, Pf[:], ALU.mult, ALU.mult, ["OH1", "Pf"], ["tmp32", "dflt0"], accum=dflt[:, 0:1])
        P.stt(tmp32[:], OH2[:], 1.0, Pf[:], ALU.mult, ALU.mult, ["OH2", "Pf"], ["tmp32", "dflt1"], accum=dflt[:, 1:2])
        P.cp("dve", dest[:, i, :], dflt[:], ["dflt0", "dflt1"], [f"dest{i}"])
        for kk in range(2):
            P.dma("pool", (lambda i, kk, s: lambda e: e.indirect_dma_start(
                out=xin_d.ap(), out_offset=bass.IndirectOffsetOnAxis(ap=dest[:, i, kk:kk + 1], axis=0),
                in_=h1b[s][:], in_offset=None))(i, kk, s), [f"h1b{s}", f"dest{i}", "xinz"], [f"xinw{i}_{kk}"], semkey=f"scat{s}{kk}")

    wgs = [P.sbuf([128, 8, DE], BF16, f"wg{i}") for i in range(2)]
    wus = [P.sbuf([128, 8, DE], BF16, f"wu{i}") for i in range(2)]
    wds = [P.sbuf([128, 4, D], BF16, f"wd{i}") for i in range(2)]
    xin = [P.sbuf([128, NSB, D], BF16, f"xin{i}") for i in range(2)]
    xT = [P.sbuf([128, 8, C], BF16, f"xT{i}") for i in range(2)]
    actT = [P.sbuf([128, 4, C], BF16, f"actT{i}") for i in range(2)]
    sg = [P.sbuf([128, C], F32, f"sg{i}") for i in range(2)]
    yb = [P.sbuf([128, D], F32, f"yb{i}") for i in range(2)]
    ycount = 0
    xin_keys = [f"xinw{i}_{kk}" for i in range(NTL) for kk in range(2)]
    y_keys = [f"y_{e_}_{sb}" for e_ in range(NEXP) for sb in range(NSB)]
    for e_ in range(NEXP):
        s = e_ % 2
        P.ld("pool", wgs[s][:], W["w_gate"][e_].rearrange("(k p) n -> p k n", p=128), [f"wg{s}"])
        P.ld("pool", wus[s][:], W["w_up"][e_].rearrange("(k p) n -> p k n", p=128), [f"wu{s}"])
        P.ld("pool", wds[s][:], W["w_down"][e_].rearrange("(k p) n -> p k n", p=128), [f"wd{s}"])
        if e_ == 0:
            P.ld("sp", xin[0][:], xin_d.ap()[0:C, :].rearrange("(b p) n -> p b n", p=128), ["xin0"], r=xin_keys)
        if e_ + 1 < NEXP:
            P.ld("sp", xin[1 - s][:], xin_d.ap()[(e_ + 1) * C:(e_ + 2) * C, :].rearrange("(b p) n -> p b n", p=128), [f"xin{1 - s}"], r=xin_keys)
        for sb in range(NSB):
            tcount[0] += 1
            pbt = pbb if tcount[0] % 2 else pbb2
            ptk = "pb6" if tcount[0] % 2 else "pb7"
            for k in range(8):
                P.tr(pbt[:, k * 128:(k + 1) * 128], xin[s][:, sb, k * 128:(k + 1) * 128], identb[:], [f"xin{s}", "identb"], [ptk])
            P.cp("dve" if sb % 2 else "act", xT[s][:, :, sb * 128:(sb + 1) * 128], pbt.rearrange("p (k t) -> p k t", k=8), [ptk], [f"xT{s}_{sb}"])
        xkeys = [f"xT{s}_{sb}" for sb in range(NSB)]
        for fc in range(4):
            pg = pb[(fc % 2) * 2]
            pu = pb[(fc % 2) * 2 + 1]
            kg, ku = f"pb{(fc % 2) * 2}", f"pb{(fc % 2) * 2 + 1}"
            for k in range(8):
                P.mm(pg[:, 0:C], wgs[s][:, k, fc * 128:(fc + 1) * 128], xT[s][:, k, :], k == 0, k == 7, xkeys + [f"wg{s}"], [kg])
            for k in range(8):
                P.mm(pu[:, 0:C], wus[s][:, k, fc * 128:(fc + 1) * 128], xT[s][:, k, :], k == 0, k == 7, xkeys + [f"wu{s}"], [ku])
            P.act(sg[fc % 2][:], pg[:, 0:C], AF.Silu, [kg], [f"sg{fc % 2}"])
            P.tt("dve", actT[s][:, fc, :], sg[fc % 2][:], pu[:, 0:C], ALU.mult, [f"sg{fc % 2}", ku], [f"actT{s}_{fc}"])
        akeys = [f"actT{s}_{fc}" for fc in range(4)]
        for sb in range(NSB):
            ys = ycount % 2
            ycount += 1
            for hf in range(2):
                py = pb[4 + hf]
                for fc in range(4):
                    P.mm(py[:], actT[s][:, fc, sb * 128:(sb + 1) * 128], wds[s][:, fc, hf * 512:(hf + 1) * 512], fc == 0, fc == 3, akeys + [f"wd{s}"], [f"pb{4 + hf}"])
                P.cp("act" if hf == 0 else "dve", yb[ys][:, hf * 512:(hf + 1) * 512], py[:], [f"pb{4 + hf}"], [f"yb{ys}"])
            r0 = e_ * C + sb * 128
            P.st("sp", y_d.ap()[r0:r0 + 128, :], yb[ys][:], [f"yb{ys}"], f"yst{ys}", [f"y_{e_}_{sb}"])

    y0 = [P.sbuf([128, D], F32, f"y0_{i}") for i in range(2)]
    y1 = [P.sbuf([128, D], F32, f"y1_{i}") for i in range(2)]
    outs = []
    def s3_loads(i):
        s = i % 2
        P.ld("sp", htile[s][:], h1_d.ap()[i * 128:(i + 1) * 128, :], [f"ht{s}"], r=[f"h1d{i}"])
        for kk, yt in ((0, y0), (1, y1)):
            P.dma("pool", (lambda i, kk, yt, s: lambda e: e.indirect_dma_start(
                out=yt[s][:], out_offset=None, in_=y_d.ap(),
                in_offset=bass.IndirectOffsetOnAxis(ap=dest[:, i, kk:kk + 1], axis=0)))(i, kk, yt, s),
                y_keys + [f"dest{i}"], [f"y{kk}_{s}"], semkey=f"gath{kk}{s}")

    s3_loads(0)
    for i in range(NTL):
        s = i % 2
        hk = f"ht{s}"
        if i + 1 < NTL:
            s3_loads(i + 1)
        r = rt[s]
        rk = f"rt{s}"
        P.act(r[:], htile[s][:], AF.Identity, [hk], [rk], scale=ALPHA)
        P.stt(r[:], y0[s][:], gates[:, i, 0:1], r[:], ALU.mult, ALU.add, [f"y0_{s}", f"gate{i}", rk], [rk])
        P.stt(r[:], y1[s][:], gates[:, i, 1:2], r[:], ALU.mult, ALU.add, [f"y1_{s}", f"gate{i}", rk], [rk])
        layer_norm(r[:], 1, r[:], [rk], rk)
        outs.append(P.st("sp", hdst_fn(i), r[:], [rk], f"ost{s}"))
    return outs


def build_mega(S, C, depth=DEPTH, NEXP=NE):
    T = S // 2
    NTL = T // 128
    nc = bass.Bass("TRN2", target_bir_lowering=False)
    P = Prog(nc)
    NA = (depth + 1) // 2
    NP = depth // 2
    inp = lambda name, shape, dt=F32: nc.dram_tensor(name, list(shape), dt, kind="ExternalInput")
    x_all = inp("x_all", [S, D])
    h0 = inp("h0", [T, D])
    oidx_d = inp("oidx", [128, NTL * 2], I32)
    hidx_d = inp("hidx", [128, 1], I32)
    wq_d = inp("wq", [NA, D, 512])
    wk_d = inp("wk", [NA, D, 512])
    wv_d = inp("wv", [NA, D, 512])
    wo_d = inp("w_o", [NA, D, D])
    lqk_d = inp("lqk", [NA, 4, 64])
    linit_d = inp("linit", [NA, 1])
    subg_d = inp("subg", [NA, 128])
    cs_d = inp("cs", [128, (S // 128) * 16])
    tri_d = inp("trimask", [128, 128])
    ident_d = inp("ident", [128, 128])
    ustr_d = inp("ustrict", [128, 128])
    ecoff_d = inp("ecoff", [1, 32])
    if NP:
        win_d = inp("w_in", [NP, D, D])
        wgrp_d = inp("w_grp", [NP, 4, 256, 256])
        lsT_d = inp("lsT", [NP, 128, 8])
        wout_d = inp("w_out", [NP, D, D])
        am0_d = inp("am0", [4, 128, 128])
        amd_d = inp("amd", [4, 128, 128])
        amo_d = inp("amo", [4, 128, 128])
    lng_d = inp("ln_g", [depth, 2, D])
    lnb_d = inp("ln_b", [depth, 2, D])
    wrt_d = inp("w_rt", [depth, D, 36])
    brt_d = inp("b_rt", [depth, 36])
    wg_d = inp("w_gate", [depth, NEXP, D, DE])
    wu_d = inp("w_up", [depth, NEXP, D, DE])
    wd_d = inp("w_down", [depth, NEXP, DE, D])
    out_d = nc.dram_tensor("out", [T, D], F32, kind="ExternalOutput")
    o_loc = nc.dram_tensor("o_loc", [S, 512], F32)
    o_all = nc.dram_tensor("o_all", [2 * S, 512], F32)
    hcur = nc.dram_tensor("hcur", [T, D], F32)
    h_all = nc.dram_tensor("h_all", [S, D], F32)
    halo_all = nc.dram_tensor("halo_all", [384, D], F32)
    scr = dict(h1=nc.dram_tensor("h1_scr", [T, D], F32), xin=nc.dram_tensor("xin_scr", [NE * C, D], BF16),
               y=nc.dram_tensor("y_scr", [NE * C, D], F32), o_all=o_all, halo_all=halo_all)
    pb = [P.psum([128, 512], F32, f"pb{i}") for i in range(8)]
    groups = [[0, 1], [2, 3], [4, 5], [6, 7]]
    CH_O = min(1024, S)
    CH_H = min(512, T)

    zf = P.sbuf([128, D], F32, "zf")
    P.op("pool", lambda e: e.memset(zf[:], 0.0), (), ["zf"])
    P.st("sp", halo_all.ap()[256:384, :], zf[:], ["zf"], "hz")
    P.fence()

    outs = []
    for i in range(depth):
        j = i // 2
        last = (i == depth - 1)
        W = dict(ident=ident_d.ap(), ustrict=ustr_d.ap(), ecoff=ecoff_d.ap(), b_rt=brt_d.ap()[i:i + 1, :], w_rt=wrt_d.ap()[i],
                 ln_g=lng_d.ap()[i], ln_b=lnb_d.ap()[i], w_gate=wg_d.ap()[i], w_up=wu_d.ap()[i], w_down=wd_d.ap()[i])
        hsrc = (lambda t: h0.ap()[t * 128:(t + 1) * 128, :]) if i == 0 else (lambda t: hcur.ap()[t * 128:(t + 1) * 128, :])
        hdst = (lambda t: out_d.ap()[t * 128:(t + 1) * 128, :]) if last else (lambda t: hcur.ap()[t * 128:(t + 1) * 128, :])
        if i % 2 == 0:
            if i == 0:
                src = x_all
                src_row = lambda tt: tt * 128
            else:
                for q in range(T // CH_H):
                    P.coll((lambda q: lambda e: e.collective_compute(
                        "AllGather", ALU.bypass, replica_groups=groups, ins=[hcur.ap()[q * CH_H:(q + 1) * CH_H, :]],
                        outs=[h_all.ap()[q * 2 * CH_H:(q + 1) * 2 * CH_H, :]]))(q), [], [], "cc")
                P.fence()
                src = h_all

                def src_row(tt):
                    r_, l_ = divmod(tt * 128, T)
                    return (l_ // CH_H) * 2 * CH_H + r_ * CH_H + l_ % CH_H
            attn_phase(P, nc, pb, S, src, src_row, wq_d.ap()[j], wk_d.ap()[j], wv_d.ap()[j], cs_d, lqk_d.ap()[j], linit_d.ap()[j:j + 1, :],
                       subg_d.ap()[j:j + 1, :], ident_d, tri_d, o_loc)
            P.fence()
            for q in range(S // CH_O):
                P.coll((lambda q: lambda e: e.collective_compute(
                    "AllGather", ALU.bypass, replica_groups=groups, ins=[o_loc.ap()[q * CH_O:(q + 1) * CH_O, :]],
                    outs=[o_all.ap()[q * 2 * CH_O:(q + 1) * 2 * CH_O, :]]))(q), [], [], "cc")
            P.fence()
            W.update(w_o=wo_d.ap()[j], oidx=oidx_d.ap())
            outs = tail_phase(P, nc, pb, "attn", T, C, NEXP, hsrc, hdst, W, scr, last)
        else:
            P.coll(lambda e: e.collective_compute("AllGather", ALU.bypass, replica_groups=groups,
                                                  ins=[hcur.ap()[T - 128:T, :]], outs=[halo_all.ap()[0:256, :]]), [], [], "cc")
            P.fence()
            W.update(w_in=win_d.ap()[j], w_grp=wgrp_d.ap()[j], lsT=lsT_d.ap()[j], w_out=wout_d.ap()[j],
                     am0=am0_d.ap(), amd=amd_d.ap(), amo=amo_d.ap(), hidx=hidx_d.ap())
            outs = tail_phase(P, nc, pb, "pool", T, C, NEXP, hsrc, hdst, W, scr, last)
        P.fence()
    P.emit(final_wait_ops=outs)
    return nc


_PROGS = {}
S_FULL = 8192
T_CORE = 4096
CAP = 384


def _prog(key):
    if key not in _PROGS:
        if key == "attn":
            _PROGS[key] = build_attn(S_FULL)
        elif key == "tail_attn":
            _PROGS[key] = build_tail("attn", T_CORE, CAP)
        else:
            _PROGS[key] = build_tail("pool", T_CORE, CAP)
    return _PROGS[key]


def _c(a):
    return np.ascontiguousarray(a, dtype=np.float32)


def mega_in_maps(inputs, S, C, depth):
    f32 = np.float32
    x = np.asarray(inputs["x"], dtype=f32)
    B = x.shape[0]
    T = S // 2
    NTL = T // 128
    NA = (depth + 1) // 2
    NP = depth // 2
    hc = host_consts(C)
    ac = attn_consts(S)
    wqkv = np.asarray(inputs["attn_w_qkv"], dtype=f32)[:NA]
    common = dict(
        w_o=_c(np.asarray(inputs["attn_w_o"])[:NA]),
        lqk=_c(np.stack([np.asarray(inputs[k])[:NA] for k in ("attn_lq1", "attn_lk1", "attn_lq2", "attn_lk2")], axis=1)),
        linit=np.array([[0.8 - 0.6 * math.exp(-0.3 * (2 * j))] for j in range(NA)], f32),
        subg=_c(np.asarray(inputs["attn_sub_g"])[:NA]),
        ln_g=_c(np.asarray(inputs["ln_g"])[:depth]), ln_b=_c(np.asarray(inputs["ln_b"])[:depth]),
        w_rt=_c(np.concatenate([np.asarray(inputs["moe_w_grp_router"])[:depth], np.asarray(inputs["moe_w_exp_router"])[:depth]], axis=2)),
        b_rt=_c(np.concatenate([np.asarray(inputs["moe_b_grp_router"])[:depth], np.asarray(inputs["moe_b_exp_router"])[:depth]], axis=1)),
        w_gate=_c(np.asarray(inputs["moe_w_gate"])[:depth]), w_up=_c(np.asarray(inputs["moe_w_up"])[:depth]),
        w_down=_c(np.asarray(inputs["moe_w_down"])[:depth]),
        cs=ac["cs"], trimask=ac["trimask"], **hc)
    if NP:
        pc = pool_consts(False)
        common.update(
            w_in=_c(np.asarray(inputs["pool_w_in"])[:NP]), w_grp=_c(np.asarray(inputs["pool_w_grp"])[:NP]),
            lsT=_c(np.asarray(inputs["pool_scale"], dtype=f32)[:NP].reshape(NP, 8, 128).transpose(0, 2, 1)),
            w_out=_c(np.asarray(inputs["pool_w_out"])[:NP]), amd=pc["amd"], amo=pc["amo"])
    p = np.arange(128, dtype=np.int64)[:, None]
    ch_o = min(1024, S)
    in_maps = []
    for c in range(NCORES):
        b, r = c // 2, c % 2
        oidx = np.zeros((128, NTL * 2), np.int32)
        for i in range(NTL):
            for g in range(2):
                t_ = r * T + i * 128 + p[:, 0]
                oidx[:, i * 2 + g] = (t_ // ch_o) * 2 * ch_o + g * ch_o + t_ % ch_o
        hidx = (p + (0 if r == 1 else 256)).astype(np.int32)
        m = dict(
            x_all=_c(x[b]), h0=_c(x[b, r * T:(r + 1) * T]), oidx=oidx, hidx=hidx,
            wq=_c(wqkv[:, :, r * 512:(r + 1) * 512]), wk=_c(wqkv[:, :, 1024 + r * 512:1024 + (r + 1) * 512]),
            wv=_c(wqkv[:, :, 2048 + r * 512:2048 + (r + 1) * 512]), **common)
        if NP:
            m["am0"] = pool_consts(r == 0)["am0"]
        in_maps.append(m)
    return in_maps


_MEGA = {}


def kernel(**inputs):
    x = np.asarray(inputs["x"])
    B, S, _ = x.shape
    if "m" not in _MEGA:
        _MEGA["m"] = build_mega(S, CAP)
    in_maps = mega_in_maps(inputs, S, CAP, DEPTH)
    res = run_bass_kernel_spmd(_MEGA["m"], in_maps, core_ids=list(range(NCORES)))
    T = S // 2
    out = np.empty((B, S, D), np.float32)
    for c in range(NCORES):
        out[c // 2, (c % 2) * T:(c % 2 + 1) * T] = res.results[c]["out"]
    return out


def kernel_unfused(**inputs):
    f32 = np.float32
    x = np.asarray(inputs["x"], dtype=f32)
    B, S, _ = x.shape
    h = x.reshape(B * S, D)
    cores = list(range(NCORES))
    hc = host_consts(CAP)
    ac = attn_consts(S)
    for i in range(DEPTH):
        j = i // 2
        tail_common = dict(
            ln_g=_c(inputs["ln_g"][i]), ln_b=_c(inputs["ln_b"][i]),
            w_rt=_c(np.concatenate([inputs["moe_w_grp_router"][i], inputs["moe_w_exp_router"][i]], axis=1)),
            b_rt=_c(np.concatenate([inputs["moe_b_grp_router"][i], inputs["moe_b_exp_router"][i]], axis=0).reshape(1, 36)),
            w_gate=_c(inputs["moe_w_gate"][i]), w_up=_c(inputs["moe_w_up"][i]), w_down=_c(inputs["moe_w_down"][i]),
            **hc)
        if i % 2 == 0:
            linit = 0.8 - 0.6 * math.exp(-0.3 * i)
            wqkv = np.asarray(inputs["attn_w_qkv"][j], dtype=f32)
            in_maps = []
            for c in cores:
                b, hg = c // 2, c % 2
                in_maps.append(dict(
                    xT=_c(h[b * S:(b + 1) * S].T),
                    wq=_c(wqkv[:, hg * 512:(hg + 1) * 512]),
                    wk=_c(wqkv[:, 1024 + hg * 512:1024 + (hg + 1) * 512]),
                    wv=_c(wqkv[:, 2048 + hg * 512:2048 + (hg + 1) * 512]),
                    lq1=_c(inputs["attn_lq1"][j]).reshape(1, 64), lk1=_c(inputs["attn_lk1"][j]).reshape(1, 64),
                    lq2=_c(inputs["attn_lq2"][j]).reshape(1, 64), lk2=_c(inputs["attn_lk2"][j]).reshape(1, 64),
                    linit=np.array([[linit]], f32), subg=_c(inputs["attn_sub_g"][j]).reshape(1, 128), **ac))
            res = run_bass_kernel_spmd(_prog("attn"), in_maps, core_ids=cores)
            o = np.empty((B * S, D), f32)
            for c in cores:
                b, hg = c // 2, c % 2
                o[b * S:(b + 1) * S, hg * 512:(hg + 1) * 512] = res.results[c]["o"]
            in_maps = []
            for c in cores:
                rows = slice(c * T_CORE, (c + 1) * T_CORE)
                in_maps.append(dict(h=_c(h[rows]), oT=_c(o[rows].T), w_o=_c(inputs["attn_w_o"][j]), **tail_common))
            res = run_bass_kernel_spmd(_prog("tail_attn"), in_maps, core_ids=cores)
        else:
            in_maps = []
            for c in cores:
                rows = slice(c * T_CORE, (c + 1) * T_CORE)
                first = (c % 2 == 0)
                halo = np.zeros((128, D), f32) if first else _c(h[c * T_CORE - 128:c * T_CORE])
                in_maps.append(dict(
                    h=_c(h[rows]), halo=halo, w_in=_c(inputs["pool_w_in"][j]), w_grp=_c(inputs["pool_w_grp"][j]),
                    lsT=_c(np.asarray(inputs["pool_scale"][j], dtype=f32).reshape(8, 128).T),
                    w_out=_c(inputs["pool_w_out"][j]), **pool_consts(first), **tail_common))
            res = run_bass_kernel_spmd(_prog("tail_pool"), in_maps, core_ids=cores)
        h = np.concatenate([res.results[c]["out"] for c in cores], axis=0)
    return h.reshape(B, S, D).astype(f32)
```
